# Optimizing a Trainium2 kernel written in Bass

```python
import math
import jax
import jax.numpy as jnp
from jax import lax
import numpy as np

D_MODEL = 1024
BATCH = 8
SEQ = 4096
DEPTH = 4

MEM_LEN = 256
HEAD_DIM = 64
N_BRANCH = 4
BRANCH_WIDTH = D_MODEL // N_BRANCH
FOX_HEADS = BRANCH_WIDTH // HEAD_DIM
DSA_HEADS = BRANCH_WIDTH // HEAD_DIM
IDX_HEADS = 8
IDX_DIM = 32
IDX_TOPK_MAX = 256
DIFF_HEADS = BRANCH_WIDTH // HEAD_DIM
DIFF_QK_DIM = HEAD_DIM // 2
DIFF_V_DIM = HEAD_DIM
MEM_HEADS = BRANCH_WIDTH // HEAD_DIM
FFN_HIDDEN = -(-8 * D_MODEL // (3 * 256)) * 256
ROPE_THETA = 500000.0
ROPE_FRACTION = 4
Q_BLOCK = 128
NORM_EPS = 1e-6

IN_SPLITS = (
    FOX_HEADS * HEAD_DIM, FOX_HEADS * HEAD_DIM, FOX_HEADS * HEAD_DIM, FOX_HEADS,
    DSA_HEADS * HEAD_DIM, HEAD_DIM, HEAD_DIM,
    IDX_HEADS * IDX_DIM, IDX_DIM, IDX_HEADS,
    DIFF_HEADS * 2 * DIFF_QK_DIM, DIFF_HEADS * 2 * DIFF_QK_DIM, DIFF_HEADS * DIFF_V_DIM,
    MEM_HEADS * HEAD_DIM,
)
IN_COLS = sum(IN_SPLITS)

kernel_name = "hybrid_fox_dsa_diff_memory_block"

F32 = jnp.float32


def rms_norm(x, g):
    xf = x.astype(F32)
    y = xf * lax.rsqrt(jnp.mean(xf * xf, axis=-1, keepdims=True) + NORM_EPS)
    return (y * g.astype(F32)).astype(x.dtype)


def rope_tables(positions, rot_dim):
    inv_freq = ROPE_THETA ** (-jnp.arange(0, rot_dim, 2, dtype=F32) / rot_dim)
    ang = positions.astype(F32)[..., None] * inv_freq
    return jnp.cos(ang)[:, :, None, :], jnp.sin(ang)[:, :, None, :]


def apply_partial_rope(x, cos, sin):
    half = cos.shape[-1]
    xf = x.astype(F32)
    x1, x2, rest = xf[..., :half], xf[..., half:2 * half], xf[..., 2 * half:]
    out = jnp.concatenate([x1 * cos - x2 * sin, x2 * cos + x1 * sin, rest], axis=-1)
    return out.astype(x.dtype)


def _to_blocks(a):
    b, s = a.shape[0], a.shape[1]
    return jnp.swapaxes(a.reshape(b, s // Q_BLOCK, Q_BLOCK, *a.shape[2:]), 0, 1)


def _from_blocks(a):
    nb, b, qb = a.shape[0], a.shape[1], a.shape[2]
    return jnp.swapaxes(a, 0, 1).reshape(b, nb * qb, *a.shape[3:])


def _query_positions(blk):
    return blk * Q_BLOCK + jnp.arange(Q_BLOCK)


def _causal_mask(blk, n_keys):
    return jnp.arange(n_keys)[None, :] <= _query_positions(blk)[:, None]


def forgetting_attention(q, k, v, f_logit):
    n_keys = k.shape[1]
    cum = jnp.cumsum(jax.nn.log_sigmoid(f_logit.astype(F32)), axis=1)
    cum_k = jnp.swapaxes(cum, 1, 2)
    scale = HEAD_DIM ** -0.5

    def block(args):
        qb, cb, blk = args
        s = jnp.einsum('bqhd,bkhd->bhqk', qb, k).astype(F32) * scale
        s = s + jnp.swapaxes(cb, 1, 2)[..., None] - cum_k[:, :, None, :]
        s = jnp.where(_causal_mask(blk, n_keys), s, -jnp.inf)
        p = jax.nn.softmax(s, axis=-1)
        return jnp.einsum('bhqk,bkhd->bqhd', p.astype(v.dtype), v)

    nb = q.shape[1] // Q_BLOCK
    out = lax.map(block, (_to_blocks(q), _to_blocks(cum), jnp.arange(nb)))
    return _from_blocks(out)


def dsa_attention(q, k, v, q_idx, k_idx, w_idx):
    n_keys = k.shape[1]
    top_k = min(IDX_TOPK_MAX, n_keys // 4)
    w_idx = w_idx.astype(F32) * IDX_HEADS ** -0.5
    scale = HEAD_DIM ** -0.5
    gather = jax.vmap(lambda kk, ii: kk[ii])

    def block(args):
        qb, qib, wb, blk = args
        rel = jax.nn.relu(jnp.einsum('bqhd,bkd->bqhk', qib, k_idx).astype(F32) * IDX_DIM ** -0.5)
        score = jnp.einsum('bqhk,bqh->bqk', rel, wb)
        score = jnp.where(_causal_mask(blk, n_keys)[None], score, -jnp.inf)
        _, sel = lax.top_k(score, top_k)
        k_sel = gather(k, sel)
        v_sel = gather(v, sel)
        valid = sel <= _query_positions(blk)[None, :, None]
        s = jnp.einsum('bqhd,bqkd->bhqk', qb, k_sel).astype(F32) * scale
        s = jnp.where(valid[:, None], s, -jnp.inf)
        p = jax.nn.softmax(s, axis=-1)
        return jnp.einsum('bhqk,bqkd->bqhd', p.astype(v.dtype), v_sel)

    nb = q.shape[1] // Q_BLOCK
    out = lax.map(block, (_to_blocks(q), _to_blocks(q_idx), _to_blocks(w_idx), jnp.arange(nb)))
    return _from_blocks(out)


def differential_attention(q, k, v, lam, subln_g, lambda_init):
    n_keys = k.shape[1]
    scale = DIFF_QK_DIM ** -0.5

    def block(args):
        qb, blk = args
        s = jnp.einsum('bqhmd,bkhmd->bmhqk', qb, k).astype(F32) * scale
        s = jnp.where(_causal_mask(blk, n_keys), s, -jnp.inf)
        p = jax.nn.softmax(s, axis=-1)
        a = p[:, 0] - lam * p[:, 1]
        return jnp.einsum('bhqk,bkhd->bqhd', a.astype(v.dtype), v)

    nb = q.shape[1] // Q_BLOCK
    out = _from_blocks(lax.map(block, (_to_blocks(q), jnp.arange(nb))))
    return rms_norm(out, subln_g) * (1.0 - lambda_init)


def memory_attention(q, mem, g, w_kv):
    b, m = mem.shape[0], mem.shape[1]
    kv = (rms_norm(mem, g) @ w_kv).reshape(b, m, 2, MEM_HEADS, HEAD_DIM)
    k, v = kv[:, :, 0], kv[:, :, 1]
    s = jnp.einsum('bshd,bmhd->bhsm', q, k).astype(F32) * HEAD_DIM ** -0.5
    p = jax.nn.softmax(s, axis=-1)
    return jnp.einsum('bhsm,bmhd->bshd', p.astype(v.dtype), v)


def setup_inputs(seed: int = 0) -> dict:
    key = jax.random.key(seed)
    ks = jax.random.split(key, 20)

    def nrm(k, shape, fan_in):
        return jax.random.normal(k, shape, F32) * fan_in ** -0.5

    def gain(k, shape):
        return 1.0 + 0.02 * jax.random.normal(k, shape, F32)

    x = jax.random.normal(ks[0], (BATCH, SEQ, D_MODEL), F32)
    mem = jax.random.normal(ks[1], (BATCH, MEM_LEN, D_MODEL), F32)
    positions = (jax.random.randint(ks[2], (BATCH, 1), 0, 1024, dtype=jnp.int32)
                 + jnp.arange(SEQ, dtype=jnp.int32)[None, :])
    return {
        "x": x,
        "mem": mem,
        "positions": positions,
        "norm_mix_g": gain(ks[3], (DEPTH, D_MODEL)),
        "w_in": nrm(ks[4], (DEPTH, D_MODEL, IN_COLS), D_MODEL),
        "b_forget": jax.random.uniform(ks[5], (DEPTH, FOX_HEADS), F32, minval=2.0, maxval=5.0),
        "diff_lambda": 0.1 * jax.random.normal(ks[6], (DEPTH, 4, DIFF_QK_DIM), F32),
        "diff_subln_g": gain(ks[7], (DEPTH, DIFF_V_DIM)),
        "norm_mem_g": gain(ks[8], (DEPTH, D_MODEL)),
        "w_mem_kv": nrm(ks[9], (DEPTH, D_MODEL, 2 * MEM_HEADS * HEAD_DIM), D_MODEL),
        "w_branch": nrm(ks[10], (DEPTH, N_BRANCH, BRANCH_WIDTH, D_MODEL), BRANCH_WIDTH),
        "w_gate": nrm(ks[11], (DEPTH, D_MODEL, N_BRANCH * D_MODEL), D_MODEL),
        "b_gate": 0.02 * jax.random.normal(ks[12], (DEPTH, N_BRANCH * D_MODEL), F32),
        "w_out": nrm(ks[13], (DEPTH, D_MODEL, D_MODEL), D_MODEL),
        "norm_ffn_g": gain(ks[14], (DEPTH, D_MODEL)),
        "w_ffn_in": nrm(ks[15], (DEPTH, D_MODEL, 2 * FFN_HIDDEN), D_MODEL),
        "w_ffn_out": nrm(ks[16], (DEPTH, FFN_HIDDEN, D_MODEL), FFN_HIDDEN),
        "final_norm_g": gain(ks[17], (D_MODEL,)),
    }


def reference(x, mem, positions, norm_mix_g, w_in, b_forget, diff_lambda, diff_subln_g,
              norm_mem_g, w_mem_kv, w_branch, w_gate, b_gate, w_out, norm_ffn_g,
              w_ffn_in, w_ffn_out, final_norm_g):
    b, s = x.shape[0], x.shape[1]
    cos16, sin16 = rope_tables(positions, HEAD_DIM // ROPE_FRACTION)
    cos8, sin8 = rope_tables(positions, DIFF_QK_DIM // ROPE_FRACTION)

    for l in range(DEPTH):
        lambda_init = 0.8 - 0.6 * math.exp(-0.3 * l)
        h = rms_norm(x, norm_mix_g[l])
        z = h @ w_in[l]
        parts = []
        off = 0
        for n in IN_SPLITS:
            parts.append(z[..., off:off + n])
            off += n
        (qa, ka, va, fa, qb, kb, vb, qi, ki, wi, qc, kc, vc, qm) = parts

        o_a = forgetting_attention(qa.reshape(b, s, FOX_HEADS, HEAD_DIM),
                                   ka.reshape(b, s, FOX_HEADS, HEAD_DIM),
                                   va.reshape(b, s, FOX_HEADS, HEAD_DIM),
                                   fa + b_forget[l])

        qb = apply_partial_rope(qb.reshape(b, s, DSA_HEADS, HEAD_DIM), cos16, sin16)
        kb = apply_partial_rope(kb[:, :, None, :], cos16, sin16)[:, :, 0]
        qi = apply_partial_rope(qi.reshape(b, s, IDX_HEADS, IDX_DIM), cos8, sin8)
        ki = apply_partial_rope(ki[:, :, None, :], cos8, sin8)[:, :, 0]
        o_b = dsa_attention(qb, kb, vb, qi, ki, wi)

        qc = apply_partial_rope(qc.reshape(b, s, DIFF_HEADS * 2, DIFF_QK_DIM), cos8, sin8)
        kc = apply_partial_rope(kc.reshape(b, s, DIFF_HEADS * 2, DIFF_QK_DIM), cos8, sin8)
        lam_vec = diff_lambda[l].astype(F32)
        lam = (jnp.exp(jnp.sum(lam_vec[0] * lam_vec[1]))
               - jnp.exp(jnp.sum(lam_vec[2] * lam_vec[3])) + lambda_init)
        o_c = differential_attention(qc.reshape(b, s, DIFF_HEADS, 2, DIFF_QK_DIM),
                                     kc.reshape(b, s, DIFF_HEADS, 2, DIFF_QK_DIM),
                                     vc.reshape(b, s, DIFF_HEADS, DIFF_V_DIM),
                                     lam, diff_subln_g[l], lambda_init)

        o_m = memory_attention(qm.reshape(b, s, MEM_HEADS, HEAD_DIM), mem, norm_mem_g[l], w_mem_kv[l])

        o = jnp.stack([o_a.reshape(b, s, BRANCH_WIDTH), o_b.reshape(b, s, BRANCH_WIDTH),
                       o_c.reshape(b, s, BRANCH_WIDTH), o_m.reshape(b, s, BRANCH_WIDTH)], axis=2)
        y = jnp.einsum('bsnc,ncd->bsnd', o, w_branch[l])
        g = jax.nn.sigmoid(h @ w_gate[l] + b_gate[l]).reshape(b, s, N_BRANCH, D_MODEL)
        x = x + jnp.sum(g * y, axis=2) @ w_out[l]

        h2 = rms_norm(x, norm_ffn_g[l])
        u = h2 @ w_ffn_in[l]
        x = x + (jax.nn.silu(u[..., :FFN_HIDDEN]) * u[..., FFN_HIDDEN:]) @ w_ffn_out[l]

    return rms_norm(x, final_norm_g)
```

```python
import math
from contextlib import ExitStack, contextmanager
import numpy as np
import concourse.bass as bass
import concourse.mybir as mybir
from concourse.bass_utils import run_bass_kernel_spmd

F32 = mybir.dt.float32
BF16 = mybir.dt.bfloat16
I32 = mybir.dt.int32
ALU = mybir.AluOpType
AF = mybir.ActivationFunctionType

D = 1024
KC = 8
MEM = 256
FFH = 2816
EPS = 1e-6
NEG_S = -1.0e4
NEG_B = -30000.0
TOPK = 256
NBIS = 14
import os
STOP = int(os.environ.get('KSTOP', '0'))
ROPE_THETA = 500000.0
TWO_PI = 2.0 * math.pi
CW_HI = 6.28125
CW_LO = TWO_PI - CW_HI


class Res:
    __slots__ = ("w", "rs")

    def __init__(self):
        self.w = None
        self.rs = {}


class KB:
    def __init__(self):
        self.nc = bass.Bass("TRN2", target_bir_lowering=False)
        nc = self.nc
        self.ges = ExitStack()
        self.E = {"pe": nc.tensor, "act": nc.scalar, "dve": nc.vector, "pool": nc.gpsimd, "sp": nc.sync}
        self.sems = {}
        self.cnt = {}
        self.cur = {}
        self.known = {n: {} for n in self.E}
        self.epoch = -1
        self.nid = 0
        self.new_epoch()

    def new_epoch(self):
        self.epoch += 1
        for n in ("pe", "act", "dve", "pool"):
            nm = "%s#%d" % (n, self.epoch)
            self.sems[nm] = self.ges.enter_context(self.nc.semaphore("e_%s_%d" % (n, self.epoch)))
            self.cnt[nm] = 0
            self.cur[n] = nm

    def chan(self, name):
        nm = "c_" + name
        if nm not in self.sems:
            self.sems[nm] = self.ges.enter_context(self.nc.semaphore(nm))
            self.cnt[nm] = 0
        return nm

    def name(self, p):
        self.nid += 1
        return "%s_%d" % (p, self.nid)

    def _waits(self, eng, r, w):
        deps = {}
        for x in r:
            if x.w is not None and deps.get(x.w[0], 0) < x.w[1]:
                deps[x.w[0]] = x.w[1]
        for x in w:
            if x.w is not None and deps.get(x.w[0], 0) < x.w[1]:
                deps[x.w[0]] = x.w[1]
            for s, v in x.rs.items():
                if deps.get(s, 0) < v:
                    deps[s] = v
        kn = self.known[eng]
        for s, v in deps.items():
            if eng == "pe" and s.startswith("pe#"):
                continue
            if kn.get(s, 0) >= v:
                continue
            self.E[eng].wait_ge(self.sems[s], v)
            kn[s] = v

    def _post(self, tok, r, w):
        s, v = tok
        for x in r:
            if x.rs.get(s, 0) < v:
                x.rs[s] = v
        for x in w:
            x.w = tok
            x.rs = {}

    def op(self, eng, fn, r=(), w=()):
        self._waits(eng, r, w)
        ins = fn(self.E[eng])
        s = self.cur[eng]
        self.cnt[s] += 1
        ins.then_inc(self.sems[s], 1)
        self._post((s, self.cnt[s]), r, w)

    def dma(self, q, out, in_, chan, r=(), w=()):
        self._waits(q, r, w)
        ins = self.E[q].dma_start(out=out, in_=in_)
        c = self.chan(chan)
        self.cnt[c] += 16
        ins.then_inc(self.sems[c], 16)
        self._post((c, self.cnt[c]), r, w)

    def barrier(self):
        for e in self.E:
            kn = self.known[e]
            for s, v in self.cnt.items():
                if v == 0 or kn.get(s, 0) >= v:
                    continue
                if e == "pe" and s.startswith("pe#"):
                    continue
                self.E[e].wait_ge(self.sems[s], v)
                kn[s] = v

    @contextmanager
    def phase(self):
        es = ExitStack()
        with es:
            yield Phase(self, es)
            self.barrier()


class Phase:
    def __init__(self, kb, es):
        self.kb = kb
        self.es = es

    def sb(self, shape, dt, nm="t"):
        return self.es.enter_context(self.kb.nc.sbuf_tensor(self.kb.name(nm), list(shape), dt))

    def ps(self, shape, dt, nm="p"):
        return self.es.enter_context(self.kb.nc.psum_tensor(self.kb.name(nm), list(shape), dt))

    def ring(self, n, shape, dt, nm="r", psum=False):
        items = []
        for i in range(n):
            t = self.ps(shape, dt, nm) if psum else self.sb(shape, dt, nm)
            items.append((t, Res(), "%s%d" % (nm, i)))
        return Ring(items)


class Ring:
    def __init__(self, items):
        self.items = items
        self.i = -1

    def next(self):
        self.i = (self.i + 1) % len(self.items)
        return self.items[self.i]


def mm(kb, out, lhsT, rhs, start, stop, r, w, tp=None):
    if tp is None:
        kb.op("pe", lambda e: e.matmul(out, lhsT=lhsT, rhs=rhs, start=start, stop=stop), r=r, w=w)
    else:
        kb.op("pe", lambda e: e.matmul(out, lhsT=lhsT, rhs=rhs, start=start, stop=stop, tile_position=tp), r=r, w=w)


def pbase_tp(pb):
    return (96, 0) if pb == 96 else None


class Prog:
    def __init__(self, S, depth, branches=(0, 1, 2, 3), debug=False):
        self.S = S
        self.depth = depth
        self.branches = tuple(branches)
        self.NT = S // 128
        self.NCH = S // 512
        self.kb = KB()
        self.nc = self.kb.nc
        self.debug = debug
        self.build()

    def dram_in(self, name, shape, dt=F32):
        return self.nc.dram_tensor(name, list(shape), dt, kind="ExternalInput").ap()

    def build(self):
        kb, nc, S, L = self.kb, self.nc, self.S, self.depth
        di = self.dram_in
        self.x_in = di("x", [S, D])
        self.mem_in = di("mem", [MEM, D])
        self.pos_in = di("pos", [1, S], I32)
        self.cmat_in = di("cmat", [128, 3 * 128])
        self.ccol_in = di("ccol", [128, 4])
        self.g_mix = di("g_mix", [L, 1, D])
        self.g_ffn = di("g_ffn", [L, 1, D])
        self.g_mem = di("g_mem", [L, 1, D])
        self.g_fin = di("g_fin", [1, D])
        self.wA_fm = di("wA_fm", [L, D, 512])
        self.wA_tm = di("wA_tm", [L, D, 260])
        self.wB_fm = di("wB_fm", [L, D, 1536])
        self.wB_tm = di("wB_tm", [L, D, 72])
        self.wC_fm = di("wC_fm", [L, D, 1024])
        self.wC_tm = di("wC_tm", [L, D, 256])
        self.wM_fm = di("wM_fm", [L, D, 256])
        self.w_kv = di("w_kv", [L, D, 512])
        self.b_forget = di("b_forget", [L, 1, 4])
        self.dlam = di("dlam", [L, 1, 128])
        self.subg = di("subg", [L, 128, 1])
        self.w_branch = di("w_branch", [L, 4, 256, D])
        self.w_gate = di("w_gate", [L, D, 4096])
        self.b_gate = di("b_gate", [L, 128, 32])
        self.w_out = di("w_out", [L, D, D])
        self.w_f1 = di("w_f1", [L, D, 2 * FFH])
        self.w_f2 = di("w_f2", [L, FFH, D])
        self.out = nc.dram_tensor("out", [S, D], F32, kind="ExternalOutput").ap()
        it = lambda name, shape, dt: nc.dram_tensor(name, list(shape), dt, kind="Internal").ap()
        self.xres = it("xres", [S, D], F32)
        self.hT = it("hT", [KC, 128, S], BF16)
        self.h2T = it("h2T", [KC, 128, S], BF16)
        self.oT = it("oT", [4, 256, S], BF16)
        self.rope = it("rope", [4, 128, S], F32)

        gsb = lambda shape, dt, nm: kb.ges.enter_context(nc.sbuf_tensor(kb.name(nm), list(shape), dt))
        self.cm = gsb([128, 3, 128], F32, "cm")
        self.cm_r = Res()
        self.ccol = gsb([128, 4], F32, "ccol")
        self.identb = gsb([128, 128], BF16, "identb")
        self.trib = gsb([128, 128], BF16, "trib")
        self.cnegb = gsb([128, 128], BF16, "cnegb")
        self.onesf = gsb([128, 128], F32, "onesf")
        self.onesb = gsb([128, 128], BF16, "onesb")
        self.cst_r = Res()
        kb.dma("sp", self.cm[:].rearrange("p a b -> p (a b)"), self.cmat_in, "cst", w=[self.cm_r])
        kb.dma("sp", self.ccol[:], self.ccol_in, "cst", w=[self.cm_r])
        kb.op("dve", lambda e: e.tensor_copy(out=self.identb[:], in_=self.cm[:, 0, :]), r=[self.cm_r], w=[self.cst_r])
        kb.op("dve", lambda e: e.tensor_copy(out=self.trib[:], in_=self.cm[:, 1, :]), r=[self.cm_r], w=[self.cst_r])
        kb.op("dve", lambda e: e.tensor_scalar(out=self.cnegb[:], in0=self.cm[:, 2, :], scalar1=NEG_B / NEG_S,
                                               scalar2=None, op0=ALU.mult), r=[self.cm_r], w=[self.cst_r])
        kb.op("pool", lambda e: e.memset(self.onesf[:], 1.0), w=[self.cst_r])
        kb.op("pool", lambda e: e.memset(self.onesb[:], 1.0), w=[self.cst_r])
        self.identf = self.cm[:, 0, :]
        self.trif = self.cm[:, 1, :]
        self.cnegf = self.cm[:, 2, :]

        self.phase_rope()
        self.phase_norm0()
        for l in range(L):
            if 0 in self.branches:
                self.phase_fox(l)
            if 1 in self.branches:
                self.phase_dsa(l)
            if 2 in self.branches:
                self.phase_diff(l)
            if 3 in self.branches:
                self.phase_mem(l)
            self.phase_merge(l)
            self.phase_ffn(l, 0, 11, False)
            self.phase_ffn(l, 11, 22, True)
            kb.new_epoch()
        kb.barrier()

    def load_w(self, dst, dst_r, src, nk, chan, q="pool"):
        for k in range(nk):
            self.kb.dma(q, dst[:, k, :], src[k * 128:(k + 1) * 128, :], chan, w=[dst_r])

    def load_bc(self, dst, dst_r, src_row, n, chan):
        self.kb.dma("sp", dst, src_row.to_broadcast([128, n]), chan, w=[dst_r])

    def phase_rope(self):
        kb, S = self.kb, self.S
        with kb.phase() as ph:
            posi = ph.sb([128, S], I32)
            posf = ph.sb([128, S], F32)
            A = ph.sb([128, S], F32)
            B = ph.sb([128, S], F32)
            C = ph.sb([128, S], I32)
            Dd = ph.sb([128, S], F32)
            r_pos, r_A, r_B, r_C, r_D = Res(), Res(), Res(), Res(), Res()
            kb.dma("sp", posi[:], self.pos_in.to_broadcast([128, S]), "rp", w=[r_pos])
            kb.op("dve", lambda e: e.tensor_copy(out=posf[:], in_=posi[:]), r=[r_pos], w=[r_pos])
            for pat in range(2):
                invf = self.ccol[:, pat:pat + 1]
                sgn = self.ccol[:, 2 + pat:3 + pat]
                kb.op("dve", lambda e: e.tensor_scalar(out=A[:], in0=posf[:], scalar1=invf, scalar2=None, op0=ALU.mult),
                      r=[r_pos, self.cm_r], w=[r_A])
                for which in range(2):
                    off = math.pi / 2 if which == 0 else 0.0
                    kb.op("dve", lambda e: e.tensor_scalar(out=B[:], in0=A[:], scalar1=off, scalar2=None, op0=ALU.add),
                          r=[r_A], w=[r_B])
                    kb.op("dve", lambda e: e.tensor_scalar(out=Dd[:], in0=B[:], scalar1=1.0 / TWO_PI, scalar2=None,
                                                           op0=ALU.mult), r=[r_B], w=[r_D])
                    kb.op("dve", lambda e: e.tensor_copy(out=C[:], in_=Dd[:]), r=[r_D], w=[r_C])
                    kb.op("dve", lambda e: e.tensor_copy(out=Dd[:], in_=C[:]), r=[r_C], w=[r_D])
                    kb.op("dve", lambda e: e.scalar_tensor_tensor(out=B[:], in0=Dd[:], scalar=-CW_HI, in1=B[:],
                                                                  op0=ALU.mult, op1=ALU.add), r=[r_D, r_B], w=[r_B])
                    kb.op("dve", lambda e: e.scalar_tensor_tensor(out=B[:], in0=Dd[:], scalar=-CW_LO, in1=B[:],
                                                                  op0=ALU.mult, op1=ALU.add), r=[r_D, r_B], w=[r_B])
                    kb.op("dve", lambda e: e.tensor_scalar(out=Dd[:], in0=B[:], scalar1=math.pi, scalar2=-TWO_PI,
                                                           op0=ALU.is_gt, op1=ALU.mult), r=[r_B], w=[r_D])
                    kb.op("dve", lambda e: e.tensor_tensor(out=B[:], in0=B[:], in1=Dd[:], op=ALU.add), r=[r_B, r_D], w=[r_B])
                    kb.op("dve", lambda e: e.tensor_scalar(out=B[:], in0=B[:], scalar1=-math.pi, scalar2=math.pi,
                                                           op0=ALU.max, op1=ALU.min), r=[r_B], w=[r_B])
                    kb.op("act", lambda e: e.activation(out=Dd[:], in_=B[:], func=AF.Sin), r=[r_B], w=[r_D])
                    if which == 1:
                        kb.op("dve", lambda e: e.tensor_scalar(out=Dd[:], in0=Dd[:], scalar1=sgn, scalar2=None,
                                                               op0=ALU.mult), r=[r_D, self.cm_r], w=[r_D])
                    kb.dma("sp", self.rope[2 * pat + which], Dd[:], "rp", r=[r_D])

    class Normer:
        def __init__(self, prog, ph, g_row, chan):
            kb = prog.kb
            self.prog, self.ph = prog, ph
            self.gbc = ph.sb([128, D], F32, "gbc")
            self.gbc_r = Res()
            prog.load_bc(self.gbc[:], self.gbc_r, g_row, D, chan)
            self.junk = ph.ring(1, [128, D], BF16, "njunk")
            self.st = ph.ring(3, [128, 4], F32, "nst")
            self.hb = ph.ring(2, [128, D], BF16, "nhb")
            self.pT = ph.ring(1, [128, KC, 128], BF16, "npT", psum=True)

        def stats(self, xt, xt_r):
            kb = self.prog.kb
            junk, junk_r, _ = self.junk.next()
            st, st_r, _ = self.st.next()
            kb.op("act", lambda e: e.activation(out=junk[:], in_=xt, func=AF.Square, accum_out=st[:, 0:1]),
                  r=[xt_r], w=[junk_r, st_r])
            kb.op("dve", lambda e: e.tensor_scalar(out=st[:, 1:2], in0=st[:, 0:1], scalar1=1.0 / D, scalar2=EPS,
                                                   op0=ALU.mult, op1=ALU.add), r=[st_r], w=[st_r])
            kb.op("act", lambda e: e.activation(out=st[:, 2:3], in_=st[:, 1:2], func=AF.Ln), r=[st_r], w=[st_r])
            kb.op("act", lambda e: e.activation(out=st[:, 3:4], in_=st[:, 2:3], func=AF.Exp, scale=-0.5), r=[st_r], w=[st_r])
            return st[:, 3:4], st_r

        def to_hT(self, xt, xt_r, dst, dst_r):
            kb, prog = self.prog.kb, self.prog
            rstd, st_r = self.stats(xt, xt_r)
            hb, hb_r, _ = self.hb.next()
            kb.op("dve", lambda e: e.scalar_tensor_tensor(out=hb[:], in0=xt, scalar=rstd, in1=self.gbc[:],
                                                          op0=ALU.mult, op1=ALU.mult), r=[xt_r, st_r, self.gbc_r], w=[hb_r])
            pT, pT_r, _ = self.pT.next()
            for k in range(KC):
                kb.op("pe", lambda e: e.transpose(pT[:, k, :], hb[:, k * 128:(k + 1) * 128], prog.identb[:]),
                      r=[hb_r, prog.cst_r], w=[pT_r])
            kb.op("act", lambda e: e.activation(out=dst, in_=pT[:], func=AF.Copy), r=[pT_r], w=[dst_r])

        def to_out(self, xt, xt_r, dst, dst_r):
            kb = self.prog.kb
            rstd, st_r = self.stats(xt, xt_r)
            kb.op("dve", lambda e: e.scalar_tensor_tensor(out=dst, in0=xt, scalar=rstd, in1=self.gbc[:],
                                                          op0=ALU.mult, op1=ALU.mult), r=[xt_r, st_r, self.gbc_r], w=[dst_r])

    def phase_norm0(self):
        kb = self.kb
        with kb.phase() as ph:
            nm = Prog.Normer(self, ph, self.g_mix[0], "n0g")
            xr = ph.ring(3, [128, D], F32, "n0x")
            hc = ph.ring(2, [128, KC, 512], BF16, "n0h")
            for c in range(self.NCH):
                h, h_r, h_ch = hc.next()
                for i in range(4):
                    t = 4 * c + i
                    xt, xt_r, x_ch = xr.next()
                    kb.dma("sp", xt[:], self.x_in[t * 128:(t + 1) * 128, :], x_ch, w=[xt_r])
                    nm.to_hT(xt[:], xt_r, h[:, :, i * 128:(i + 1) * 128], h_r)
                kb.dma("sp", self.hT[:, :, c * 512:(c + 1) * 512].rearrange("k p s -> p k s"), h[:], h_ch, r=[h_r])

    def attn_finalize(self, po, po_r, rd_ring, dst, dst_r, eng_mul="dve"):
        kb = self.kb
        rd, rd_r, _ = rd_ring.next()
        kb.op("dve", lambda e: e.reciprocal(out=rd[64:128, :], in_=po[64:128, :]), r=[po_r], w=[rd_r])
        kb.op("dve", lambda e: e.tensor_tensor(out=dst, in0=po[0:64, :], in1=rd[64:128, :], op=ALU.mult),
              r=[po_r, rd_r], w=[dst_r])

    def causal_attn(self, *args, **kw):
        for _ in self.causal_attn_gen(*args, **kw):
            pass

    def causal_attn_gen(self, c, kT_ap, kT_r, qT_ap, qT_r, V_ap, V_r, ps_ring, pt_ring, po, po_r, scale,
                        bias_fn=None, bias_r=(), mask_fn=None, tp=None):
        kb = self.kb
        J = 4 * c + 4
        sts = {}

        def emit_S(j):
            i = j - 4 * c
            col0 = max(i, 0) * 128
            ps, ps_r, _ = ps_ring.next()
            extra = mask_fn(j, col0) if mask_fn is not None else []
            mm(kb, ps[:, col0:512], kT_ap(j), qT_ap(col0), True, len(extra) == 0, [kT_r(j), qT_r], [ps_r], tp)
            for n, (lt, rh, c0, c1, rr) in enumerate(extra):
                mm(kb, ps[:, c0:c1], lt, rh, False, n == len(extra) - 1, rr, [ps_r])
            sts[j] = (ps, ps_r, col0, i)

        emit_S(0)
        for j in range(J):
            if j + 1 < J:
                emit_S(j + 1)
            ps, ps_r, col0, i = sts.pop(j)
            pt, pt_r, _ = pt_ring.next()
            if bias_fn is not None:
                b = bias_fn(j)
                kb.op("act", lambda e: e.activation(out=pt[:, col0:512], in_=ps[:, col0:512], func=AF.Exp, scale=scale, bias=b),
                      r=[ps_r] + list(bias_r), w=[pt_r])
            else:
                kb.op("act", lambda e: e.activation(out=pt[:, col0:512], in_=ps[:, col0:512], func=AF.Exp, scale=scale),
                      r=[ps_r], w=[pt_r])
            if i >= 0:
                kb.op("pool", lambda e: e.tensor_tensor(out=pt[:, col0:col0 + 128], in0=pt[:, col0:col0 + 128],
                                                        in1=self.trib[:], op=ALU.mult), r=[pt_r, self.cst_r], w=[pt_r])
            mm(kb, po[:, col0:512], V_ap(j), pt[:, col0:512], j == 0, j == J - 1, [V_r(j), pt_r], [po_r])
            yield

    def store_oT(self, n, c, oTs, oTs_r, chan):
        self.kb.dma("sp", self.oT[n].rearrange("(h p) s -> p h s", p=64)[:, :, c * 512:(c + 1) * 512], oTs[0:64, :, :],
                    chan, r=[oTs_r])

    def proj_fm(self, ps, ps_r, W, W_r, col, h, h_r, n=512):
        for k in range(KC):
            mm(self.kb, ps[:, 0:n], W[:, k, col:col + 128], h[:, k, 0:n], k == 0, k == KC - 1, [W_r, h_r], [ps_r])

    def proj_tm(self, ps, ps_r, W, W_r, col, ncol, h, h_r, i):
        for k in range(KC):
            mm(self.kb, ps[:, 0:ncol], h[:, k, i * 128:(i + 1) * 128], W[:, k, col:col + ncol], k == 0, k == KC - 1,
               [W_r, h_r], [ps_r])

    def load_hT(self, src, ring, c):
        h, h_r, h_ch = ring.next()
        self.kb.dma("sp", h[:], src[:, :, c * 512:(c + 1) * 512].rearrange("k p s -> p k s"), h_ch, w=[h_r])
        return h, h_r

    def rope_evac(self, q_ps, q_r, p_ps, p_r, Ct, St, tab_r, tmp_ring, dst, dst_r):
        kb = self.kb
        t1, t1_r, _ = tmp_ring.next()
        t2, t2_r, _ = tmp_ring.next()
        kb.op("dve", lambda e: e.tensor_tensor(out=t1[:], in0=p_ps, in1=St, op=ALU.mult), r=[p_r, tab_r], w=[t1_r])
        kb.op("dve", lambda e: e.tensor_tensor(out=t2[:], in0=q_ps, in1=Ct, op=ALU.mult), r=[q_r, tab_r], w=[t2_r])
        kb.op("pool", lambda e: e.tensor_tensor(out=dst, in0=t1[:], in1=t2[:], op=ALU.add), r=[t1_r, t2_r], w=[dst_r])

    def phase_fox(self, l):
        kb, S, NT, NCH = self.kb, self.S, self.NT, self.NCH
        with kb.phase() as ph:
            W = ph.sb([128, KC, 772], BF16, "fW")
            W_r = Res()
            self.load_w(W[:, :, 0:512], W_r, self.wA_fm[l], KC, "fW")
            self.load_w(W[:, :, 512:772], W_r, self.wA_tm[l], KC, "fW")
            kT = ph.sb([128, 2, S], BF16, "fkT")
            kT_rs = [Res() for _ in range(NCH)]
            V1 = ph.sb([128, NT, 4, 128], BF16, "fV1")
            V_rs = [Res() for _ in range(NT)]
            ones_r = Res()
            kb.op("pool", lambda e: e.memset(V1[:, :, :, 64:128], 1.0), w=[ones_r])
            for r_ in V_rs:
                r_.w = ones_r.w
            cumn = ph.sb([128, NT, 4], F32, "fcum")
            carry = ph.sb([128, NT + 1, 4], F32, "fcar")
            cum_r = Res()
            kb.op("pool", lambda e: e.memset(carry[:, 0, :], 0.0), w=[cum_r])
            bfg = ph.sb([128, 4], F32, "fbf")
            bfg_r = Res()
            self.load_bc(bfg[:], bfg_r, self.b_forget[l], 4, "fbf")
            hring = ph.ring(2, [128, KC, 512], BF16, "fh")
            qring = ph.ring(2, [128, 2, 512], BF16, "fq")
            pp = ph.ring(2, [128, 512], F32, "fpp", psum=True)
            psr = ph.ring(3, [128, 512], F32, "fps", psum=True)
            por = ph.ring(2, [128, 512], F32, "fpo", psum=True)
            pcs = ph.ring(1, [128, 8], F32, "fpc", psum=True)
            ptr = ph.ring(3, [128, 512], BF16, "fpt")
            rdr = ph.ring(2, [128, 512], F32, "frd")
            otr = ph.ring(2, [128, 4, 512], BF16, "fot")
            fsm = ph.ring(2, [128, 12], F32, "fsm")
            fhl = ph.ring(2, [128, 8], BF16, "fhl")
            bias = ph.ring(2, [128, NT, 4], F32, "fbi")
            for c in range(NCH):
                h, h_r = self.load_hT(self.hT, hring, c)
                qT, qT_r, _ = qring.next()
                for ci in range(2):
                    ps, ps_r, _ = pp.next()
                    self.proj_fm(ps, ps_r, W, W_r, ci * 128, h, h_r)
                    kb.op("act", lambda e: e.activation(out=qT[:, ci, :], in_=ps[:], func=AF.Copy), r=[ps_r], w=[qT_r])
                for ci in range(2):
                    ps, ps_r, _ = pp.next()
                    self.proj_fm(ps, ps_r, W, W_r, 256 + ci * 128, h, h_r)
                    kb.op("dve", lambda e: e.tensor_copy(out=kT[:, ci, c * 512:(c + 1) * 512], in_=ps[:]), r=[ps_r], w=[kT_rs[c]])
                for i in range(4):
                    t = 4 * c + i
                    ps, ps_r, _ = pp.next()
                    self.proj_tm(ps, ps_r, W, W_r, 512, 260, h, h_r, i)
                    kb.op("act", lambda e: e.activation(out=V1[:, t, :, 0:64], in_=ps[:, 0:256].rearrange("p (h d) -> p h d", h=4),
                                                        func=AF.Copy), r=[ps_r], w=[V_rs[t]])
                    if STOP == 1:
                        continue
                    sm, sm_r, _ = fsm.next()
                    kb.op("dve", lambda e: e.tensor_tensor(out=sm[:, 0:4], in0=ps[:, 256:260], in1=bfg[:], op=ALU.add),
                          r=[ps_r, bfg_r], w=[sm_r])
                    kb.op("act", lambda e: e.activation(out=sm[:, 4:8], in_=sm[:, 0:4], func=AF.Exp, scale=-1.0), r=[sm_r], w=[sm_r])
                    kb.op("act", lambda e: e.activation(out=sm[:, 8:12], in_=sm[:, 4:8], func=AF.Ln, bias=1.0), r=[sm_r], w=[sm_r])
                    hl, hl_r, _ = fhl.next()
                    kb.op("dve", lambda e: e.tensor_copy(out=hl[:, 0:4], in_=sm[:, 8:12]), r=[sm_r], w=[hl_r])
                    kb.op("dve", lambda e: e.tensor_copy(out=sm[:, 4:8], in_=hl[:, 0:4]), r=[hl_r], w=[sm_r])
                    kb.op("dve", lambda e: e.tensor_tensor(out=hl[:, 4:8], in0=sm[:, 8:12], in1=sm[:, 4:8], op=ALU.subtract),
                          r=[sm_r], w=[hl_r])
                    pc, pc_r, _ = pcs.next()
                    mm(kb, pc[:, 0:4], self.trib[:], hl[:, 0:4], True, False, [hl_r, self.cst_r], [pc_r])
                    mm(kb, pc[:, 0:4], self.trib[:], hl[:, 4:8], False, True, [hl_r, self.cst_r], [pc_r])
                    mm(kb, pc[:, 4:8], self.onesb[:], hl[:, 0:4], True, False, [hl_r, self.cst_r], [pc_r])
                    mm(kb, pc[:, 4:8], self.onesb[:], hl[:, 4:8], False, True, [hl_r, self.cst_r], [pc_r])
                    kb.op("dve", lambda e: e.tensor_tensor(out=cumn[:, t, :], in0=pc[:, 0:4], in1=carry[:, t, :], op=ALU.add),
                          r=[pc_r, cum_r], w=[cum_r])
                    kb.op("dve", lambda e: e.tensor_tensor(out=carry[:, t + 1, :], in0=pc[:, 4:8], in1=carry[:, t, :], op=ALU.add),
                          r=[pc_r, cum_r], w=[cum_r])
                if STOP in (1, 2):
                    continue
                J = 4 * c + 4
                bi, bi_r, _ = bias.next()
                kb.op("dve", lambda e: e.tensor_tensor(out=bi[:, 0:J, :], in0=cumn[:, 0:J, :],
                                                       in1=carry[:, 4 * c + 2:4 * c + 3, :].to_broadcast([128, J, 4]),
                                                       op=ALU.subtract), r=[cum_r], w=[bi_r])
                if STOP == 3:
                    continue
                oTs, oTs_r, o_ch = otr.next()
                for hd in range(4):
                    cb, pb = hd // 2, (hd % 2) * 64
                    po, po_r, _ = por.next()
                    self.causal_attn(
                        c,
                        lambda j: kT[pb:pb + 64, cb, j * 128:(j + 1) * 128], lambda j: kT_rs[j // 4],
                        lambda col0: qT[pb:pb + 64, cb, col0:512], qT_r,
                        lambda j: V1[:, j, hd, :], lambda j: V_rs[j],
                        psr, ptr, po, po_r, 0.125,
                        bias_fn=lambda j: bi[:, j, hd:hd + 1], bias_r=[bi_r])
                    if STOP == 4:
                        continue
                    self.attn_finalize(po, po_r, rdr, oTs[0:64, hd, :], oTs_r)
                if STOP in (4, 5):
                    continue
                self.store_oT(0, c, oTs, oTs_r, o_ch)

    def phase_dsa(self, l):
        kb, S, NT, NCH = self.kb, self.S, self.NT, self.NCH
        with kb.phase() as ph:
            W = ph.sb([128, KC, 1608], BF16, "bW")
            W_r = Res()
            self.load_w(W[:, :, 0:1536], W_r, self.wB_fm[l], KC, "bW")
            self.load_w(W[:, :, 1536:1608], W_r, self.wB_tm[l], KC, "bW")
            kbT = ph.sb([128, S], BF16, "bkb")
            kiT = ph.sb([128, S], BF16, "bki")
            k_rs = [Res() for _ in range(NCH)]
            V1 = ph.sb([128, NT, 128], BF16, "bV1")
            V_rs = [Res() for _ in range(NT)]
            ones_r = Res()
            kb.op("pool", lambda e: e.memset(V1[:, :, 64:128], 1.0), w=[ones_r])
            for r_ in V_rs:
                r_.w = ones_r.w
            hring = ph.ring(1, [128, KC, 512], BF16, "bh")
            tabr = ph.ring(1, [128, 4, 512], F32, "btab")
            qbr = ph.ring(3, [128, 2, 512], BF16, "bqb")
            qir = ph.ring(2, [128, 2, 512], BF16, "bqi")
            tmpr = ph.ring(2, [128, 512], F32, "btmp")
            wir = ph.ring(2, [128, 4, 8], F32, "bwi")
            dgr = ph.ring(1, [128, 8, 128], BF16, "bdg")
            Rr = ph.ring(8, [128, 512], BF16, "bR")
            scs = [ph.sb([128, S], F32, "bsc") for _ in range(2)]
            sc_rs = [Res(), Res()]
            mbs = [[ph.sb([128, S], BF16, "bmb") for _ in range(4)] for _ in range(2)]
            mb_rs = [[Res() for _ in range(4)] for _ in range(2)]
            bst = ph.ring(3, [128, 8 + 2 * (NBIS + 1) + 2], F32, "bst")
            pp = ph.ring(2, [128, 512], F32, "bpp", psum=True)
            psr = ph.ring(3, [128, 512], F32, "bps", psum=True)
            por = ph.ring(2, [128, 512], F32, "bpo", psum=True)
            pscr = ph.ring(1, [128, 512], F32, "bpsc", psum=True)
            ptr = ph.ring(2, [128, 512], BF16, "bpt")
            rdr = ph.ring(1, [128, 512], F32, "brd")
            otr = ph.ring(1, [128, 4, 512], BF16, "bot")
            W0 = 8
            M0 = W0 + NBIS + 1

            def proj(c):
                h, h_r = self.load_hT(self.hT, hring, c)
                tab, tab_r, t_ch = tabr.next()
                kb.dma("sp", tab[:], self.rope[:, :, c * 512:(c + 1) * 512].rearrange("t p s -> p t s"), t_ch, w=[tab_r])
                qb, qb_r, _ = qbr.next()
                qi, qi_r, _ = qir.next()

                def roped2(col, colp, pat, dst, dst_r):
                    ps, ps_r, _ = pp.next()
                    self.proj_fm(ps, ps_r, W, W_r, col, h, h_r)
                    p2, p2_r, _ = pp.next()
                    self.proj_fm(p2, p2_r, W, W_r, colp, h, h_r)
                    self.rope_evac(ps[:], ps_r, p2[:], p2_r, tab[:, 2 * pat, :], tab[:, 2 * pat + 1, :], tab_r, tmpr, dst, dst_r)

                for ci in range(2):
                    roped2(ci * 128, 256 + ci * 128, 0, qb[:, ci, :], qb_r)
                roped2(512, 640, 0, kbT[:, c * 512:(c + 1) * 512], k_rs[c])
                for ci in range(2):
                    roped2(768 + ci * 128, 1024 + ci * 128, 1, qi[:, ci, :], qi_r)
                roped2(1280, 1408, 1, kiT[:, c * 512:(c + 1) * 512], k_rs[c])
                wi, wi_r, _ = wir.next()
                for i in range(4):
                    t = 4 * c + i
                    ps, ps_r, _ = pp.next()
                    self.proj_tm(ps, ps_r, W, W_r, 1536, 72, h, h_r, i)
                    kb.op("act", lambda e: e.activation(out=V1[:, t, 0:64], in_=ps[:, 0:64], func=AF.Copy), r=[ps_r], w=[V_rs[t]])
                    kb.op("act", lambda e: e.activation(out=wi[:, i, :], in_=ps[:, 64:72], func=AF.Copy,
                                                        scale=(8.0 ** -0.5) * (32.0 ** -0.5)), r=[ps_r], w=[wi_r])
                return dict(qb=qb, qb_r=qb_r, qi=qi, qi_r=qi_r, wi=wi, wi_r=wi_r)

            def trivial(c, i):
                qt = 4 * c + i
                N = (qt + 1) * 128
                mb, mb_r = mbs[c % 2][i], mb_rs[c % 2][i]
                if qt == 1:
                    kb.op("pool", lambda e: e.memset(mb[:, 0:128], 0.0), w=[mb_r])
                kb.op("pool", lambda e: e.tensor_copy(out=mb[:, N - 128:N], in_=self.cnegb[:]), r=[self.cst_r], w=[mb_r])

            def scores_gen(qt):
                c, i = qt // 4, qt % 4
                P = Ps[c]
                sc, sc_r = scs[qt % 2], sc_rs[qt % 2]
                N = (qt + 1) * 128
                qi, qi_r, wi, wi_r = P["qi"], P["qi_r"], P["wi"], P["wi_r"]
                dg, dg_r, _ = dgr.next()
                for hh in range(8):
                    kb.op("dve", lambda e: e.tensor_scalar(out=dg[:, hh, :], in0=self.identf, scalar1=wi[:, i, hh:hh + 1],
                                                           scalar2=None, op0=ALU.mult), r=[wi_r, self.cm_r], w=[dg_r])
                nkc = (N + 511) // 512
                for kc_ in range(nkc):
                    k0 = kc_ * 512
                    kn = min(512, N - k0)
                    Rs = []
                    for hh in range(8):
                        pbh = (hh % 4) * 32
                        ps, ps_r, _ = pp.next()
                        mm(kb, ps[:, 0:kn], qi[pbh:pbh + 32, hh // 4, i * 128:(i + 1) * 128], kiT[pbh:pbh + 32, k0:k0 + kn],
                           True, True, [qi_r, k_rs[kc_]], [ps_r], pbase_tp(pbh))
                        R, R_r, _ = Rr.next()
                        if hh % 4 != 3:
                            kb.op("act", lambda e: e.activation(out=R[:, 0:kn], in_=ps[:, 0:kn], func=AF.Relu), r=[ps_r], w=[R_r])
                        else:
                            kb.op("dve", lambda e: e.tensor_scalar(out=R[:, 0:kn], in0=ps[:, 0:kn], scalar1=0.0, scalar2=None,
                                                                   op0=ALU.max), r=[ps_r], w=[R_r])
                        Rs.append((R, R_r))
                    psc, psc_r, _ = pscr.next()
                    for hh in range(8):
                        mm(kb, psc[:, 0:kn], dg[:, hh, :], Rs[hh][0][:, 0:kn], hh == 0, hh == 7, [dg_r, Rs[hh][1]], [psc_r])
                    if kc_ == nkc - 1:
                        dcol = kn - 128
                        if dcol > 0:
                            kb.op("act", lambda e: e.activation(out=sc[:, k0:k0 + dcol], in_=psc[:, 0:dcol], func=AF.Copy),
                                  r=[psc_r], w=[sc_r])
                        kb.op("dve", lambda e: e.tensor_tensor(out=sc[:, k0 + dcol:k0 + kn], in0=psc[:, dcol:kn],
                                                               in1=self.cnegf, op=ALU.add), r=[psc_r, self.cm_r], w=[sc_r])
                    else:
                        kb.op("act", lambda e: e.activation(out=sc[:, k0:k0 + kn], in_=psc[:, 0:kn], func=AF.Copy),
                              r=[psc_r], w=[sc_r])
                    yield

            def bisect_gen(qt):
                c, i = qt // 4, qt % 4
                sc, sc_r = scs[qt % 2], sc_rs[qt % 2]
                N = (qt + 1) * 128
                Nh = ((N // 2) // 128) * 128
                st, st_r, _ = bst.next()
                kb.op("dve", lambda e: e.tensor_reduce(out=st[:, 0:1], in_=sc[:, 0:N], axis=mybir.AxisListType.X, op=ALU.max),
                      r=[sc_r], w=[st_r])
                kb.op("dve", lambda e: e.tensor_reduce(out=st[:, 1:2], in_=sc[:, 0:N - 128], axis=mybir.AxisListType.X, op=ALU.min),
                      r=[sc_r], w=[st_r])
                kb.op("dve", lambda e: e.tensor_tensor(out=st[:, 2:3], in0=st[:, 0:1], in1=st[:, 1:2], op=ALU.subtract), r=[st_r], w=[st_r])
                kb.op("dve", lambda e: e.tensor_tensor(out=st[:, W0:W0 + NBIS + 1], in0=st[:, 2:3].to_broadcast([128, NBIS + 1]),
                                                       in1=wconst[:, 0:NBIS + 1], op=ALU.mult), r=[st_r, wc_r], w=[st_r])
                kb.op("dve", lambda e: e.tensor_tensor(out=st[:, M0:M0 + 1], in0=st[:, 1:2], in1=st[:, W0:W0 + 1], op=ALU.add), r=[st_r], w=[st_r])
                yield
                cmpv = float(TOPK) - 0.5 * (N - Nh)
                for k in range(NBIS):
                    mid = st[:, M0 + k:M0 + k + 1]
                    kb.op("dve", lambda e: e.tensor_scalar(out=jD[:, 0:Nh], in0=sc[:, 0:Nh], scalar1=mid, scalar2=0.0,
                                                           op0=ALU.is_ge, op1=ALU.add, accum_out=st[:, 3:4]),
                          r=[sc_r, st_r], w=[jD_r, st_r])
                    kb.op("act", lambda e: e.activation(out=jA[:, 0:N - Nh], in_=sc[:, Nh:N], func=AF.Sign, scale=-1.0, bias=mid,
                                                        accum_out=st[:, 6:7]), r=[sc_r, st_r], w=[jA_r, st_r])
                    kb.op("dve", lambda e: e.scalar_tensor_tensor(out=st[:, 7:8], in0=st[:, 6:7], scalar=-0.5, in1=st[:, 3:4],
                                                                  op0=ALU.mult, op1=ALU.add), r=[st_r], w=[st_r])
                    kb.op("dve", lambda e: e.tensor_scalar(out=st[:, 4:5], in0=st[:, 7:8], scalar1=cmpv, scalar2=st[:, W0 + k:W0 + k + 1],
                                                           op0=ALU.is_ge, op1=ALU.mult), r=[st_r], w=[st_r])
                    kb.op("dve", lambda e: e.scalar_tensor_tensor(out=st[:, M0 + k + 1:M0 + k + 2], in0=st[:, 4:5],
                                                                  scalar=st[:, W0 + k + 1:W0 + k + 2], in1=mid,
                                                                  op0=ALU.subtract, op1=ALU.add), r=[st_r], w=[st_r])
                    yield
                mb, mb_r = mbs[c % 2][i], mb_rs[c % 2][i]
                kb.op("dve", lambda e: e.tensor_tensor(out=st[:, 5:6], in0=st[:, M0 + NBIS:M0 + NBIS + 1],
                                                       in1=st[:, W0 + NBIS:W0 + NBIS + 1], op=ALU.subtract), r=[st_r], w=[st_r])
                kb.op("dve", lambda e: e.tensor_scalar(out=mb[:, 0:N], in0=sc[:, 0:N], scalar1=st[:, 5:6], scalar2=NEG_B,
                                                       op0=ALU.is_lt, op1=ALU.mult), r=[sc_r, st_r], w=[mb_r])
                yield

            otiles = {}

            def attn_head_gen(c, hd):
                P = Ps[c]
                qb, qb_r = P["qb"], P["qb_r"]
                if hd == 0:
                    otiles[c] = otr.next()
                oTs, oTs_r, o_ch = otiles[c]
                cb, pb = hd // 2, (hd % 2) * 64
                po, po_r, _ = por.next()

                def mask_fn(j, col0):
                    ex = []
                    for i2 in range(col0 // 128, 4):
                        ex.append((mbs[c % 2][i2][:, j * 128:(j + 1) * 128], self.identb[:], i2 * 128, (i2 + 1) * 128,
                                   [mb_rs[c % 2][i2], self.cst_r]))
                    return ex

                yield from self.causal_attn_gen(
                    c,
                    lambda j: kbT[pb:pb + 64, j * 128:(j + 1) * 128], lambda j: k_rs[j // 4],
                    lambda col0: qb[pb:pb + 64, cb, col0:512], qb_r,
                    lambda j: V1[:, j, :], lambda j: V_rs[j],
                    psr, ptr, po, po_r, 0.125, mask_fn=mask_fn)
                self.attn_finalize(po, po_r, rdr, oTs[0:64, hd, :], oTs_r)
                if hd == 3:
                    self.store_oT(1, c, oTs, oTs_r, o_ch)
                yield

            def proj_gen(c):
                Ps[c] = proj(c)
                yield

            def chain(gens):
                for g in gens:
                    yield from g

            def zip_run(ga, na, gb, nb):
                done_a = done_b = False
                ia = ib = 0
                while not (done_a and done_b):
                    if not done_a and (done_b or ia * max(nb, 1) <= ib * max(na, 1)):
                        try:
                            next(ga)
                            ia += 1
                        except StopIteration:
                            done_a = True
                    else:
                        try:
                            next(gb)
                            ib += 1
                        except StopIteration:
                            done_b = True

            wconst = ph.sb([128, NBIS + 1], F32, "bwc")
            wc_r = Res()
            for k in range(NBIS + 1):
                kb.op("pool", lambda e: e.memset(wconst[:, k:k + 1], (1.0 + 2.0 ** -10) * 2.0 ** -(k + 1)), w=[wc_r])
            jDt = ph.sb([128, S // 2], BF16, "bjd")
            jAt = ph.sb([128, S // 2 + 128], BF16, "bja")
            jD, jA, jD_r, jA_r = jDt[:], jAt[:], Res(), Res()

            Ps = {}
            Ps[0] = proj(0)
            trivial(0, 0)
            trivial(0, 1)
            for _ in scores_gen(2):
                pass
            for qt in range(2, NT):
                c, i = qt // 4, qt % 4
                bg = []
                nbg = 0
                if qt + 1 < NT:
                    bg.append(scores_gen(qt + 1))
                    nbg += (qt + 2 + 3) // 4
                if i == 2 and c + 1 < NCH:
                    bg.append(proj_gen(c + 1))
                    nbg += 1
                if c >= 1:
                    bg.append(attn_head_gen(c - 1, i))
                    nbg += 4 * (c - 1) + 5
                zip_run(bisect_gen(qt), NBIS + 2, chain(bg), nbg)
            for hd in range(4):
                for _ in attn_head_gen(NCH - 1, hd):
                    pass

    def phase_diff(self, l):
        kb, S, NT, NCH = self.kb, self.S, self.NT, self.NCH
        lam_init = 0.8 - 0.6 * math.exp(-0.3 * l)
        with kb.phase() as ph:
            W = ph.sb([128, KC, 1280], BF16, "cW")
            W_r = Res()
            self.load_w(W[:, :, 0:1024], W_r, self.wC_fm[l], KC, "cW")
            self.load_w(W[:, :, 1024:1280], W_r, self.wC_tm[l], KC, "cW")
            kT = ph.sb([128, 2, S], BF16, "ckT")
            kT_rs = [Res() for _ in range(NCH)]
            V1 = ph.sb([128, NT, 4, 128], BF16, "cV1")
            V_rs = [Res() for _ in range(NT)]
            ones_r = Res()
            kb.op("pool", lambda e: e.memset(V1[:, :, :, 64:128], 1.0), w=[ones_r])
            for r_ in V_rs:
                r_.w = ones_r.w
            lm = ph.sb([128, 128], F32, "clm")
            ls = ph.sb([128, 8], F32, "cls")
            lm_r = Res()
            self.load_bc(lm[:], lm_r, self.dlam[l], 128, "clm")
            kb.op("dve", lambda e: e.tensor_tensor(out=lm[:, 0:32], in0=lm[:, 0:32], in1=lm[:, 32:64], op=ALU.mult), r=[lm_r], w=[lm_r])
            kb.op("dve", lambda e: e.tensor_tensor(out=lm[:, 64:96], in0=lm[:, 64:96], in1=lm[:, 96:128], op=ALU.mult), r=[lm_r], w=[lm_r])
            kb.op("dve", lambda e: e.tensor_reduce(out=ls[:, 0:1], in_=lm[:, 0:32], axis=mybir.AxisListType.X, op=ALU.add), r=[lm_r], w=[lm_r])
            kb.op("dve", lambda e: e.tensor_reduce(out=ls[:, 1:2], in_=lm[:, 64:96], axis=mybir.AxisListType.X, op=ALU.add), r=[lm_r], w=[lm_r])
            kb.op("act", lambda e: e.activation(out=ls[:, 2:4], in_=ls[:, 0:2], func=AF.Exp), r=[lm_r], w=[lm_r])
            kb.op("dve", lambda e: e.tensor_tensor(out=ls[:, 4:5], in0=ls[:, 2:3], in1=ls[:, 3:4], op=ALU.subtract), r=[lm_r], w=[lm_r])
            kb.op("dve", lambda e: e.tensor_scalar(out=ls[:, 5:6], in0=ls[:, 4:5], scalar1=lam_init, scalar2=-1.0, op0=ALU.add, op1=ALU.mult),
                  r=[lm_r], w=[lm_r])
            sg = ph.sb([128, 2], F32, "csg")
            sg_r = Res()
            kb.dma("sp", sg[:, 0:1], self.subg[l], "csg", w=[sg_r])
            kb.op("dve", lambda e: e.tensor_scalar(out=sg[:, 1:2], in0=sg[:, 0:1], scalar1=(1.0 - lam_init), scalar2=None, op0=ALU.mult),
                  r=[sg_r], w=[sg_r])
            hring = ph.ring(2, [128, KC, 512], BF16, "ch")
            tabr = ph.ring(1, [128, 2, 512], F32, "ctab")
            qring = ph.ring(2, [128, 2, 512], BF16, "cq")
            tmpr = ph.ring(4, [128, 512], F32, "ctmp")
            pp = ph.ring(2, [128, 512], F32, "cpp", psum=True)
            psr = ph.ring(3, [128, 512], F32, "cps", psum=True)
            por = ph.ring(2, [128, 512], F32, "cpo", psum=True)
            pssr = ph.ring(1, [128, 512], F32, "cpss", psum=True)
            ptr = ph.ring(3, [128, 512], BF16, "cpt")
            rdr = ph.ring(2, [128, 512], F32, "crd")
            otr = ph.ring(2, [128, 4, 512], BF16, "cot")
            o0r = ph.ring(2, [128, 512], F32, "co0")
            o1r = ph.ring(2, [128, 512], F32, "co1")
            sqr = ph.ring(2, [128, 512], F32, "csq")
            sqbr = ph.ring(2, [128, 512], BF16, "csqb")
            for c in range(NCH):
                h, h_r = self.load_hT(self.hT, hring, c)
                tab, tab_r, t_ch = tabr.next()
                kb.dma("sp", tab[:], self.rope[2:4, :, c * 512:(c + 1) * 512].rearrange("t p s -> p t s"), t_ch, w=[tab_r])
                qT, qT_r, _ = qring.next()

                def roped2(col, colp, dst, dst_r):
                    ps, ps_r, _ = pp.next()
                    self.proj_fm(ps, ps_r, W, W_r, col, h, h_r)
                    p2, p2_r, _ = pp.next()
                    self.proj_fm(p2, p2_r, W, W_r, colp, h, h_r)
                    self.rope_evac(ps[:], ps_r, p2[:], p2_r, tab[:, 0, :], tab[:, 1, :], tab_r, tmpr, dst, dst_r)

                for ci in range(2):
                    roped2(ci * 128, 256 + ci * 128, qT[:, ci, :], qT_r)
                for ci in range(2):
                    roped2(512 + ci * 128, 768 + ci * 128, kT[:, ci, c * 512:(c + 1) * 512], kT_rs[c])
                for i in range(4):
                    t = 4 * c + i
                    ps, ps_r, _ = pp.next()
                    self.proj_tm(ps, ps_r, W, W_r, 1024, 256, h, h_r, i)
                    kb.op("act", lambda e: e.activation(out=V1[:, t, :, 0:64], in_=ps[:, 0:256].rearrange("p (h d) -> p h d", h=4),
                                                        func=AF.Copy), r=[ps_r], w=[V_rs[t]])
                oTs, oTs_r, o_ch = otr.next()
                for hd in range(4):
                    cb = hd // 2
                    outs = []
                    for m in range(2):
                        pb = ((hd % 2) * 2 + m) * 32
                        po, po_r, _ = por.next()
                        self.causal_attn(
                            c,
                            lambda j: kT[pb:pb + 32, cb, j * 128:(j + 1) * 128], lambda j: kT_rs[j // 4],
                            lambda col0: qT[pb:pb + 32, cb, col0:512], qT_r,
                            lambda j: V1[:, j, hd, :], lambda j: V_rs[j],
                            psr, ptr, po, po_r, 32.0 ** -0.5, tp=pbase_tp(pb))
                        o, o_r, _ = (o0r if m == 0 else o1r).next()
                        self.attn_finalize(po, po_r, rdr, o[0:64, :], o_r)
                        outs.append((o, o_r))
                    (o0, o0_r), (o1, o1_r) = outs
                    kb.op("dve", lambda e: e.scalar_tensor_tensor(out=o0[0:64, :], in0=o1[0:64, :], scalar=ls[0:64, 5:6], in1=o0[0:64, :],
                                                                  op0=ALU.mult, op1=ALU.add), r=[o0_r, o1_r, lm_r], w=[o0_r])
                    sq, sq_r, _ = sqr.next()
                    sqb, sqb_r, _ = sqbr.next()
                    kb.op("pool", lambda e: e.tensor_tensor(out=sqb[0:64, :], in0=o0[0:64, :], in1=o0[0:64, :], op=ALU.mult), r=[o0_r], w=[sqb_r])
                    pss, pss_r, _ = pssr.next()
                    mm(kb, pss[0:64, :], self.onesb[0:64, 0:64], sqb[0:64, :], True, True, [sqb_r, self.cst_r], [pss_r])
                    kb.op("dve", lambda e: e.tensor_scalar(out=sq[0:64, :], in0=pss[0:64, :], scalar1=1.0 / 64, scalar2=EPS,
                                                           op0=ALU.mult, op1=ALU.add), r=[pss_r], w=[sq_r])
                    kb.op("act", lambda e: e.activation(out=sq[0:64, :], in_=sq[0:64, :], func=AF.Ln), r=[sq_r], w=[sq_r])
                    kb.op("act", lambda e: e.activation(out=sq[0:64, :], in_=sq[0:64, :], func=AF.Exp, scale=-0.5), r=[sq_r], w=[sq_r])
                    kb.op("dve", lambda e: e.scalar_tensor_tensor(out=oTs[0:64, hd, :], in0=o0[0:64, :], scalar=sg[0:64, 1:2], in1=sq[0:64, :],
                                                                  op0=ALU.mult, op1=ALU.mult), r=[o0_r, sq_r, sg_r], w=[oTs_r])
                self.store_oT(2, c, oTs, oTs_r, o_ch)

    def phase_mem(self, l):
        kb, S, NT, NCH = self.kb, self.S, self.NT, self.NCH
        with kb.phase() as ph:
            W = ph.sb([128, KC, 768], BF16, "mW")
            W_r = Res()
            self.load_w(W[:, :, 0:256], W_r, self.wM_fm[l], KC, "mW")
            self.load_w(W[:, :, 256:768], W_r, self.w_kv[l], KC, "mW")
            nm = Prog.Normer(self, ph, self.g_mem[l], "mg")
            xr = ph.ring(2, [128, D], F32, "mx")
            hm = ph.sb([128, KC, MEM], BF16, "mhm")
            hm_r = Res()
            for t in range(2):
                xt, xt_r, x_ch = xr.next()
                kb.dma("sp", xt[:], self.mem_in[t * 128:(t + 1) * 128, :], x_ch, w=[xt_r])
                nm.to_hT(xt[:], xt_r, hm[:, :, t * 128:(t + 1) * 128], hm_r)
            KmT = ph.sb([128, 2, MEM], BF16, "mK")
            Vm = ph.sb([128, 2, 4, 128], BF16, "mV")
            kv_r = Res()
            kb.op("pool", lambda e: e.memset(Vm[:, :, :, 64:128], 1.0), w=[kv_r])
            pp = ph.ring(2, [128, 512], F32, "mpp", psum=True)
            psr = ph.ring(2, [128, 512], F32, "mps", psum=True)
            por = ph.ring(2, [128, 512], F32, "mpo", psum=True)
            for ci in range(2):
                ps, ps_r, _ = pp.next()
                self.proj_fm(ps, ps_r, W, W_r, 256 + ci * 128, hm, hm_r, n=MEM)
                kb.op("act", lambda e: e.activation(out=KmT[:, ci, :], in_=ps[:, 0:MEM], func=AF.Copy), r=[ps_r], w=[kv_r])
            for t in range(2):
                ps, ps_r, _ = pp.next()
                self.proj_tm(ps, ps_r, W, W_r, 512, 256, hm, hm_r, t)
                kb.op("act", lambda e: e.activation(out=Vm[:, t, :, 0:64], in_=ps[:, 0:256].rearrange("p (h d) -> p h d", h=4),
                                                    func=AF.Copy), r=[ps_r], w=[kv_r])
            hring = ph.ring(2, [128, KC, 512], BF16, "mh")
            qring = ph.ring(2, [128, 2, 512], BF16, "mq")
            ptr = ph.ring(3, [128, 512], BF16, "mpt")
            rdr = ph.ring(2, [128, 512], F32, "mrd")
            otr = ph.ring(2, [128, 4, 512], BF16, "mot")
            for c in range(NCH):
                h, h_r = self.load_hT(self.hT, hring, c)
                qT, qT_r, _ = qring.next()
                for ci in range(2):
                    ps, ps_r, _ = pp.next()
                    self.proj_fm(ps, ps_r, W, W_r, ci * 128, h, h_r)
                    kb.op("act", lambda e: e.activation(out=qT[:, ci, :], in_=ps[:], func=AF.Copy), r=[ps_r], w=[qT_r])
                oTs, oTs_r, o_ch = otr.next()
                for hd in range(4):
                    cb, pb = hd // 2, (hd % 2) * 64
                    po, po_r, _ = por.next()
                    for t in range(2):
                        ps, ps_r, _ = psr.next()
                        mm(kb, ps[:], KmT[pb:pb + 64, cb, t * 128:(t + 1) * 128], qT[pb:pb + 64, cb, :], True, True, [kv_r, qT_r], [ps_r])
                        pt, pt_r, _ = ptr.next()
                        kb.op("act", lambda e: e.activation(out=pt[:], in_=ps[:], func=AF.Exp, scale=0.125), r=[ps_r], w=[pt_r])
                        mm(kb, po[:], Vm[:, t, hd, :], pt[:], t == 0, t == 1, [kv_r, pt_r], [po_r])
                    self.attn_finalize(po, po_r, rdr, oTs[0:64, hd, :], oTs_r)
                self.store_oT(3, c, oTs, oTs_r, o_ch)

    def phase_merge(self, l):
        kb, S, NT, NCH = self.kb, self.S, self.NT, self.NCH
        x_src = self.x_in if l == 0 else self.xres
        with kb.phase() as ph:
            nb = len(self.branches)
            Wg = ph.sb([128, KC, 4096], BF16, "gWg")
            Wb = ph.sb([128, 4, 2, D], BF16, "gWb")
            Wo = ph.sb([128, KC, D], BF16, "gWo")
            bg = ph.sb([128, 32], F32, "gbg")
            Wg_r, Wb_r, Wo_r, bg_r = Res(), Res(), Res(), Res()
            kb.dma("sp", bg[:], self.b_gate[l], "gbg", w=[bg_r])
            for n in self.branches:
                self.load_w(Wb[:, n, :, :], Wb_r, self.w_branch[l, n], 2, "gWb")
            for k in range(KC):
                for n in self.branches:
                    kb.dma("pool", Wg[:, k, n * 1024:(n + 1) * 1024], self.w_gate[l, k * 128:(k + 1) * 128, n * 1024:(n + 1) * 1024],
                           "gWg", w=[Wg_r])
            self.load_w(Wo, Wo_r, self.w_out[l], KC, "gWo")
            nm = Prog.Normer(self, ph, self.g_ffn[l], "gng")
            hring = ph.ring(1, [128, KC, 512], BF16, "gh")
            oring = ph.ring(1, [128, 4, 2, 512], BF16, "go")
            xr = ph.ring(3, [128, D], F32, "gx")
            gsr = ph.ring(2, [128, 512], F32, "ggs")
            tmr = ph.ring(2, [128, 512], F32, "gtm")
            acr = ph.ring(2, [128, 512], F32, "gac")
            mT = ph.sb([128, KC, 512], BF16, "gmT")
            mT_r = Res()
            h2r = ph.ring(1, [128, KC, 512], BF16, "gh2")
            pyr = ph.ring(2, [128, 512], F32, "gpy", psum=True)
            pgr = ph.ring(2, [128, 512], F32, "gpg", psum=True)
            pxr = ph.ring(2, [128, 512], F32, "gpx", psum=True)
            for c in range(NCH):
                h, h_r = self.load_hT(self.hT, hring, c)
                o, o_r, o_ch = oring.next()
                for n in self.branches:
                    kb.dma("sp", o[:, n, :, :], self.oT[n, :, c * 512:(c + 1) * 512].rearrange("(k p) s -> p k s", p=128), o_ch, w=[o_r])
                for f in range(KC):
                    acc, acc_r, _ = acr.next()
                    for bi_, n in enumerate(self.branches):
                        py, py_r, _ = pyr.next()
                        for k in range(2):
                            mm(kb, py[:], Wb[:, n, k, f * 128:(f + 1) * 128], o[:, n, k, :], k == 0, k == 1, [Wb_r, o_r], [py_r])
                        pg, pg_r, _ = pgr.next()
                        for k in range(KC):
                            mm(kb, pg[:], Wg[:, k, n * 1024 + f * 128:n * 1024 + (f + 1) * 128], h[:, k, :], k == 0, k == KC - 1,
                               [Wg_r, h_r], [pg_r])
                        gs, gs_r, _ = gsr.next()
                        kb.op("act", lambda e: e.activation(out=gs[:], in_=pg[:], func=AF.Sigmoid, bias=bg[:, n * 8 + f:n * 8 + f + 1]),
                              r=[pg_r, bg_r], w=[gs_r])
                        lastb = (bi_ == nb - 1)
                        if bi_ == 0:
                            dst, dst_r = (mT[:, f, :], mT_r) if lastb else (acc[:], acc_r)
                            kb.op("dve", lambda e: e.tensor_tensor(out=dst, in0=py[:], in1=gs[:], op=ALU.mult), r=[py_r, gs_r], w=[dst_r])
                        else:
                            tm, tm_r, _ = tmr.next()
                            kb.op("dve", lambda e: e.tensor_tensor(out=tm[:], in0=py[:], in1=gs[:], op=ALU.mult), r=[py_r, gs_r], w=[tm_r])
                            dst, dst_r = (mT[:, f, :], mT_r) if lastb else (acc[:], acc_r)
                            kb.op("pool", lambda e: e.tensor_tensor(out=dst, in0=acc[:], in1=tm[:], op=ALU.add), r=[acc_r, tm_r], w=[dst_r] if lastb else [acc_r])
                h2, h2_r, h2_ch = h2r.next()
                for i in range(4):
                    t = 4 * c + i
                    xt, xt_r, x_ch = xr.next()
                    kb.dma("sp", xt[:], x_src[t * 128:(t + 1) * 128, :], x_ch, w=[xt_r])
                    for hh in range(2):
                        px, px_r, _ = pxr.next()
                        for k in range(KC):
                            mm(kb, px[:], mT[:, k, i * 128:(i + 1) * 128], Wo[:, k, hh * 512:(hh + 1) * 512], k == 0, k == KC - 1,
                               [mT_r, Wo_r], [px_r])
                        kb.op("dve", lambda e: e.tensor_tensor(out=xt[:, hh * 512:(hh + 1) * 512], in0=px[:], in1=xt[:, hh * 512:(hh + 1) * 512],
                                                               op=ALU.add), r=[px_r, xt_r], w=[xt_r])
                    kb.dma("sp", self.xres[t * 128:(t + 1) * 128, :], xt[:], x_ch, r=[xt_r])
                    nm.to_hT(xt[:], xt_r, h2[:, :, i * 128:(i + 1) * 128], h2_r)
                kb.dma("sp", self.h2T[:, :, c * 512:(c + 1) * 512].rearrange("k p s -> p k s"), h2[:], h2_ch, r=[h2_r])

    def phase_ffn(self, l, j0, j1, last):
        kb, S, NT, NCH = self.kb, self.S, self.NT, self.NCH
        nj = j1 - j0
        final = last and (l == self.depth - 1)
        with kb.phase() as ph:
            W1 = ph.sb([128, KC, 2, nj * 128], BF16, "fW1")
            W2 = ph.sb([128, nj, D], BF16, "fW2")
            W1_r, W2_r = Res(), Res()
            for k in range(KC):
                for u in range(2):
                    kb.dma("pool", W1[:, k, u, :], self.w_f1[l, k * 128:(k + 1) * 128, u * FFH + j0 * 128:u * FFH + j1 * 128], "fW1", w=[W1_r])
            self.load_w(W2, W2_r, self.w_f2[l, j0 * 128:j1 * 128, :], nj, "fW2")
            if last:
                g_row = self.g_fin if final else self.g_mix[l + 1]
                nm = Prog.Normer(self, ph, g_row, "fng")
            hring = ph.ring(2, [128, KC, 512], BF16, "fh")
            xr = ph.ring(3, [128, D], F32, "fx")
            sir = ph.ring(2, [128, 512], F32, "fsi")
            aT = ph.sb([128, nj, 512], BF16, "faT")
            aT_r = Res()
            hor = ph.ring(1, [128, KC, 512], BF16, "fho")
            outr = ph.ring(2, [128, D], F32, "fout")
            p1r = ph.ring(2, [128, 512], F32, "fp1", psum=True)
            p2r = ph.ring(2, [128, 512], F32, "fp2", psum=True)
            pxr = ph.ring(2, [128, 512], F32, "fpx", psum=True)
            for c in range(NCH):
                h, h_r = self.load_hT(self.h2T, hring, c)
                for j in range(nj):
                    p1, p1_r, _ = p1r.next()
                    for k in range(KC):
                        mm(kb, p1[:], W1[:, k, 0, j * 128:(j + 1) * 128], h[:, k, :], k == 0, k == KC - 1, [W1_r, h_r], [p1_r])
                    p2, p2_r, _ = p2r.next()
                    for k in range(KC):
                        mm(kb, p2[:], W1[:, k, 1, j * 128:(j + 1) * 128], h[:, k, :], k == 0, k == KC - 1, [W1_r, h_r], [p2_r])
                    si, si_r, _ = sir.next()
                    kb.op("act", lambda e: e.activation(out=si[:], in_=p1[:], func=AF.Silu), r=[p1_r], w=[si_r])
                    kb.op("dve", lambda e: e.tensor_tensor(out=aT[:, j, :], in0=p2[:], in1=si[:], op=ALU.mult), r=[p2_r, si_r], w=[aT_r])
                if last:
                    ho, ho_r, ho_ch = hor.next()
                for i in range(4):
                    t = 4 * c + i
                    xt, xt_r, x_ch = xr.next()
                    kb.dma("sp", xt[:], self.xres[t * 128:(t + 1) * 128, :], x_ch, w=[xt_r])
                    for hh in range(2):
                        px, px_r, _ = pxr.next()
                        for j in range(nj):
                            mm(kb, px[:], aT[:, j, i * 128:(i + 1) * 128], W2[:, j, hh * 512:(hh + 1) * 512], j == 0, j == nj - 1,
                               [aT_r, W2_r], [px_r])
                        kb.op("dve", lambda e: e.tensor_tensor(out=xt[:, hh * 512:(hh + 1) * 512], in0=px[:], in1=xt[:, hh * 512:(hh + 1) * 512],
                                                               op=ALU.add), r=[px_r, xt_r], w=[xt_r])
                    if final:
                        ot, ot_r, ot_ch = outr.next()
                        nm.to_out(xt[:], xt_r, ot[:], ot_r)
                        kb.dma("sp", self.out[t * 128:(t + 1) * 128, :], ot[:], ot_ch, r=[ot_r])
                    else:
                        kb.dma("sp", self.xres[t * 128:(t + 1) * 128, :], xt[:], x_ch, r=[xt_r])
                        if last:
                            nm.to_hT(xt[:], xt_r, ho[:, :, i * 128:(i + 1) * 128], ho_r)
                if last and not final:
                    kb.dma("sp", self.hT[:, :, c * 512:(c + 1) * 512].rearrange("k p s -> p k s"), ho[:], ho_ch, r=[ho_r])


IN_OFF = {}
_off = 0
for _n, _w in (("qa", 256), ("ka", 256), ("va", 256), ("fa", 4), ("qb", 256), ("kb", 64), ("vb", 64), ("qi", 256), ("ki", 32),
               ("wi", 8), ("qc", 256), ("kc", 256), ("vc", 256), ("qm", 256)):
    IN_OFF[_n] = (_off, _off + _w)
    _off += _w


def _cols(name):
    a, b = IN_OFF[name]
    return np.arange(a, b)


def _partner(cols, hd, half):
    c = cols.copy().reshape(-1, hd)
    p = c.copy()
    p[:, 0:half] = c[:, half:2 * half]
    p[:, half:2 * half] = c[:, 0:half]
    return p.reshape(-1)


def _host_consts():
    ident = np.eye(128, dtype=np.float32)
    k = np.arange(128)[:, None]
    q = np.arange(128)[None, :]
    tri = (k <= q).astype(np.float32)
    cneg = np.where(q > k, np.float32(NEG_S), np.float32(0.0)).astype(np.float32)
    cmat = np.concatenate([ident, tri, cneg], axis=1).astype(np.float32)
    p = np.arange(128)
    ccol = np.zeros((128, 4), np.float32)
    for pat, (hd, rot) in enumerate(((64, 16), (32, 8))):
        half = rot // 2
        inv = (np.float32(ROPE_THETA) ** (-(np.arange(0, rot, 2, dtype=np.float32) / np.float32(rot)))).astype(np.float32)
        d = p % hd
        ccol[:, pat] = np.where(d < rot, inv[d % half], 0.0)
        ccol[:, 2 + pat] = np.where(d < half, -1.0, np.where(d < rot, 1.0, 0.0))
    return cmat, ccol


def _prep_shared(inp):
    w_in = np.asarray(inp["w_in"], np.float32)
    L = w_in.shape[0]
    g = lambda cols: np.ascontiguousarray(w_in[:, :, cols])
    qb, kb_, qi, ki, qc, kc = _cols("qb"), _cols("kb"), _cols("qi"), _cols("ki"), _cols("qc"), _cols("kc")
    kb2 = np.concatenate([kb_, kb_])
    ki4 = np.concatenate([ki, ki, ki, ki])
    cmat, ccol = _host_consts()
    sh = {
        "cmat": cmat, "ccol": ccol,
        "g_mix": np.asarray(inp["norm_mix_g"], np.float32).reshape(L, 1, D),
        "g_ffn": np.asarray(inp["norm_ffn_g"], np.float32).reshape(L, 1, D),
        "g_mem": np.asarray(inp["norm_mem_g"], np.float32).reshape(L, 1, D),
        "g_fin": np.asarray(inp["final_norm_g"], np.float32).reshape(1, D),
        "wA_fm": g(np.concatenate([_cols("qa"), _cols("ka")])),
        "wA_tm": g(np.concatenate([_cols("va"), _cols("fa")])),
        "wB_fm": g(np.concatenate([qb, _partner(qb, 64, 8), kb2, _partner(kb2, 64, 8), qi, _partner(qi, 32, 4), ki4, _partner(ki4, 32, 4)])),
        "wB_tm": g(np.concatenate([_cols("vb"), _cols("wi")])),
        "wC_fm": g(np.concatenate([qc, _partner(qc, 32, 4), kc, _partner(kc, 32, 4)])),
        "wC_tm": g(_cols("vc")),
        "wM_fm": g(_cols("qm")),
        "w_kv": np.ascontiguousarray(np.asarray(inp["w_mem_kv"], np.float32)),
        "b_forget": np.asarray(inp["b_forget"], np.float32).reshape(L, 1, 4),
        "dlam": np.asarray(inp["diff_lambda"], np.float32).reshape(L, 1, 128),
        "subg": np.ascontiguousarray(np.tile(np.asarray(inp["diff_subln_g"], np.float32), (1, 2)).reshape(L, 128, 1)),
        "w_branch": np.ascontiguousarray(np.asarray(inp["w_branch"], np.float32)),
        "w_gate": np.ascontiguousarray(np.asarray(inp["w_gate"], np.float32)),
        "b_gate": np.ascontiguousarray(np.asarray(inp["b_gate"], np.float32).reshape(L, 32, 128).transpose(0, 2, 1)),
        "w_out": np.ascontiguousarray(np.asarray(inp["w_out"], np.float32)),
        "w_f1": np.ascontiguousarray(np.asarray(inp["w_ffn_in"], np.float32)),
        "w_f2": np.ascontiguousarray(np.asarray(inp["w_ffn_out"], np.float32)),
    }
    return sh


_PROG_CACHE = {}


def run(inp, S, depth, n_cores, branches=(0, 1, 2, 3), trace=False):
    key = (S, depth, tuple(branches))
    if key not in _PROG_CACHE:
        _PROG_CACHE[key] = Prog(S, depth, branches)
    prog = _PROG_CACHE[key]
    sh = _prep_shared(inp)
    x = np.asarray(inp["x"], np.float32)
    mem = np.asarray(inp["mem"], np.float32)
    pos = np.asarray(inp["positions"], np.int32)
    in_maps = []
    for b in range(n_cores):
        m = dict(sh)
        m["x"] = np.ascontiguousarray(x[b])
        m["mem"] = np.ascontiguousarray(mem[b])
        m["pos"] = np.ascontiguousarray(pos[b].reshape(1, S))
        in_maps.append(m)
    res = run_bass_kernel_spmd(prog.nc, in_maps, core_ids=list(range(n_cores)), trace=trace)
    out = np.stack([np.asarray(r["out"], np.float32) for r in res.results], axis=0)
    return out, res


def kernel(**inputs):
    out, _ = run(inputs, 4096, 4, 8)
    return out
```

```python
import math
from contextlib import ExitStack, contextmanager
import numpy as np
import concourse.bass as bass
import concourse.mybir as mybir
from concourse.bass_utils import run_bass_kernel_spmd

F32 = mybir.dt.float32
BF16 = mybir.dt.bfloat16
I32 = mybir.dt.int32
ALU = mybir.AluOpType
AF = mybir.ActivationFunctionType

D = 1024
KC = 8
MEM = 256
FFH = 2816
EPS = 1e-6
NEG_S = -1.0e4
NEG_B = -30000.0
TOPK = 256
NBIS = 14
import os
STOP = int(os.environ.get('KSTOP', '0'))
ROPE_THETA = 500000.0
TWO_PI = 2.0 * math.pi
CW_HI = 6.28125
CW_LO = TWO_PI - CW_HI


class Res:
    __slots__ = ("w", "rs")

    def __init__(self):
        self.w = None
        self.rs = {}


class KB:
    def __init__(self):
        self.nc = bass.Bass("TRN2", target_bir_lowering=False)
        nc = self.nc
        self.ges = ExitStack()
        self.E = {"pe": nc.tensor, "act": nc.scalar, "dve": nc.vector, "pool": nc.gpsimd, "sp": nc.sync}
        self.sems = {}
        self.cnt = {}
        self.cur = {}
        self.known = {n: {} for n in self.E}
        self.epoch = -1
        self.nid = 0
        self.new_epoch()

    def new_epoch(self):
        self.epoch += 1
        for n in ("pe", "act", "dve", "pool"):
            nm = "%s#%d" % (n, self.epoch)
            self.sems[nm] = self.ges.enter_context(self.nc.semaphore("e_%s_%d" % (n, self.epoch)))
            self.cnt[nm] = 0
            self.cur[n] = nm

    def chan(self, name):
        nm = "c_" + name
        if nm not in self.sems:
            self.sems[nm] = self.ges.enter_context(self.nc.semaphore(nm))
            self.cnt[nm] = 0
        return nm

    def name(self, p):
        self.nid += 1
        return "%s_%d" % (p, self.nid)

    def _waits(self, eng, r, w):
        deps = {}
        for x in r:
            if x.w is not None and deps.get(x.w[0], 0) < x.w[1]:
                deps[x.w[0]] = x.w[1]
        for x in w:
            if x.w is not None and deps.get(x.w[0], 0) < x.w[1]:
                deps[x.w[0]] = x.w[1]
            for s, v in x.rs.items():
                if deps.get(s, 0) < v:
                    deps[s] = v
        kn = self.known[eng]
        for s, v in deps.items():
            if eng == "pe" and s.startswith("pe#"):
                continue
            if kn.get(s, 0) >= v:
                continue
            self.E[eng].wait_ge(self.sems[s], v)
            kn[s] = v

    def _post(self, tok, r, w):
        s, v = tok
        for x in r:
            if x.rs.get(s, 0) < v:
                x.rs[s] = v
        for x in w:
            x.w = tok
            x.rs = {}

    def op(self, eng, fn, r=(), w=()):
        self._waits(eng, r, w)
        ins = fn(self.E[eng])
        s = self.cur[eng]
        self.cnt[s] += 1
        ins.then_inc(self.sems[s], 1)
        self._post((s, self.cnt[s]), r, w)

    def dma(self, q, out, in_, chan, r=(), w=()):
        self._waits(q, r, w)
        ins = self.E[q].dma_start(out=out, in_=in_)
        c = self.chan(chan)
        self.cnt[c] += 16
        ins.then_inc(self.sems[c], 16)
        self._post((c, self.cnt[c]), r, w)

    def barrier(self):
        for e in self.E:
            kn = self.known[e]
            for s, v in self.cnt.items():
                if v == 0 or kn.get(s, 0) >= v:
                    continue
                if e == "pe" and s.startswith("pe#"):
                    continue
                self.E[e].wait_ge(self.sems[s], v)
                kn[s] = v

    @contextmanager
    def phase(self):
        es = ExitStack()
        with es:
            yield Phase(self, es)
            self.barrier()


class Phase:
    def __init__(self, kb, es):
        self.kb = kb
        self.es = es

    def sb(self, shape, dt, nm="t"):
        return self.es.enter_context(self.kb.nc.sbuf_tensor(self.kb.name(nm), list(shape), dt))

    def ps(self, shape, dt, nm="p"):
        return self.es.enter_context(self.kb.nc.psum_tensor(self.kb.name(nm), list(shape), dt))

    def ring(self, n, shape, dt, nm="r", psum=False):
        items = []
        for i in range(n):
            t = self.ps(shape, dt, nm) if psum else self.sb(shape, dt, nm)
            items.append((t, Res(), "%s%d" % (nm, i)))
        return Ring(items)


class Ring:
    def __init__(self, items):
        self.items = items
        self.i = -1

    def next(self):
        self.i = (self.i + 1) % len(self.items)
        return self.items[self.i]


def mm(kb, out, lhsT, rhs, start, stop, r, w, tp=None):
    if tp is None:
        kb.op("pe", lambda e: e.matmul(out, lhsT=lhsT, rhs=rhs, start=start, stop=stop), r=r, w=w)
    else:
        kb.op("pe", lambda e: e.matmul(out, lhsT=lhsT, rhs=rhs, start=start, stop=stop, tile_position=tp), r=r, w=w)


def pbase_tp(pb):
    return (96, 0) if pb == 96 else None


class Prog:
    def __init__(self, S, depth, branches=(0, 1, 2, 3), debug=False):
        self.S = S
        self.depth = depth
        self.branches = tuple(branches)
        self.NT = S // 128
        self.NCH = S // 512
        self.kb = KB()
        self.nc = self.kb.nc
        self.debug = debug
        self.build()

    def dram_in(self, name, shape, dt=F32):
        return self.nc.dram_tensor(name, list(shape), dt, kind="ExternalInput").ap()

    def build(self):
        kb, nc, S, L = self.kb, self.nc, self.S, self.depth
        di = self.dram_in
        self.x_in = di("x", [S, D])
        self.mem_in = di("mem", [MEM, D])
        self.pos_in = di("pos", [1, S], I32)
        self.cmat_in = di("cmat", [128, 3 * 128])
        self.ccol_in = di("ccol", [128, 4])
        self.g_mix = di("g_mix", [L, 1, D])
        self.g_ffn = di("g_ffn", [L, 1, D])
        self.g_mem = di("g_mem", [L, 1, D])
        self.g_fin = di("g_fin", [1, D])
        self.wA_fm = di("wA_fm", [L, D, 512])
        self.wA_tm = di("wA_tm", [L, D, 260])
        self.wB_fm = di("wB_fm", [L, D, 1536])
        self.wB_tm = di("wB_tm", [L, D, 72])
        self.wC_fm = di("wC_fm", [L, D, 1024])
        self.wC_tm = di("wC_tm", [L, D, 256])
        self.wM_fm = di("wM_fm", [L, D, 256])
        self.w_kv = di("w_kv", [L, D, 512])
        self.b_forget = di("b_forget", [L, 1, 4])
        self.dlam = di("dlam", [L, 1, 128])
        self.subg = di("subg", [L, 128, 1])
        self.w_branch = di("w_branch", [L, 4, 256, D])
        self.w_gate = di("w_gate", [L, D, 4096])
        self.b_gate = di("b_gate", [L, 128, 32])
        self.w_out = di("w_out", [L, D, D])
        self.w_f1 = di("w_f1", [L, D, 2 * FFH])
        self.w_f2 = di("w_f2", [L, FFH, D])
        self.out = nc.dram_tensor("out", [S, D], F32, kind="ExternalOutput").ap()
        it = lambda name, shape, dt: nc.dram_tensor(name, list(shape), dt, kind="Internal").ap()
        self.xres = it("xres", [S, D], F32)
        self.hT = it("hT", [KC, 128, S], BF16)
        self.h2T = it("h2T", [KC, 128, S], BF16)
        self.oT = it("oT", [4, 256, S], BF16)
        self.rope = it("rope", [4, 128, S], F32)

        gsb = lambda shape, dt, nm: kb.ges.enter_context(nc.sbuf_tensor(kb.name(nm), list(shape), dt))
        self.cm = gsb([128, 3, 128], F32, "cm")
        self.cm_r = Res()
        self.ccol = gsb([128, 4], F32, "ccol")
        self.identb = gsb([128, 128], BF16, "identb")
        self.trib = gsb([128, 128], BF16, "trib")
        self.cnegb = gsb([128, 128], BF16, "cnegb")
        self.onesf = gsb([128, 128], F32, "onesf")
        self.onesb = gsb([128, 128], BF16, "onesb")
        self.cst_r = Res()
        kb.dma("sp", self.cm[:].rearrange("p a b -> p (a b)"), self.cmat_in, "cst", w=[self.cm_r])
        kb.dma("sp", self.ccol[:], self.ccol_in, "cst", w=[self.cm_r])
        kb.op("dve", lambda e: e.tensor_copy(out=self.identb[:], in_=self.cm[:, 0, :]), r=[self.cm_r], w=[self.cst_r])
        kb.op("dve", lambda e: e.tensor_copy(out=self.trib[:], in_=self.cm[:, 1, :]), r=[self.cm_r], w=[self.cst_r])
        kb.op("dve", lambda e: e.tensor_scalar(out=self.cnegb[:], in0=self.cm[:, 2, :], scalar1=NEG_B / NEG_S,
                                               scalar2=None, op0=ALU.mult), r=[self.cm_r], w=[self.cst_r])
        kb.op("pool", lambda e: e.memset(self.onesf[:], 1.0), w=[self.cst_r])
        kb.op("pool", lambda e: e.memset(self.onesb[:], 1.0), w=[self.cst_r])
        self.identf = self.cm[:, 0, :]
        self.trif = self.cm[:, 1, :]
        self.cnegf = self.cm[:, 2, :]

        self.phase_rope()
        self.phase_norm0()
        for l in range(L):
            if 0 in self.branches:
                self.phase_fox(l)
            if 1 in self.branches:
                self.phase_dsa(l)
            if 2 in self.branches:
                self.phase_diff(l)
            self.phase_merge(l)
            self.phase_ffn(l, 0, 11, False)
            self.phase_ffn(l, 11, 22, True)
            kb.new_epoch()
        kb.barrier()

    def load_w(self, dst, dst_r, src, nk, chan, q="pool"):
        for k in range(nk):
            self.kb.dma(q, dst[:, k, :], src[k * 128:(k + 1) * 128, :], chan, w=[dst_r])

    def load_bc(self, dst, dst_r, src_row, n, chan):
        self.kb.dma("sp", dst, src_row.to_broadcast([128, n]), chan, w=[dst_r])

    def phase_rope(self):
        kb, S = self.kb, self.S
        with kb.phase() as ph:
            posi = ph.sb([128, S], I32)
            posf = ph.sb([128, S], F32)
            A = ph.sb([128, S], F32)
            B = ph.sb([128, S], F32)
            C = ph.sb([128, S], I32)
            Dd = ph.sb([128, S], F32)
            r_pos, r_A, r_B, r_C, r_D = Res(), Res(), Res(), Res(), Res()
            kb.dma("sp", posi[:], self.pos_in.to_broadcast([128, S]), "rp", w=[r_pos])
            kb.op("dve", lambda e: e.tensor_copy(out=posf[:], in_=posi[:]), r=[r_pos], w=[r_pos])
            for pat in range(2):
                invf = self.ccol[:, pat:pat + 1]
                sgn = self.ccol[:, 2 + pat:3 + pat]
                kb.op("dve", lambda e: e.tensor_scalar(out=A[:], in0=posf[:], scalar1=invf, scalar2=None, op0=ALU.mult),
                      r=[r_pos, self.cm_r], w=[r_A])
                for which in range(2):
                    off = math.pi / 2 if which == 0 else 0.0
                    kb.op("dve", lambda e: e.tensor_scalar(out=B[:], in0=A[:], scalar1=off, scalar2=None, op0=ALU.add),
                          r=[r_A], w=[r_B])
                    kb.op("dve", lambda e: e.tensor_scalar(out=Dd[:], in0=B[:], scalar1=1.0 / TWO_PI, scalar2=None,
                                                           op0=ALU.mult), r=[r_B], w=[r_D])
                    kb.op("dve", lambda e: e.tensor_copy(out=C[:], in_=Dd[:]), r=[r_D], w=[r_C])
                    kb.op("dve", lambda e: e.tensor_copy(out=Dd[:], in_=C[:]), r=[r_C], w=[r_D])
                    kb.op("dve", lambda e: e.scalar_tensor_tensor(out=B[:], in0=Dd[:], scalar=-CW_HI, in1=B[:],
                                                                  op0=ALU.mult, op1=ALU.add), r=[r_D, r_B], w=[r_B])
                    kb.op("dve", lambda e: e.scalar_tensor_tensor(out=B[:], in0=Dd[:], scalar=-CW_LO, in1=B[:],
                                                                  op0=ALU.mult, op1=ALU.add), r=[r_D, r_B], w=[r_B])
                    kb.op("dve", lambda e: e.tensor_scalar(out=Dd[:], in0=B[:], scalar1=math.pi, scalar2=-TWO_PI,
                                                           op0=ALU.is_gt, op1=ALU.mult), r=[r_B], w=[r_D])
                    kb.op("dve", lambda e: e.tensor_tensor(out=B[:], in0=B[:], in1=Dd[:], op=ALU.add), r=[r_B, r_D], w=[r_B])
                    kb.op("dve", lambda e: e.tensor_scalar(out=B[:], in0=B[:], scalar1=-math.pi, scalar2=math.pi,
                                                           op0=ALU.max, op1=ALU.min), r=[r_B], w=[r_B])
                    kb.op("act", lambda e: e.activation(out=Dd[:], in_=B[:], func=AF.Sin), r=[r_B], w=[r_D])
                    if which == 1:
                        kb.op("dve", lambda e: e.tensor_scalar(out=Dd[:], in0=Dd[:], scalar1=sgn, scalar2=None,
                                                               op0=ALU.mult), r=[r_D, self.cm_r], w=[r_D])
                    kb.dma("sp", self.rope[2 * pat + which], Dd[:], "rp", r=[r_D])

    class Normer:
        def __init__(self, prog, ph, g_row, chan):
            kb = prog.kb
            self.prog, self.ph = prog, ph
            self.gbc = ph.sb([128, D], F32, "gbc")
            self.gbc_r = Res()
            prog.load_bc(self.gbc[:], self.gbc_r, g_row, D, chan)
            self.junk = ph.ring(1, [128, D], BF16, "njunk")
            self.st = ph.ring(3, [128, 4], F32, "nst")
            self.hb = ph.ring(2, [128, D], BF16, "nhb")
            self.pT = ph.ring(1, [128, KC, 128], BF16, "npT", psum=True)

        def stats(self, xt, xt_r):
            kb = self.prog.kb
            junk, junk_r, _ = self.junk.next()
            st, st_r, _ = self.st.next()
            kb.op("act", lambda e: e.activation(out=junk[:], in_=xt, func=AF.Square, accum_out=st[:, 0:1]),
                  r=[xt_r], w=[junk_r, st_r])
            kb.op("dve", lambda e: e.tensor_scalar(out=st[:, 1:2], in0=st[:, 0:1], scalar1=1.0 / D, scalar2=EPS,
                                                   op0=ALU.mult, op1=ALU.add), r=[st_r], w=[st_r])
            kb.op("act", lambda e: e.activation(out=st[:, 2:3], in_=st[:, 1:2], func=AF.Ln), r=[st_r], w=[st_r])
            kb.op("act", lambda e: e.activation(out=st[:, 3:4], in_=st[:, 2:3], func=AF.Exp, scale=-0.5), r=[st_r], w=[st_r])
            return st[:, 3:4], st_r

        def to_hT(self, xt, xt_r, dst, dst_r):
            kb, prog = self.prog.kb, self.prog
            rstd, st_r = self.stats(xt, xt_r)
            hb, hb_r, _ = self.hb.next()
            kb.op("dve", lambda e: e.scalar_tensor_tensor(out=hb[:], in0=xt, scalar=rstd, in1=self.gbc[:],
                                                          op0=ALU.mult, op1=ALU.mult), r=[xt_r, st_r, self.gbc_r], w=[hb_r])
            pT, pT_r, _ = self.pT.next()
            for k in range(KC):
                kb.op("pe", lambda e: e.transpose(pT[:, k, :], hb[:, k * 128:(k + 1) * 128], prog.identb[:]),
                      r=[hb_r, prog.cst_r], w=[pT_r])
            kb.op("act", lambda e: e.activation(out=dst, in_=pT[:], func=AF.Copy), r=[pT_r], w=[dst_r])

        def to_out(self, xt, xt_r, dst, dst_r):
            kb = self.prog.kb
            rstd, st_r = self.stats(xt, xt_r)
            kb.op("dve", lambda e: e.scalar_tensor_tensor(out=dst, in0=xt, scalar=rstd, in1=self.gbc[:],
                                                          op0=ALU.mult, op1=ALU.mult), r=[xt_r, st_r, self.gbc_r], w=[dst_r])

    def phase_norm0(self):
        kb = self.kb
        with kb.phase() as ph:
            nm = Prog.Normer(self, ph, self.g_mix[0], "n0g")
            xr = ph.ring(3, [128, D], F32, "n0x")
            hc = ph.ring(2, [128, KC, 512], BF16, "n0h")
            for c in range(self.NCH):
                h, h_r, h_ch = hc.next()
                for i in range(4):
                    t = 4 * c + i
                    xt, xt_r, x_ch = xr.next()
                    kb.dma("sp", xt[:], self.x_in[t * 128:(t + 1) * 128, :], x_ch, w=[xt_r])
                    nm.to_hT(xt[:], xt_r, h[:, :, i * 128:(i + 1) * 128], h_r)
                kb.dma("sp", self.hT[:, :, c * 512:(c + 1) * 512].rearrange("k p s -> p k s"), h[:], h_ch, r=[h_r])

    def attn_finalize(self, po, po_r, rd_ring, dst, dst_r, eng_mul="dve"):
        kb = self.kb
        rd, rd_r, _ = rd_ring.next()
        kb.op("dve", lambda e: e.reciprocal(out=rd[64:128, :], in_=po[64:128, :]), r=[po_r], w=[rd_r])
        kb.op("dve", lambda e: e.tensor_tensor(out=dst, in0=po[0:64, :], in1=rd[64:128, :], op=ALU.mult),
              r=[po_r, rd_r], w=[dst_r])

    def causal_attn(self, *args, **kw):
        for _ in self.causal_attn_gen(*args, **kw):
            pass

    def causal_attn_gen(self, c, kT_ap, kT_r, qT_ap, qT_r, V_ap, V_r, ps_ring, pt_ring, po, po_r, scale,
                        bias_fn=None, bias_r=(), mask_fn=None, tp=None):
        kb = self.kb
        J = 4 * c + 4
        sts = {}

        def emit_S(j):
            i = j - 4 * c
            col0 = max(i, 0) * 128
            ps, ps_r, _ = ps_ring.next()
            extra = mask_fn(j, col0) if mask_fn is not None else []
            mm(kb, ps[:, col0:512], kT_ap(j), qT_ap(col0), True, len(extra) == 0, [kT_r(j), qT_r], [ps_r], tp)
            for n, (lt, rh, c0, c1, rr) in enumerate(extra):
                mm(kb, ps[:, c0:c1], lt, rh, False, n == len(extra) - 1, rr, [ps_r])
            sts[j] = (ps, ps_r, col0, i)

        emit_S(0)
        for j in range(J):
            if j + 1 < J:
                emit_S(j + 1)
            ps, ps_r, col0, i = sts.pop(j)
            pt, pt_r, _ = pt_ring.next()
            if bias_fn is not None:
                b = bias_fn(j)
                kb.op("act", lambda e: e.activation(out=pt[:, col0:512], in_=ps[:, col0:512], func=AF.Exp, scale=scale, bias=b),
                      r=[ps_r] + list(bias_r), w=[pt_r])
            else:
                kb.op("act", lambda e: e.activation(out=pt[:, col0:512], in_=ps[:, col0:512], func=AF.Exp, scale=scale),
                      r=[ps_r], w=[pt_r])
            if i >= 0:
                kb.op("pool", lambda e: e.tensor_tensor(out=pt[:, col0:col0 + 128], in0=pt[:, col0:col0 + 128],
                                                        in1=self.trib[:], op=ALU.mult), r=[pt_r, self.cst_r], w=[pt_r])
            mm(kb, po[:, col0:512], V_ap(j), pt[:, col0:512], j == 0, j == J - 1, [V_r(j), pt_r], [po_r])
            yield

    def store_oT(self, n, c, oTs, oTs_r, chan):
        self.kb.dma("sp", self.oT[n].rearrange("(h p) s -> p h s", p=64)[:, :, c * 512:(c + 1) * 512], oTs[0:64, :, :],
                    chan, r=[oTs_r])

    def proj_fm(self, ps, ps_r, W, W_r, col, h, h_r, n=512):
        for k in range(KC):
            mm(self.kb, ps[:, 0:n], W[:, k, col:col + 128], h[:, k, 0:n], k == 0, k == KC - 1, [W_r, h_r], [ps_r])

    def proj_tm(self, ps, ps_r, W, W_r, col, ncol, h, h_r, i):
        for k in range(KC):
            mm(self.kb, ps[:, 0:ncol], h[:, k, i * 128:(i + 1) * 128], W[:, k, col:col + ncol], k == 0, k == KC - 1,
               [W_r, h_r], [ps_r])

    def load_hT(self, src, ring, c):
        h, h_r, h_ch = ring.next()
        self.kb.dma("sp", h[:], src[:, :, c * 512:(c + 1) * 512].rearrange("k p s -> p k s"), h_ch, w=[h_r])
        return h, h_r

    def rope_evac(self, q_ps, q_r, p_ps, p_r, Ct, St, tab_r, tmp_ring, dst, dst_r):
        kb = self.kb
        t1, t1_r, _ = tmp_ring.next()
        t2, t2_r, _ = tmp_ring.next()
        kb.op("dve", lambda e: e.tensor_tensor(out=t1[:], in0=p_ps, in1=St, op=ALU.mult), r=[p_r, tab_r], w=[t1_r])
        kb.op("dve", lambda e: e.tensor_tensor(out=t2[:], in0=q_ps, in1=Ct, op=ALU.mult), r=[q_r, tab_r], w=[t2_r])
        kb.op("pool", lambda e: e.tensor_tensor(out=dst, in0=t1[:], in1=t2[:], op=ALU.add), r=[t1_r, t2_r], w=[dst_r])

    def phase_fox(self, l):
        kb, S, NT, NCH = self.kb, self.S, self.NT, self.NCH
        with kb.phase() as ph:
            W = ph.sb([128, KC, 772], BF16, "fW")
            W_r = Res()
            self.load_w(W[:, :, 0:512], W_r, self.wA_fm[l], KC, "fW")
            self.load_w(W[:, :, 512:772], W_r, self.wA_tm[l], KC, "fW")
            kT = ph.sb([128, 2, S], BF16, "fkT")
            kT_rs = [Res() for _ in range(NCH)]
            V1 = ph.sb([128, NT, 4, 128], BF16, "fV1")
            V_rs = [Res() for _ in range(NT)]
            ones_r = Res()
            kb.op("pool", lambda e: e.memset(V1[:, :, :, 64:128], 1.0), w=[ones_r])
            for r_ in V_rs:
                r_.w = ones_r.w
            cumn = ph.sb([128, NT, 4], F32, "fcum")
            carry = ph.sb([128, NT + 1, 4], F32, "fcar")
            cum_r = Res()
            kb.op("pool", lambda e: e.memset(carry[:, 0, :], 0.0), w=[cum_r])
            bfg = ph.sb([128, 4], F32, "fbf")
            bfg_r = Res()
            self.load_bc(bfg[:], bfg_r, self.b_forget[l], 4, "fbf")
            hring = ph.ring(2, [128, KC, 512], BF16, "fh")
            qring = ph.ring(2, [128, 2, 512], BF16, "fq")
            pp = ph.ring(2, [128, 512], F32, "fpp", psum=True)
            psr = ph.ring(3, [128, 512], F32, "fps", psum=True)
            por = ph.ring(2, [128, 512], F32, "fpo", psum=True)
            pcs = ph.ring(1, [128, 8], F32, "fpc", psum=True)
            ptr = ph.ring(3, [128, 512], BF16, "fpt")
            rdr = ph.ring(2, [128, 512], F32, "frd")
            otr = ph.ring(2, [128, 4, 512], BF16, "fot")
            fsm = ph.ring(2, [128, 12], F32, "fsm")
            fhl = ph.ring(2, [128, 8], BF16, "fhl")
            bias = ph.ring(2, [128, NT, 4], F32, "fbi")
            for c in range(NCH):
                h, h_r = self.load_hT(self.hT, hring, c)
                qT, qT_r, _ = qring.next()
                for ci in range(2):
                    ps, ps_r, _ = pp.next()
                    self.proj_fm(ps, ps_r, W, W_r, ci * 128, h, h_r)
                    kb.op("act", lambda e: e.activation(out=qT[:, ci, :], in_=ps[:], func=AF.Copy), r=[ps_r], w=[qT_r])
                for ci in range(2):
                    ps, ps_r, _ = pp.next()
                    self.proj_fm(ps, ps_r, W, W_r, 256 + ci * 128, h, h_r)
                    kb.op("dve", lambda e: e.tensor_copy(out=kT[:, ci, c * 512:(c + 1) * 512], in_=ps[:]), r=[ps_r], w=[kT_rs[c]])
                for i in range(4):
                    t = 4 * c + i
                    ps, ps_r, _ = pp.next()
                    self.proj_tm(ps, ps_r, W, W_r, 512, 260, h, h_r, i)
                    kb.op("act", lambda e: e.activation(out=V1[:, t, :, 0:64], in_=ps[:, 0:256].rearrange("p (h d) -> p h d", h=4),
                                                        func=AF.Copy), r=[ps_r], w=[V_rs[t]])
                    if STOP == 1:
                        continue
                    sm, sm_r, _ = fsm.next()
                    kb.op("dve", lambda e: e.tensor_tensor(out=sm[:, 0:4], in0=ps[:, 256:260], in1=bfg[:], op=ALU.add),
                          r=[ps_r, bfg_r], w=[sm_r])
                    kb.op("act", lambda e: e.activation(out=sm[:, 4:8], in_=sm[:, 0:4], func=AF.Exp, scale=-1.0), r=[sm_r], w=[sm_r])
                    kb.op("act", lambda e: e.activation(out=sm[:, 8:12], in_=sm[:, 4:8], func=AF.Ln, bias=1.0), r=[sm_r], w=[sm_r])
                    hl, hl_r, _ = fhl.next()
                    kb.op("dve", lambda e: e.tensor_copy(out=hl[:, 0:4], in_=sm[:, 8:12]), r=[sm_r], w=[hl_r])
                    kb.op("dve", lambda e: e.tensor_copy(out=sm[:, 4:8], in_=hl[:, 0:4]), r=[hl_r], w=[sm_r])
                    kb.op("dve", lambda e: e.tensor_tensor(out=hl[:, 4:8], in0=sm[:, 8:12], in1=sm[:, 4:8], op=ALU.subtract),
                          r=[sm_r], w=[hl_r])
                    pc, pc_r, _ = pcs.next()
                    mm(kb, pc[:, 0:4], self.trib[:], hl[:, 0:4], True, False, [hl_r, self.cst_r], [pc_r])
                    mm(kb, pc[:, 0:4], self.trib[:], hl[:, 4:8], False, True, [hl_r, self.cst_r], [pc_r])
                    mm(kb, pc[:, 4:8], self.onesb[:], hl[:, 0:4], True, False, [hl_r, self.cst_r], [pc_r])
                    mm(kb, pc[:, 4:8], self.onesb[:], hl[:, 4:8], False, True, [hl_r, self.cst_r], [pc_r])
                    kb.op("dve", lambda e: e.tensor_tensor(out=cumn[:, t, :], in0=pc[:, 0:4], in1=carry[:, t, :], op=ALU.add),
                          r=[pc_r, cum_r], w=[cum_r])
                    kb.op("dve", lambda e: e.tensor_tensor(out=carry[:, t + 1, :], in0=pc[:, 4:8], in1=carry[:, t, :], op=ALU.add),
                          r=[pc_r, cum_r], w=[cum_r])
                if STOP in (1, 2):
                    continue
                J = 4 * c + 4
                bi, bi_r, _ = bias.next()
                kb.op("dve", lambda e: e.tensor_tensor(out=bi[:, 0:J, :], in0=cumn[:, 0:J, :],
                                                       in1=carry[:, 4 * c + 2:4 * c + 3, :].to_broadcast([128, J, 4]),
                                                       op=ALU.subtract), r=[cum_r], w=[bi_r])
                if STOP == 3:
                    continue
                oTs, oTs_r, o_ch = otr.next()
                for hd in range(4):
                    cb, pb = hd // 2, (hd % 2) * 64
                    po, po_r, _ = por.next()
                    self.causal_attn(
                        c,
                        lambda j: kT[pb:pb + 64, cb, j * 128:(j + 1) * 128], lambda j: kT_rs[j // 4],
                        lambda col0: qT[pb:pb + 64, cb, col0:512], qT_r,
                        lambda j: V1[:, j, hd, :], lambda j: V_rs[j],
                        psr, ptr, po, po_r, 0.125,
                        bias_fn=lambda j: bi[:, j, hd:hd + 1], bias_r=[bi_r])
                    if STOP == 4:
                        continue
                    self.attn_finalize(po, po_r, rdr, oTs[0:64, hd, :], oTs_r)
                if STOP in (4, 5):
                    continue
                self.store_oT(0, c, oTs, oTs_r, o_ch)

    def phase_dsa(self, l):
        kb, S, NT, NCH = self.kb, self.S, self.NT, self.NCH
        with kb.phase() as ph:
            W = ph.sb([128, KC, 1608], BF16, "bW")
            W_r = Res()
            self.load_w(W[:, :, 0:1536], W_r, self.wB_fm[l], KC, "bW")
            self.load_w(W[:, :, 1536:1608], W_r, self.wB_tm[l], KC, "bW")
            kbT = ph.sb([128, S], BF16, "bkb")
            kiT = ph.sb([128, S], BF16, "bki")
            k_rs = [Res() for _ in range(NCH)]
            V1 = ph.sb([128, NT, 128], BF16, "bV1")
            V_rs = [Res() for _ in range(NT)]
            ones_r = Res()
            kb.op("pool", lambda e: e.memset(V1[:, :, 64:128], 1.0), w=[ones_r])
            for r_ in V_rs:
                r_.w = ones_r.w
            hring = ph.ring(1, [128, KC, 512], BF16, "bh")
            tabr = ph.ring(1, [128, 4, 512], F32, "btab")
            qbr = ph.ring(2, [128, 2, 512], BF16, "bqb")
            qir = ph.ring(2, [128, 2, 512], BF16, "bqi")
            tmpr = ph.ring(2, [128, 512], F32, "btmp")
            wir = ph.ring(2, [128, 4, 8], F32, "bwi")
            dgr = ph.ring(2, [128, 8, 128], BF16, "bdg")
            Rr = ph.ring(8, [128, 512], BF16, "bR")
            scs = [ph.sb([128, S], F32, "bsc") for _ in range(2)]
            sc_rs = [Res(), Res()]
            mbs = [[ph.sb([128, S], BF16, "bmb") for _ in range(4)] for _ in range(2)]
            mb_rs = [[Res() for _ in range(4)] for _ in range(2)]
            bst = ph.ring(4, [128, 8 + 2 * (NBIS + 1) + 2], F32, "bst")
            pp = ph.ring(2, [128, 512], F32, "bpp", psum=True)
            psr = ph.ring(3, [128, 512], F32, "bps", psum=True)
            por = ph.ring(2, [128, 512], F32, "bpo", psum=True)
            pscr = ph.ring(1, [128, 512], F32, "bpsc", psum=True)
            ptr = ph.ring(3, [128, 512], BF16, "bpt")
            rdr = ph.ring(1, [128, 512], F32, "brd")
            otr = ph.ring(2, [128, 4, 512], BF16, "bot")
            W0 = 8
            M0 = W0 + NBIS + 1

            def proj(c):
                h, h_r = self.load_hT(self.hT, hring, c)
                tab, tab_r, t_ch = tabr.next()
                kb.dma("sp", tab[:], self.rope[:, :, c * 512:(c + 1) * 512].rearrange("t p s -> p t s"), t_ch, w=[tab_r])
                qb, qb_r, _ = qbr.next()
                qi, qi_r, _ = qir.next()

                def roped2(col, colp, pat, dst, dst_r):
                    ps, ps_r, _ = pp.next()
                    self.proj_fm(ps, ps_r, W, W_r, col, h, h_r)
                    p2, p2_r, _ = pp.next()
                    self.proj_fm(p2, p2_r, W, W_r, colp, h, h_r)
                    self.rope_evac(ps[:], ps_r, p2[:], p2_r, tab[:, 2 * pat, :], tab[:, 2 * pat + 1, :], tab_r, tmpr, dst, dst_r)

                for ci in range(2):
                    roped2(ci * 128, 256 + ci * 128, 0, qb[:, ci, :], qb_r)
                roped2(512, 640, 0, kbT[:, c * 512:(c + 1) * 512], k_rs[c])
                for ci in range(2):
                    roped2(768 + ci * 128, 1024 + ci * 128, 1, qi[:, ci, :], qi_r)
                roped2(1280, 1408, 1, kiT[:, c * 512:(c + 1) * 512], k_rs[c])
                wi, wi_r, _ = wir.next()
                for i in range(4):
                    t = 4 * c + i
                    ps, ps_r, _ = pp.next()
                    self.proj_tm(ps, ps_r, W, W_r, 1536, 72, h, h_r, i)
                    kb.op("act", lambda e: e.activation(out=V1[:, t, 0:64], in_=ps[:, 0:64], func=AF.Copy), r=[ps_r], w=[V_rs[t]])
                    kb.op("act", lambda e: e.activation(out=wi[:, i, :], in_=ps[:, 64:72], func=AF.Copy,
                                                        scale=(8.0 ** -0.5) * (32.0 ** -0.5)), r=[ps_r], w=[wi_r])
                jD = tab[:].rearrange("p a b -> p (a b)").bitcast(BF16)
                jA = h[:].rearrange("p k s -> p (k s)")
                return dict(qb=qb, qb_r=qb_r, qi=qi, qi_r=qi_r, wi=wi, wi_r=wi_r, jD=jD, jD_r=tab_r, jA=jA, jA_r=h_r)

            def trivial(c, i):
                qt = 4 * c + i
                N = (qt + 1) * 128
                mb, mb_r = mbs[c % 2][i], mb_rs[c % 2][i]
                if qt == 1:
                    kb.op("pool", lambda e: e.memset(mb[:, 0:128], 0.0), w=[mb_r])
                kb.op("pool", lambda e: e.tensor_copy(out=mb[:, N - 128:N], in_=self.cnegb[:]), r=[self.cst_r], w=[mb_r])

            def scores(c, i, P, sc, sc_r):
                qt = 4 * c + i
                N = (qt + 1) * 128
                qi, qi_r, wi, wi_r = P["qi"], P["qi_r"], P["wi"], P["wi_r"]
                dg, dg_r, _ = dgr.next()
                for hh in range(8):
                    kb.op("dve", lambda e: e.tensor_scalar(out=dg[:, hh, :], in0=self.identf, scalar1=wi[:, i, hh:hh + 1],
                                                           scalar2=None, op0=ALU.mult), r=[wi_r, self.cm_r], w=[dg_r])
                nkc = (N + 511) // 512
                for kc_ in range(nkc):
                    k0 = kc_ * 512
                    kn = min(512, N - k0)
                    Rs = []
                    for hh in range(8):
                        pbh = (hh % 4) * 32
                        ps, ps_r, _ = pp.next()
                        mm(kb, ps[:, 0:kn], qi[pbh:pbh + 32, hh // 4, i * 128:(i + 1) * 128], kiT[pbh:pbh + 32, k0:k0 + kn],
                           True, True, [qi_r, k_rs[kc_]], [ps_r], pbase_tp(pbh))
                        R, R_r, _ = Rr.next()
                        if hh % 2 == 0:
                            kb.op("act", lambda e: e.activation(out=R[:, 0:kn], in_=ps[:, 0:kn], func=AF.Relu), r=[ps_r], w=[R_r])
                        else:
                            kb.op("dve", lambda e: e.tensor_scalar(out=R[:, 0:kn], in0=ps[:, 0:kn], scalar1=0.0, scalar2=None,
                                                                   op0=ALU.max), r=[ps_r], w=[R_r])
                        Rs.append((R, R_r))
                    psc, psc_r, _ = pscr.next()
                    for hh in range(8):
                        mm(kb, psc[:, 0:kn], dg[:, hh, :], Rs[hh][0][:, 0:kn], hh == 0, hh == 7, [dg_r, Rs[hh][1]], [psc_r])
                    if kc_ == nkc - 1:
                        dcol = kn - 128
                        if dcol > 0:
                            kb.op("act", lambda e: e.activation(out=sc[:, k0:k0 + dcol], in_=psc[:, 0:dcol], func=AF.Copy),
                                  r=[psc_r], w=[sc_r])
                        kb.op("dve", lambda e: e.tensor_tensor(out=sc[:, k0 + dcol:k0 + kn], in0=psc[:, dcol:kn],
                                                               in1=self.cnegf, op=ALU.add), r=[psc_r, self.cm_r], w=[sc_r])
                    else:
                        kb.op("act", lambda e: e.activation(out=sc[:, k0:k0 + kn], in_=psc[:, 0:kn], func=AF.Copy),
                              r=[psc_r], w=[sc_r])

            def bisect_pair(c, P):
                def gen(iA, iB):
                    tiles = []
                    for slot, i in ((0, iA), (1, iB)):
                        scores(c, i, P, scs[slot], sc_rs[slot])
                        yield
                    for slot, i in ((0, iA), (1, iB)):
                        neg = (slot == 1)
                        sc, sc_r = scs[slot], sc_rs[slot]
                        N = (4 * c + i + 1) * 128
                        st, st_r, _ = bst.next()
                        kb.op("dve", lambda e: e.tensor_reduce(out=st[:, 0:1], in_=sc[:, 0:N], axis=mybir.AxisListType.X, op=ALU.max),
                              r=[sc_r], w=[st_r])
                        kb.op("dve", lambda e: e.tensor_reduce(out=st[:, 1:2], in_=sc[:, 0:N - 128], axis=mybir.AxisListType.X, op=ALU.min),
                              r=[sc_r], w=[st_r])
                        kb.op("dve", lambda e: e.tensor_tensor(out=st[:, 2:3], in0=st[:, 0:1], in1=st[:, 1:2], op=ALU.subtract), r=[st_r], w=[st_r])
                        sg = -1.0 if neg else 1.0
                        for k in range(NBIS + 1):
                            kb.op("dve", lambda e: e.tensor_scalar(out=st[:, W0 + k:W0 + k + 1], in0=st[:, 2:3],
                                                                   scalar1=sg * (1.0 + 2.0 ** -10) * 2.0 ** -(k + 1), scalar2=None, op0=ALU.mult),
                                  r=[st_r], w=[st_r])
                        if not neg:
                            kb.op("dve", lambda e: e.tensor_tensor(out=st[:, M0:M0 + 1], in0=st[:, 1:2], in1=st[:, W0:W0 + 1], op=ALU.add), r=[st_r], w=[st_r])
                        else:
                            kb.op("dve", lambda e: e.tensor_tensor(out=st[:, M0:M0 + 1], in0=st[:, W0:W0 + 1], in1=st[:, 1:2], op=ALU.subtract), r=[st_r], w=[st_r])
                        tiles.append((i, N, sc, sc_r, st, st_r, neg))
                    yield
                    for k in range(NBIS):
                        for (i, N, sc, sc_r, st, st_r, neg) in tiles:
                            mid = st[:, M0 + k:M0 + k + 1]
                            if not neg:
                                kb.op("dve", lambda e: e.tensor_scalar(out=P["jD"][:, 0:N], in0=sc[:, 0:N], scalar1=mid, scalar2=0.0,
                                                                       op0=ALU.is_ge, op1=ALU.add, accum_out=st[:, 3:4]),
                                      r=[sc_r, st_r], w=[P["jD_r"], st_r])
                            else:
                                kb.op("act", lambda e: e.activation(out=P["jA"][:, 0:N], in_=sc[:, 0:N], func=AF.Sign, bias=mid,
                                                                    accum_out=st[:, 3:4]), r=[sc_r, st_r], w=[P["jA_r"], st_r])
                        for (i, N, sc, sc_r, st, st_r, neg) in tiles:
                            mid = st[:, M0 + k:M0 + k + 1]
                            cmpv = float(2 * TOPK - N) if neg else float(TOPK)
                            kb.op("dve", lambda e: e.tensor_scalar(out=st[:, 4:5], in0=st[:, 3:4], scalar1=cmpv, scalar2=st[:, W0 + k:W0 + k + 1],
                                                                   op0=ALU.is_ge, op1=ALU.mult), r=[st_r], w=[st_r])
                            kb.op("dve", lambda e: e.scalar_tensor_tensor(out=st[:, M0 + k + 1:M0 + k + 2], in0=st[:, 4:5],
                                                                          scalar=st[:, W0 + k + 1:W0 + k + 2], in1=mid,
                                                                          op0=ALU.subtract, op1=ALU.add), r=[st_r], w=[st_r])
                        yield
                    for (i, N, sc, sc_r, st, st_r, neg) in tiles:
                        mb, mb_r = mbs[c % 2][i], mb_rs[c % 2][i]
                        if not neg:
                            kb.op("dve", lambda e: e.tensor_tensor(out=st[:, 5:6], in0=st[:, M0 + NBIS:M0 + NBIS + 1],
                                                                   in1=st[:, W0 + NBIS:W0 + NBIS + 1], op=ALU.subtract), r=[st_r], w=[st_r])
                        else:
                            kb.op("dve", lambda e: e.scalar_tensor_tensor(out=st[:, 5:6], in0=st[:, M0 + NBIS:M0 + NBIS + 1], scalar=-1.0,
                                                                          in1=st[:, W0 + NBIS:W0 + NBIS + 1], op0=ALU.mult, op1=ALU.add),
                                  r=[st_r], w=[st_r])
                        kb.op("dve", lambda e: e.tensor_scalar(out=mb[:, 0:N], in0=sc[:, 0:N], scalar1=st[:, 5:6], scalar2=NEG_B,
                                                               op0=ALU.is_lt, op1=ALU.mult), r=[sc_r, st_r], w=[mb_r])
                        yield
                return gen

            def attn_heads(c, P, heads, oTs, oTs_r):
                qb, qb_r = P["qb"], P["qb_r"]
                for hd in heads:
                    cb, pb = hd // 2, (hd % 2) * 64
                    po, po_r, _ = por.next()

                    def mask_fn(j, col0):
                        ex = []
                        for i2 in range(col0 // 128, 4):
                            ex.append((mbs[c % 2][i2][:, j * 128:(j + 1) * 128], self.identb[:], i2 * 128, (i2 + 1) * 128,
                                       [mb_rs[c % 2][i2], self.cst_r]))
                        return ex

                    yield from self.causal_attn_gen(
                        c,
                        lambda j: kbT[pb:pb + 64, j * 128:(j + 1) * 128], lambda j: k_rs[j // 4],
                        lambda col0: qb[pb:pb + 64, cb, col0:512], qb_r,
                        lambda j: V1[:, j, :], lambda j: V_rs[j],
                        psr, ptr, po, po_r, 0.125, mask_fn=mask_fn)
                    self.attn_finalize(po, po_r, rdr, oTs[0:64, hd, :], oTs_r)
                    yield

            def zip_run(ga, na, gb, nb):
                done_a = done_b = False
                ia = ib = 0
                while not (done_a and done_b):
                    if not done_a and (done_b or ia * max(nb, 1) <= ib * max(na, 1)):
                        try:
                            next(ga)
                            ia += 1
                        except StopIteration:
                            done_a = True
                    else:
                        try:
                            next(gb)
                            ib += 1
                        except StopIteration:
                            done_b = True

            def empty():
                return
                yield

            Ps = {0: proj(0)}
            trivial(0, 0)
            trivial(0, 1)
            for _ in bisect_pair(0, Ps[0])(2, 3):
                pass
            for c in range(NCH):
                nxt = c + 1 < NCH
                if nxt:
                    Ps[c + 1] = proj(c + 1)
                oTs, oTs_r, o_ch = otr.next()
                for half in range(2):
                    ga = attn_heads(c, Ps[c], (2 * half, 2 * half + 1), oTs, oTs_r)
                    na = 2 * (4 * c + 5)
                    if nxt:
                        gb = bisect_pair(c + 1, Ps[c + 1])(2 * half, 2 * half + 1)
                        nb = NBIS + 5
                    else:
                        gb, nb = empty(), 0
                    zip_run(ga, na, gb, nb)
                self.store_oT(1, c, oTs, oTs_r, o_ch)

    def phase_diff(self, l):
        kb, S, NT, NCH = self.kb, self.S, self.NT, self.NCH
        lam_init = 0.8 - 0.6 * math.exp(-0.3 * l)
        with kb.phase() as ph:
            W = ph.sb([128, KC, 1280], BF16, "cW")
            W_r = Res()
            self.load_w(W[:, :, 0:1024], W_r, self.wC_fm[l], KC, "cW")
            self.load_w(W[:, :, 1024:1280], W_r, self.wC_tm[l], KC, "cW")
            kT = ph.sb([128, 2, S], BF16, "ckT")
            kT_rs = [Res() for _ in range(NCH)]
            V1 = ph.sb([128, NT, 4, 128], BF16, "cV1")
            V_rs = [Res() for _ in range(NT)]
            ones_r = Res()
            kb.op("pool", lambda e: e.memset(V1[:, :, :, 64:128], 1.0), w=[ones_r])
            for r_ in V_rs:
                r_.w = ones_r.w
            lm = ph.sb([128, 128], F32, "clm")
            ls = ph.sb([128, 8], F32, "cls")
            lm_r = Res()
            self.load_bc(lm[:], lm_r, self.dlam[l], 128, "clm")
            kb.op("dve", lambda e: e.tensor_tensor(out=lm[:, 0:32], in0=lm[:, 0:32], in1=lm[:, 32:64], op=ALU.mult), r=[lm_r], w=[lm_r])
            kb.op("dve", lambda e: e.tensor_tensor(out=lm[:, 64:96], in0=lm[:, 64:96], in1=lm[:, 96:128], op=ALU.mult), r=[lm_r], w=[lm_r])
            kb.op("dve", lambda e: e.tensor_reduce(out=ls[:, 0:1], in_=lm[:, 0:32], axis=mybir.AxisListType.X, op=ALU.add), r=[lm_r], w=[lm_r])
            kb.op("dve", lambda e: e.tensor_reduce(out=ls[:, 1:2], in_=lm[:, 64:96], axis=mybir.AxisListType.X, op=ALU.add), r=[lm_r], w=[lm_r])
            kb.op("act", lambda e: e.activation(out=ls[:, 2:4], in_=ls[:, 0:2], func=AF.Exp), r=[lm_r], w=[lm_r])
            kb.op("dve", lambda e: e.tensor_tensor(out=ls[:, 4:5], in0=ls[:, 2:3], in1=ls[:, 3:4], op=ALU.subtract), r=[lm_r], w=[lm_r])
            kb.op("dve", lambda e: e.tensor_scalar(out=ls[:, 5:6], in0=ls[:, 4:5], scalar1=lam_init, scalar2=-1.0, op0=ALU.add, op1=ALU.mult),
                  r=[lm_r], w=[lm_r])
            sg = ph.sb([128, 2], F32, "csg")
            sg_r = Res()
            kb.dma("sp", sg[:, 0:1], self.subg[l], "csg", w=[sg_r])
            kb.op("dve", lambda e: e.tensor_scalar(out=sg[:, 1:2], in0=sg[:, 0:1], scalar1=(1.0 - lam_init), scalar2=None, op0=ALU.mult),
                  r=[sg_r], w=[sg_r])
            hring = ph.ring(2, [128, KC, 512], BF16, "ch")
            tabr = ph.ring(1, [128, 2, 512], F32, "ctab")
            qring = ph.ring(2, [128, 2, 512], BF16, "cq")
            tmpr = ph.ring(4, [128, 512], F32, "ctmp")
            pp = ph.ring(2, [128, 512], F32, "cpp", psum=True)
            psr = ph.ring(3, [128, 512], F32, "cps", psum=True)
            por = ph.ring(2, [128, 512], F32, "cpo", psum=True)
            pssr = ph.ring(1, [128, 512], F32, "cpss", psum=True)
            ptr = ph.ring(3, [128, 512], BF16, "cpt")
            rdr = ph.ring(2, [128, 512], F32, "crd")
            otr = ph.ring(2, [128, 4, 512], BF16, "cot")
            o0r = ph.ring(2, [128, 512], F32, "co0")
            o1r = ph.ring(2, [128, 512], F32, "co1")
            sqr = ph.ring(2, [128, 512], F32, "csq")
            sqbr = ph.ring(2, [128, 512], BF16, "csqb")
            for c in range(NCH):
                h, h_r = self.load_hT(self.hT, hring, c)
                tab, tab_r, t_ch = tabr.next()
                kb.dma("sp", tab[:], self.rope[2:4, :, c * 512:(c + 1) * 512].rearrange("t p s -> p t s"), t_ch, w=[tab_r])
                qT, qT_r, _ = qring.next()

                def roped2(col, colp, dst, dst_r):
                    ps, ps_r, _ = pp.next()
                    self.proj_fm(ps, ps_r, W, W_r, col, h, h_r)
                    p2, p2_r, _ = pp.next()
                    self.proj_fm(p2, p2_r, W, W_r, colp, h, h_r)
                    self.rope_evac(ps[:], ps_r, p2[:], p2_r, tab[:, 0, :], tab[:, 1, :], tab_r, tmpr, dst, dst_r)

                for ci in range(2):
                    roped2(ci * 128, 256 + ci * 128, qT[:, ci, :], qT_r)
                for ci in range(2):
                    roped2(512 + ci * 128, 768 + ci * 128, kT[:, ci, c * 512:(c + 1) * 512], kT_rs[c])
                for i in range(4):
                    t = 4 * c + i
                    ps, ps_r, _ = pp.next()
                    self.proj_tm(ps, ps_r, W, W_r, 1024, 256, h, h_r, i)
                    kb.op("act", lambda e: e.activation(out=V1[:, t, :, 0:64], in_=ps[:, 0:256].rearrange("p (h d) -> p h d", h=4),
                                                        func=AF.Copy), r=[ps_r], w=[V_rs[t]])
                oTs, oTs_r, o_ch = otr.next()
                for hd in range(4):
                    cb = hd // 2
                    outs = []
                    for m in range(2):
                        pb = ((hd % 2) * 2 + m) * 32
                        po, po_r, _ = por.next()
                        self.causal_attn(
                            c,
                            lambda j: kT[pb:pb + 32, cb, j * 128:(j + 1) * 128], lambda j: kT_rs[j // 4],
                            lambda col0: qT[pb:pb + 32, cb, col0:512], qT_r,
                            lambda j: V1[:, j, hd, :], lambda j: V_rs[j],
                            psr, ptr, po, po_r, 32.0 ** -0.5, tp=pbase_tp(pb))
                        o, o_r, _ = (o0r if m == 0 else o1r).next()
                        self.attn_finalize(po, po_r, rdr, o[0:64, :], o_r)
                        outs.append((o, o_r))
                    (o0, o0_r), (o1, o1_r) = outs
                    kb.op("dve", lambda e: e.scalar_tensor_tensor(out=o0[0:64, :], in0=o1[0:64, :], scalar=ls[0:64, 5:6], in1=o0[0:64, :],
                                                                  op0=ALU.mult, op1=ALU.add), r=[o0_r, o1_r, lm_r], w=[o0_r])
                    sq, sq_r, _ = sqr.next()
                    sqb, sqb_r, _ = sqbr.next()
                    kb.op("pool", lambda e: e.tensor_tensor(out=sqb[0:64, :], in0=o0[0:64, :], in1=o0[0:64, :], op=ALU.mult), r=[o0_r], w=[sqb_r])
                    pss, pss_r, _ = pssr.next()
                    mm(kb, pss[0:64, :], self.onesb[0:64, 0:64], sqb[0:64, :], True, True, [sqb_r, self.cst_r], [pss_r])
                    kb.op("dve", lambda e: e.tensor_scalar(out=sq[0:64, :], in0=pss[0:64, :], scalar1=1.0 / 64, scalar2=EPS,
                                                           op0=ALU.mult, op1=ALU.add), r=[pss_r], w=[sq_r])
                    kb.op("act", lambda e: e.activation(out=sq[0:64, :], in_=sq[0:64, :], func=AF.Ln), r=[sq_r], w=[sq_r])
                    kb.op("act", lambda e: e.activation(out=sq[0:64, :], in_=sq[0:64, :], func=AF.Exp, scale=-0.5), r=[sq_r], w=[sq_r])
                    kb.op("dve", lambda e: e.scalar_tensor_tensor(out=oTs[0:64, hd, :], in0=o0[0:64, :], scalar=sg[0:64, 1:2], in1=sq[0:64, :],
                                                                  op0=ALU.mult, op1=ALU.mult), r=[o0_r, sq_r, sg_r], w=[oTs_r])
                self.store_oT(2, c, oTs, oTs_r, o_ch)

    def phase_mem(self, l, after_w=None):
        kb, S, NT, NCH = self.kb, self.S, self.NT, self.NCH
        with kb.phase() as ph:
            W = ph.sb([128, KC, 768], BF16, "mW")
            W_r = Res()
            self.load_w(W[:, :, 0:256], W_r, self.wM_fm[l], KC, "mW")
            self.load_w(W[:, :, 256:768], W_r, self.w_kv[l], KC, "mW")
            if after_w is not None:
                after_w()
            nm = Prog.Normer(self, ph, self.g_mem[l], "mg")
            xr = ph.ring(2, [128, D], F32, "mx")
            hm = ph.sb([128, KC, MEM], BF16, "mhm")
            hm_r = Res()
            for t in range(2):
                xt, xt_r, x_ch = xr.next()
                kb.dma("sp", xt[:], self.mem_in[t * 128:(t + 1) * 128, :], x_ch, w=[xt_r])
                nm.to_hT(xt[:], xt_r, hm[:, :, t * 128:(t + 1) * 128], hm_r)
            KmT = ph.sb([128, 2, MEM], BF16, "mK")
            Vm = ph.sb([128, 2, 4, 128], BF16, "mV")
            kv_r = Res()
            kb.op("pool", lambda e: e.memset(Vm[:, :, :, 64:128], 1.0), w=[kv_r])
            pp = ph.ring(2, [128, 512], F32, "mpp", psum=True)
            psr = ph.ring(2, [128, 512], F32, "mps", psum=True)
            por = ph.ring(2, [128, 512], F32, "mpo", psum=True)
            for ci in range(2):
                ps, ps_r, _ = pp.next()
                self.proj_fm(ps, ps_r, W, W_r, 256 + ci * 128, hm, hm_r, n=MEM)
                kb.op("act", lambda e: e.activation(out=KmT[:, ci, :], in_=ps[:, 0:MEM], func=AF.Copy), r=[ps_r], w=[kv_r])
            for t in range(2):
                ps, ps_r, _ = pp.next()
                self.proj_tm(ps, ps_r, W, W_r, 512, 256, hm, hm_r, t)
                kb.op("act", lambda e: e.activation(out=Vm[:, t, :, 0:64], in_=ps[:, 0:256].rearrange("p (h d) -> p h d", h=4),
                                                    func=AF.Copy), r=[ps_r], w=[kv_r])
            hring = ph.ring(2, [128, KC, 512], BF16, "mh")
            qring = ph.ring(2, [128, 2, 512], BF16, "mq")
            ptr = ph.ring(3, [128, 512], BF16, "mpt")
            rdr = ph.ring(2, [128, 512], F32, "mrd")
            otr = ph.ring(2, [128, 4, 512], BF16, "mot")
            for c in range(NCH):
                h, h_r = self.load_hT(self.hT, hring, c)
                qT, qT_r, _ = qring.next()
                for ci in range(2):
                    ps, ps_r, _ = pp.next()
                    self.proj_fm(ps, ps_r, W, W_r, ci * 128, h, h_r)
                    kb.op("act", lambda e: e.activation(out=qT[:, ci, :], in_=ps[:], func=AF.Copy), r=[ps_r], w=[qT_r])
                oTs, oTs_r, o_ch = otr.next()
                for hd in range(4):
                    cb, pb = hd // 2, (hd % 2) * 64
                    po, po_r, _ = por.next()
                    for t in range(2):
                        ps, ps_r, _ = psr.next()
                        mm(kb, ps[:], KmT[pb:pb + 64, cb, t * 128:(t + 1) * 128], qT[pb:pb + 64, cb, :], True, True, [kv_r, qT_r], [ps_r])
                        pt, pt_r, _ = ptr.next()
                        kb.op("act", lambda e: e.activation(out=pt[:], in_=ps[:], func=AF.Exp, scale=0.125), r=[ps_r], w=[pt_r])
                        mm(kb, po[:], Vm[:, t, hd, :], pt[:], t == 0, t == 1, [kv_r, pt_r], [po_r])
                    self.attn_finalize(po, po_r, rdr, oTs[0:64, hd, :], oTs_r)
                self.store_oT(3, c, oTs, oTs_r, o_ch)

    def phase_merge(self, l):
        kb, S, NT, NCH = self.kb, self.S, self.NT, self.NCH
        x_src = self.x_in if l == 0 else self.xres
        with kb.phase() as ph:
            nb = len(self.branches)
            Wg = ph.sb([128, KC, 4096], BF16, "gWg")
            Wb = ph.sb([128, 4, 2, D], BF16, "gWb")
            Wo = ph.sb([128, KC, D], BF16, "gWo")
            bg = ph.sb([128, 32], F32, "gbg")
            Wg_r, Wb_r, Wo_r, bg_r = Res(), Res(), Res(), Res()

            def loads():
                kb.dma("sp", bg[:], self.b_gate[l], "gbg", w=[bg_r])
                for n in self.branches:
                    self.load_w(Wb[:, n, :, :], Wb_r, self.w_branch[l, n], 2, "gWb")
                for k in range(KC):
                    for n in self.branches:
                        kb.dma("pool", Wg[:, k, n * 1024:(n + 1) * 1024], self.w_gate[l, k * 128:(k + 1) * 128, n * 1024:(n + 1) * 1024],
                               "gWg", w=[Wg_r])
                self.load_w(Wo, Wo_r, self.w_out[l], KC, "gWo")

            if 3 in self.branches:
                self.phase_mem(l, after_w=loads)
            else:
                loads()
            nm = Prog.Normer(self, ph, self.g_ffn[l], "gng")
            hring = ph.ring(1, [128, KC, 512], BF16, "gh")
            oring = ph.ring(1, [128, 4, 2, 512], BF16, "go")
            xr = ph.ring(3, [128, D], F32, "gx")
            gsr = ph.ring(2, [128, 512], F32, "ggs")
            tmr = ph.ring(2, [128, 512], F32, "gtm")
            acr = ph.ring(2, [128, 512], F32, "gac")
            mT = ph.sb([128, KC, 512], BF16, "gmT")
            mT_r = Res()
            h2r = ph.ring(1, [128, KC, 512], BF16, "gh2")
            pyr = ph.ring(2, [128, 512], F32, "gpy", psum=True)
            pgr = ph.ring(2, [128, 512], F32, "gpg", psum=True)
            pxr = ph.ring(2, [128, 512], F32, "gpx", psum=True)
            for c in range(NCH):
                h, h_r = self.load_hT(self.hT, hring, c)
                o, o_r, o_ch = oring.next()
                for n in self.branches:
                    kb.dma("sp", o[:, n, :, :], self.oT[n, :, c * 512:(c + 1) * 512].rearrange("(k p) s -> p k s", p=128), o_ch, w=[o_r])
                for f in range(KC):
                    acc, acc_r, _ = acr.next()
                    for bi_, n in enumerate(self.branches):
                        py, py_r, _ = pyr.next()
                        for k in range(2):
                            mm(kb, py[:], Wb[:, n, k, f * 128:(f + 1) * 128], o[:, n, k, :], k == 0, k == 1, [Wb_r, o_r], [py_r])
                        pg, pg_r, _ = pgr.next()
                        for k in range(KC):
                            mm(kb, pg[:], Wg[:, k, n * 1024 + f * 128:n * 1024 + (f + 1) * 128], h[:, k, :], k == 0, k == KC - 1,
                               [Wg_r, h_r], [pg_r])
                        gs, gs_r, _ = gsr.next()
                        kb.op("act", lambda e: e.activation(out=gs[:], in_=pg[:], func=AF.Sigmoid, bias=bg[:, n * 8 + f:n * 8 + f + 1]),
                              r=[pg_r, bg_r], w=[gs_r])
                        lastb = (bi_ == nb - 1)
                        if bi_ == 0:
                            dst, dst_r = (mT[:, f, :], mT_r) if lastb else (acc[:], acc_r)
                            kb.op("dve", lambda e: e.tensor_tensor(out=dst, in0=py[:], in1=gs[:], op=ALU.mult), r=[py_r, gs_r], w=[dst_r])
                        else:
                            tm, tm_r, _ = tmr.next()
                            kb.op("dve", lambda e: e.tensor_tensor(out=tm[:], in0=py[:], in1=gs[:], op=ALU.mult), r=[py_r, gs_r], w=[tm_r])
                            dst, dst_r = (mT[:, f, :], mT_r) if lastb else (acc[:], acc_r)
                            kb.op("pool", lambda e: e.tensor_tensor(out=dst, in0=acc[:], in1=tm[:], op=ALU.add), r=[acc_r, tm_r], w=[dst_r] if lastb else [acc_r])
                h2, h2_r, h2_ch = h2r.next()
                for i in range(4):
                    t = 4 * c + i
                    xt, xt_r, x_ch = xr.next()
                    kb.dma("sp", xt[:], x_src[t * 128:(t + 1) * 128, :], x_ch, w=[xt_r])
                    for hh in range(2):
                        px, px_r, _ = pxr.next()
                        for k in range(KC):
                            mm(kb, px[:], mT[:, k, i * 128:(i + 1) * 128], Wo[:, k, hh * 512:(hh + 1) * 512], k == 0, k == KC - 1,
                               [mT_r, Wo_r], [px_r])
                        kb.op("dve", lambda e: e.tensor_tensor(out=xt[:, hh * 512:(hh + 1) * 512], in0=px[:], in1=xt[:, hh * 512:(hh + 1) * 512],
                                                               op=ALU.add), r=[px_r, xt_r], w=[xt_r])
                    kb.dma("sp", self.xres[t * 128:(t + 1) * 128, :], xt[:], x_ch, r=[xt_r])
                    nm.to_hT(xt[:], xt_r, h2[:, :, i * 128:(i + 1) * 128], h2_r)
                kb.dma("sp", self.h2T[:, :, c * 512:(c + 1) * 512].rearrange("k p s -> p k s"), h2[:], h2_ch, r=[h2_r])

    def phase_ffn(self, l, j0, j1, last):
        kb, S, NT, NCH = self.kb, self.S, self.NT, self.NCH
        nj = j1 - j0
        final = last and (l == self.depth - 1)
        with kb.phase() as ph:
            W1 = ph.sb([128, KC, 2, nj * 128], BF16, "fW1")
            W2 = ph.sb([128, nj, D], BF16, "fW2")
            W1_r, W2_r = Res(), Res()
            for k in range(KC):
                for u in range(2):
                    kb.dma("pool", W1[:, k, u, :], self.w_f1[l, k * 128:(k + 1) * 128, u * FFH + j0 * 128:u * FFH + j1 * 128], "fW1", w=[W1_r])
            self.load_w(W2, W2_r, self.w_f2[l, j0 * 128:j1 * 128, :], nj, "fW2")
            if last:
                g_row = self.g_fin if final else self.g_mix[l + 1]
                nm = Prog.Normer(self, ph, g_row, "fng")
            hring = ph.ring(2, [128, KC, 512], BF16, "fh")
            xr = ph.ring(3, [128, D], F32, "fx")
            sir = ph.ring(2, [128, 512], F32, "fsi")
            aT = ph.sb([128, nj, 512], BF16, "faT")
            aT_r = Res()
            hor = ph.ring(1, [128, KC, 512], BF16, "fho")
            outr = ph.ring(2, [128, D], F32, "fout")
            p1r = ph.ring(2, [128, 512], F32, "fp1", psum=True)
            p2r = ph.ring(2, [128, 512], F32, "fp2", psum=True)
            pxr = ph.ring(2, [128, 512], F32, "fpx", psum=True)
            for c in range(NCH):
                h, h_r = self.load_hT(self.h2T, hring, c)
                for j in range(nj):
                    p1, p1_r, _ = p1r.next()
                    for k in range(KC):
                        mm(kb, p1[:], W1[:, k, 0, j * 128:(j + 1) * 128], h[:, k, :], k == 0, k == KC - 1, [W1_r, h_r], [p1_r])
                    p2, p2_r, _ = p2r.next()
                    for k in range(KC):
                        mm(kb, p2[:], W1[:, k, 1, j * 128:(j + 1) * 128], h[:, k, :], k == 0, k == KC - 1, [W1_r, h_r], [p2_r])
                    si, si_r, _ = sir.next()
                    kb.op("act", lambda e: e.activation(out=si[:], in_=p1[:], func=AF.Silu), r=[p1_r], w=[si_r])
                    kb.op("dve", lambda e: e.tensor_tensor(out=aT[:, j, :], in0=p2[:], in1=si[:], op=ALU.mult), r=[p2_r, si_r], w=[aT_r])
                if last:
                    ho, ho_r, ho_ch = hor.next()
                for i in range(4):
                    t = 4 * c + i
                    xt, xt_r, x_ch = xr.next()
                    kb.dma("sp", xt[:], self.xres[t * 128:(t + 1) * 128, :], x_ch, w=[xt_r])
                    for hh in range(2):
                        px, px_r, _ = pxr.next()
                        for j in range(nj):
                            mm(kb, px[:], aT[:, j, i * 128:(i + 1) * 128], W2[:, j, hh * 512:(hh + 1) * 512], j == 0, j == nj - 1,
                               [aT_r, W2_r], [px_r])
                        kb.op("dve", lambda e: e.tensor_tensor(out=xt[:, hh * 512:(hh + 1) * 512], in0=px[:], in1=xt[:, hh * 512:(hh + 1) * 512],
                                                               op=ALU.add), r=[px_r, xt_r], w=[xt_r])
                    if final:
                        ot, ot_r, ot_ch = outr.next()
                        nm.to_out(xt[:], xt_r, ot[:], ot_r)
                        kb.dma("sp", self.out[t * 128:(t + 1) * 128, :], ot[:], ot_ch, r=[ot_r])
                    else:
                        kb.dma("sp", self.xres[t * 128:(t + 1) * 128, :], xt[:], x_ch, r=[xt_r])
                        if last:
                            nm.to_hT(xt[:], xt_r, ho[:, :, i * 128:(i + 1) * 128], ho_r)
                if last and not final:
                    kb.dma("sp", self.hT[:, :, c * 512:(c + 1) * 512].rearrange("k p s -> p k s"), ho[:], ho_ch, r=[ho_r])


IN_OFF = {}
_off = 0
for _n, _w in (("qa", 256), ("ka", 256), ("va", 256), ("fa", 4), ("qb", 256), ("kb", 64), ("vb", 64), ("qi", 256), ("ki", 32),
               ("wi", 8), ("qc", 256), ("kc", 256), ("vc", 256), ("qm", 256)):
    IN_OFF[_n] = (_off, _off + _w)
    _off += _w


def _cols(name):
    a, b = IN_OFF[name]
    return np.arange(a, b)


def _partner(cols, hd, half):
    c = cols.copy().reshape(-1, hd)
    p = c.copy()
    p[:, 0:half] = c[:, half:2 * half]
    p[:, half:2 * half] = c[:, 0:half]
    return p.reshape(-1)


def _host_consts():
    ident = np.eye(128, dtype=np.float32)
    k = np.arange(128)[:, None]
    q = np.arange(128)[None, :]
    tri = (k <= q).astype(np.float32)
    cneg = np.where(q > k, np.float32(NEG_S), np.float32(0.0)).astype(np.float32)
    cmat = np.concatenate([ident, tri, cneg], axis=1).astype(np.float32)
    p = np.arange(128)
    ccol = np.zeros((128, 4), np.float32)
    for pat, (hd, rot) in enumerate(((64, 16), (32, 8))):
        half = rot // 2
        inv = (np.float32(ROPE_THETA) ** (-(np.arange(0, rot, 2, dtype=np.float32) / np.float32(rot)))).astype(np.float32)
        d = p % hd
        ccol[:, pat] = np.where(d < rot, inv[d % half], 0.0)
        ccol[:, 2 + pat] = np.where(d < half, -1.0, np.where(d < rot, 1.0, 0.0))
    return cmat, ccol


def _prep_shared(inp):
    w_in = np.asarray(inp["w_in"], np.float32)
    L = w_in.shape[0]
    g = lambda cols: np.ascontiguousarray(w_in[:, :, cols])
    qb, kb_, qi, ki, qc, kc = _cols("qb"), _cols("kb"), _cols("qi"), _cols("ki"), _cols("qc"), _cols("kc")
    kb2 = np.concatenate([kb_, kb_])
    ki4 = np.concatenate([ki, ki, ki, ki])
    cmat, ccol = _host_consts()
    sh = {
        "cmat": cmat, "ccol": ccol,
        "g_mix": np.asarray(inp["norm_mix_g"], np.float32).reshape(L, 1, D),
        "g_ffn": np.asarray(inp["norm_ffn_g"], np.float32).reshape(L, 1, D),
        "g_mem": np.asarray(inp["norm_mem_g"], np.float32).reshape(L, 1, D),
        "g_fin": np.asarray(inp["final_norm_g"], np.float32).reshape(1, D),
        "wA_fm": g(np.concatenate([_cols("qa"), _cols("ka")])),
        "wA_tm": g(np.concatenate([_cols("va"), _cols("fa")])),
        "wB_fm": g(np.concatenate([qb, _partner(qb, 64, 8), kb2, _partner(kb2, 64, 8), qi, _partner(qi, 32, 4), ki4, _partner(ki4, 32, 4)])),
        "wB_tm": g(np.concatenate([_cols("vb"), _cols("wi")])),
        "wC_fm": g(np.concatenate([qc, _partner(qc, 32, 4), kc, _partner(kc, 32, 4)])),
        "wC_tm": g(_cols("vc")),
        "wM_fm": g(_cols("qm")),
        "w_kv": np.ascontiguousarray(np.asarray(inp["w_mem_kv"], np.float32)),
        "b_forget": np.asarray(inp["b_forget"], np.float32).reshape(L, 1, 4),
        "dlam": np.asarray(inp["diff_lambda"], np.float32).reshape(L, 1, 128),
        "subg": np.ascontiguousarray(np.tile(np.asarray(inp["diff_subln_g"], np.float32), (1, 2)).reshape(L, 128, 1)),
        "w_branch": np.ascontiguousarray(np.asarray(inp["w_branch"], np.float32)),
        "w_gate": np.ascontiguousarray(np.asarray(inp["w_gate"], np.float32)),
        "b_gate": np.ascontiguousarray(np.asarray(inp["b_gate"], np.float32).reshape(L, 32, 128).transpose(0, 2, 1)),
        "w_out": np.ascontiguousarray(np.asarray(inp["w_out"], np.float32)),
        "w_f1": np.ascontiguousarray(np.asarray(inp["w_ffn_in"], np.float32)),
        "w_f2": np.ascontiguousarray(np.asarray(inp["w_ffn_out"], np.float32)),
    }
    return sh


_PROG_CACHE = {}


def run(inp, S, depth, n_cores, branches=(0, 1, 2, 3), trace=False):
    key = (S, depth, tuple(branches))
    if key not in _PROG_CACHE:
        _PROG_CACHE[key] = Prog(S, depth, branches)
    prog = _PROG_CACHE[key]
    sh = _prep_shared(inp)
    x = np.asarray(inp["x"], np.float32)
    mem = np.asarray(inp["mem"], np.float32)
    pos = np.asarray(inp["positions"], np.int32)
    in_maps = []
    for b in range(n_cores):
        m = dict(sh)
        m["x"] = np.ascontiguousarray(x[b])
        m["mem"] = np.ascontiguousarray(mem[b])
        m["pos"] = np.ascontiguousarray(pos[b].reshape(1, S))
        in_maps.append(m)
    res = run_bass_kernel_spmd(prog.nc, in_maps, core_ids=list(range(n_cores)), trace=trace)
    out = np.stack([np.asarray(r["out"], np.float32) for r in res.results], axis=0)
    return out, res


def kernel(**inputs):
    out, _ = run(inputs, 4096, 4, 8)
    return out
```

```python
import math
from contextlib import ExitStack, contextmanager
import numpy as np
import concourse.bass as bass
import concourse.mybir as mybir
from concourse.bass_utils import run_bass_kernel_spmd

F32 = mybir.dt.float32
BF16 = mybir.dt.bfloat16
I32 = mybir.dt.int32
ALU = mybir.AluOpType
AF = mybir.ActivationFunctionType

D = 1024
KC = 8
MEM = 256
FFH = 2816
EPS = 1e-6
NEG_S = -1.0e4
NEG_B = -30000.0
TOPK = 256
NBIS = 14
import os
STOP = int(os.environ.get('KSTOP', '0'))
ROPE_THETA = 500000.0
TWO_PI = 2.0 * math.pi
CW_HI = 6.28125
CW_LO = TWO_PI - CW_HI


class Res:
    __slots__ = ("w", "rs")

    def __init__(self):
        self.w = None
        self.rs = {}


class KB:
    def __init__(self):
        self.nc = bass.Bass("TRN2", target_bir_lowering=False)
        nc = self.nc
        self.ges = ExitStack()
        self.E = {"pe": nc.tensor, "act": nc.scalar, "dve": nc.vector, "pool": nc.gpsimd, "sp": nc.sync}
        self.sems = {}
        self.cnt = {}
        self.cur = {}
        self.known = {n: {} for n in self.E}
        self.epoch = -1
        self.nid = 0
        self.new_epoch()

    def new_epoch(self):
        self.epoch += 1
        for n in ("pe", "act", "dve", "pool"):
            nm = "%s#%d" % (n, self.epoch)
            self.sems[nm] = self.ges.enter_context(self.nc.semaphore("e_%s_%d" % (n, self.epoch)))
            self.cnt[nm] = 0
            self.cur[n] = nm

    def chan(self, name):
        nm = "c_" + name
        if nm not in self.sems:
            self.sems[nm] = self.ges.enter_context(self.nc.semaphore(nm))
            self.cnt[nm] = 0
        return nm

    def name(self, p):
        self.nid += 1
        return "%s_%d" % (p, self.nid)

    def _waits(self, eng, r, w):
        deps = {}
        for x in r:
            if x.w is not None and deps.get(x.w[0], 0) < x.w[1]:
                deps[x.w[0]] = x.w[1]
        for x in w:
            if x.w is not None and deps.get(x.w[0], 0) < x.w[1]:
                deps[x.w[0]] = x.w[1]
            for s, v in x.rs.items():
                if deps.get(s, 0) < v:
                    deps[s] = v
        kn = self.known[eng]
        for s, v in deps.items():
            if eng == "pe" and s.startswith("pe#"):
                continue
            if kn.get(s, 0) >= v:
                continue
            self.E[eng].wait_ge(self.sems[s], v)
            kn[s] = v

    def _post(self, tok, r, w):
        s, v = tok
        for x in r:
            if x.rs.get(s, 0) < v:
                x.rs[s] = v
        for x in w:
            x.w = tok
            x.rs = {}

    def op(self, eng, fn, r=(), w=()):
        self._waits(eng, r, w)
        ins = fn(self.E[eng])
        s = self.cur[eng]
        self.cnt[s] += 1
        ins.then_inc(self.sems[s], 1)
        self._post((s, self.cnt[s]), r, w)

    def dma(self, q, out, in_, chan, r=(), w=()):
        self._waits(q, r, w)
        ins = self.E[q].dma_start(out=out, in_=in_)
        c = self.chan(chan)
        self.cnt[c] += 16
        ins.then_inc(self.sems[c], 16)
        self._post((c, self.cnt[c]), r, w)

    def barrier(self):
        for e in self.E:
            kn = self.known[e]
            for s, v in self.cnt.items():
                if v == 0 or kn.get(s, 0) >= v:
                    continue
                if e == "pe" and s.startswith("pe#"):
                    continue
                self.E[e].wait_ge(self.sems[s], v)
                kn[s] = v

    @contextmanager
    def phase(self):
        es = ExitStack()
        with es:
            yield Phase(self, es)
            self.barrier()


class Phase:
    def __init__(self, kb, es):
        self.kb = kb
        self.es = es

    def sb(self, shape, dt, nm="t"):
        return self.es.enter_context(self.kb.nc.sbuf_tensor(self.kb.name(nm), list(shape), dt))

    def ps(self, shape, dt, nm="p"):
        return self.es.enter_context(self.kb.nc.psum_tensor(self.kb.name(nm), list(shape), dt))

    def ring(self, n, shape, dt, nm="r", psum=False):
        items = []
        for i in range(n):
            t = self.ps(shape, dt, nm) if psum else self.sb(shape, dt, nm)
            items.append((t, Res(), "%s%d" % (nm, i)))
        return Ring(items)


class Ring:
    def __init__(self, items):
        self.items = items
        self.i = -1

    def next(self):
        self.i = (self.i + 1) % len(self.items)
        return self.items[self.i]


def mm(kb, out, lhsT, rhs, start, stop, r, w, tp=None):
    if tp is None:
        kb.op("pe", lambda e: e.matmul(out, lhsT=lhsT, rhs=rhs, start=start, stop=stop), r=r, w=w)
    else:
        kb.op("pe", lambda e: e.matmul(out, lhsT=lhsT, rhs=rhs, start=start, stop=stop, tile_position=tp), r=r, w=w)


def pbase_tp(pb):
    return (96, 0) if pb == 96 else None


class Prog:
    def __init__(self, S, depth, branches=(0, 1, 2, 3), debug=False):
        self.S = S
        self.depth = depth
        self.branches = tuple(branches)
        self.NT = S // 128
        self.NCH = S // 512
        self.kb = KB()
        self.nc = self.kb.nc
        self.debug = debug
        self.build()

    def dram_in(self, name, shape, dt=F32):
        return self.nc.dram_tensor(name, list(shape), dt, kind="ExternalInput").ap()

    def build(self):
        kb, nc, S, L = self.kb, self.nc, self.S, self.depth
        di = self.dram_in
        self.x_in = di("x", [S, D])
        self.mem_in = di("mem", [MEM, D])
        self.pos_in = di("pos", [1, S], I32)
        self.cmat_in = di("cmat", [128, 3 * 128])
        self.ccol_in = di("ccol", [128, 4])
        self.g_mix = di("g_mix", [L, 1, D])
        self.g_ffn = di("g_ffn", [L, 1, D])
        self.g_mem = di("g_mem", [L, 1, D])
        self.g_fin = di("g_fin", [1, D])
        self.wA_fm = di("wA_fm", [L, D, 512])
        self.wA_tm = di("wA_tm", [L, D, 260])
        self.wB_fm = di("wB_fm", [L, D, 1536])
        self.wB_tm = di("wB_tm", [L, D, 72])
        self.wC_fm = di("wC_fm", [L, D, 1024])
        self.wC_tm = di("wC_tm", [L, D, 256])
        self.wM_fm = di("wM_fm", [L, D, 256])
        self.w_kv = di("w_kv", [L, D, 512])
        self.b_forget = di("b_forget", [L, 1, 4])
        self.dlam = di("dlam", [L, 1, 128])
        self.subg = di("subg", [L, 128, 1])
        self.w_branch = di("w_branch", [L, 4, 256, D])
        self.w_gate = di("w_gate", [L, D, 4096])
        self.b_gate = di("b_gate", [L, 128, 32])
        self.w_out = di("w_out", [L, D, D])
        self.w_f1 = di("w_f1", [L, D, 2 * FFH])
        self.w_f2 = di("w_f2", [L, FFH, D])
        self.out = nc.dram_tensor("out", [S, D], F32, kind="ExternalOutput").ap()
        it = lambda name, shape, dt: nc.dram_tensor(name, list(shape), dt, kind="Internal").ap()
        self.xres = it("xres", [S, D], F32)
        self.hT = it("hT", [KC, 128, S], BF16)
        self.h2T = it("h2T", [KC, 128, S], BF16)
        self.oT = it("oT", [4, 256, S], BF16)
        self.rope = it("rope", [4, 128, S], F32)

        gsb = lambda shape, dt, nm: kb.ges.enter_context(nc.sbuf_tensor(kb.name(nm), list(shape), dt))
        self.cm = gsb([128, 3, 128], F32, "cm")
        self.cm_r = Res()
        self.ccol = gsb([128, 4], F32, "ccol")
        self.identb = gsb([128, 128], BF16, "identb")
        self.trib = gsb([128, 128], BF16, "trib")
        self.cnegb = gsb([128, 128], BF16, "cnegb")
        self.onesf = gsb([128, 128], F32, "onesf")
        self.onesb = gsb([128, 128], BF16, "onesb")
        self.cst_r = Res()
        kb.dma("sp", self.cm[:].rearrange("p a b -> p (a b)"), self.cmat_in, "cst", w=[self.cm_r])
        kb.dma("sp", self.ccol[:], self.ccol_in, "cst", w=[self.cm_r])
        kb.op("dve", lambda e: e.tensor_copy(out=self.identb[:], in_=self.cm[:, 0, :]), r=[self.cm_r], w=[self.cst_r])
        kb.op("dve", lambda e: e.tensor_copy(out=self.trib[:], in_=self.cm[:, 1, :]), r=[self.cm_r], w=[self.cst_r])
        kb.op("dve", lambda e: e.tensor_scalar(out=self.cnegb[:], in0=self.cm[:, 2, :], scalar1=NEG_B / NEG_S,
                                               scalar2=None, op0=ALU.mult), r=[self.cm_r], w=[self.cst_r])
        kb.op("pool", lambda e: e.memset(self.onesf[:], 1.0), w=[self.cst_r])
        kb.op("pool", lambda e: e.memset(self.onesb[:], 1.0), w=[self.cst_r])
        self.identf = self.cm[:, 0, :]
        self.trif = self.cm[:, 1, :]
        self.cnegf = self.cm[:, 2, :]

        self.phase_rope()
        self.phase_norm0()
        for l in range(L):
            if 0 in self.branches:
                self.phase_fox(l)
            if 1 in self.branches:
                self.phase_dsa(l)
            if 2 in self.branches:
                self.phase_diff(l)
            if 3 in self.branches:
                self.phase_mem(l)
            self.phase_merge(l)
            self.phase_ffn(l, 0, 11, False)
            self.phase_ffn(l, 11, 22, True)
            kb.new_epoch()
        kb.barrier()

    def load_w(self, dst, dst_r, src, nk, chan, q="pool"):
        for k in range(nk):
            self.kb.dma(q, dst[:, k, :], src[k * 128:(k + 1) * 128, :], chan, w=[dst_r])

    def load_bc(self, dst, dst_r, src_row, n, chan):
        self.kb.dma("sp", dst, src_row.to_broadcast([128, n]), chan, w=[dst_r])

    def phase_rope(self):
        kb, S = self.kb, self.S
        with kb.phase() as ph:
            posi = ph.sb([128, S], I32)
            posf = ph.sb([128, S], F32)
            A = ph.sb([128, S], F32)
            B = ph.sb([128, S], F32)
            C = ph.sb([128, S], I32)
            Dd = ph.sb([128, S], F32)
            r_pos, r_A, r_B, r_C, r_D = Res(), Res(), Res(), Res(), Res()
            kb.dma("sp", posi[:], self.pos_in.to_broadcast([128, S]), "rp", w=[r_pos])
            kb.op("dve", lambda e: e.tensor_copy(out=posf[:], in_=posi[:]), r=[r_pos], w=[r_pos])
            for pat in range(2):
                invf = self.ccol[:, pat:pat + 1]
                sgn = self.ccol[:, 2 + pat:3 + pat]
                kb.op("dve", lambda e: e.tensor_scalar(out=A[:], in0=posf[:], scalar1=invf, scalar2=None, op0=ALU.mult),
                      r=[r_pos, self.cm_r], w=[r_A])
                for which in range(2):
                    off = math.pi / 2 if which == 0 else 0.0
                    kb.op("dve", lambda e: e.tensor_scalar(out=B[:], in0=A[:], scalar1=off, scalar2=None, op0=ALU.add),
                          r=[r_A], w=[r_B])
                    kb.op("dve", lambda e: e.tensor_scalar(out=Dd[:], in0=B[:], scalar1=1.0 / TWO_PI, scalar2=None,
                                                           op0=ALU.mult), r=[r_B], w=[r_D])
                    kb.op("dve", lambda e: e.tensor_copy(out=C[:], in_=Dd[:]), r=[r_D], w=[r_C])
                    kb.op("dve", lambda e: e.tensor_copy(out=Dd[:], in_=C[:]), r=[r_C], w=[r_D])
                    kb.op("dve", lambda e: e.scalar_tensor_tensor(out=B[:], in0=Dd[:], scalar=-CW_HI, in1=B[:],
                                                                  op0=ALU.mult, op1=ALU.add), r=[r_D, r_B], w=[r_B])
                    kb.op("dve", lambda e: e.scalar_tensor_tensor(out=B[:], in0=Dd[:], scalar=-CW_LO, in1=B[:],
                                                                  op0=ALU.mult, op1=ALU.add), r=[r_D, r_B], w=[r_B])
                    kb.op("dve", lambda e: e.tensor_scalar(out=Dd[:], in0=B[:], scalar1=math.pi, scalar2=-TWO_PI,
                                                           op0=ALU.is_gt, op1=ALU.mult), r=[r_B], w=[r_D])
                    kb.op("dve", lambda e: e.tensor_tensor(out=B[:], in0=B[:], in1=Dd[:], op=ALU.add), r=[r_B, r_D], w=[r_B])
                    kb.op("dve", lambda e: e.tensor_scalar(out=B[:], in0=B[:], scalar1=-math.pi, scalar2=math.pi,
                                                           op0=ALU.max, op1=ALU.min), r=[r_B], w=[r_B])
                    kb.op("act", lambda e: e.activation(out=Dd[:], in_=B[:], func=AF.Sin), r=[r_B], w=[r_D])
                    if which == 1:
                        kb.op("dve", lambda e: e.tensor_scalar(out=Dd[:], in0=Dd[:], scalar1=sgn, scalar2=None,
                                                               op0=ALU.mult), r=[r_D, self.cm_r], w=[r_D])
                    kb.dma("sp", self.rope[2 * pat + which], Dd[:], "rp", r=[r_D])

    class Normer:
        def __init__(self, prog, ph, g_row, chan):
            kb = prog.kb
            self.prog, self.ph = prog, ph
            self.gbc = ph.sb([128, D], F32, "gbc")
            self.gbc_r = Res()
            prog.load_bc(self.gbc[:], self.gbc_r, g_row, D, chan)
            self.junk = ph.ring(1, [128, D], BF16, "njunk")
            self.st = ph.ring(3, [128, 4], F32, "nst")
            self.hb = ph.ring(2, [128, D], BF16, "nhb")
            self.pT = ph.ring(1, [128, KC, 128], BF16, "npT", psum=True)

        def stats(self, xt, xt_r):
            kb = self.prog.kb
            junk, junk_r, _ = self.junk.next()
            st, st_r, _ = self.st.next()
            kb.op("act", lambda e: e.activation(out=junk[:], in_=xt, func=AF.Square, accum_out=st[:, 0:1]),
                  r=[xt_r], w=[junk_r, st_r])
            kb.op("dve", lambda e: e.tensor_scalar(out=st[:, 1:2], in0=st[:, 0:1], scalar1=1.0 / D, scalar2=EPS,
                                                   op0=ALU.mult, op1=ALU.add), r=[st_r], w=[st_r])
            kb.op("act", lambda e: e.activation(out=st[:, 2:3], in_=st[:, 1:2], func=AF.Ln), r=[st_r], w=[st_r])
            kb.op("act", lambda e: e.activation(out=st[:, 3:4], in_=st[:, 2:3], func=AF.Exp, scale=-0.5), r=[st_r], w=[st_r])
            return st[:, 3:4], st_r

        def to_hT(self, xt, xt_r, dst, dst_r):
            kb, prog = self.prog.kb, self.prog
            rstd, st_r = self.stats(xt, xt_r)
            hb, hb_r, _ = self.hb.next()
            kb.op("dve", lambda e: e.scalar_tensor_tensor(out=hb[:], in0=xt, scalar=rstd, in1=self.gbc[:],
                                                          op0=ALU.mult, op1=ALU.mult), r=[xt_r, st_r, self.gbc_r], w=[hb_r])
            pT, pT_r, _ = self.pT.next()
            for k in range(KC):
                kb.op("pe", lambda e: e.transpose(pT[:, k, :], hb[:, k * 128:(k + 1) * 128], prog.identb[:]),
                      r=[hb_r, prog.cst_r], w=[pT_r])
            kb.op("act", lambda e: e.activation(out=dst, in_=pT[:], func=AF.Copy), r=[pT_r], w=[dst_r])

        def to_out(self, xt, xt_r, dst, dst_r):
            kb = self.prog.kb
            rstd, st_r = self.stats(xt, xt_r)
            kb.op("dve", lambda e: e.scalar_tensor_tensor(out=dst, in0=xt, scalar=rstd, in1=self.gbc[:],
                                                          op0=ALU.mult, op1=ALU.mult), r=[xt_r, st_r, self.gbc_r], w=[dst_r])

    def phase_norm0(self):
        kb = self.kb
        with kb.phase() as ph:
            nm = Prog.Normer(self, ph, self.g_mix[0], "n0g")
            xr = ph.ring(3, [128, D], F32, "n0x")
            hc = ph.ring(2, [128, KC, 512], BF16, "n0h")
            for c in range(self.NCH):
                h, h_r, h_ch = hc.next()
                for i in range(4):
                    t = 4 * c + i
                    xt, xt_r, x_ch = xr.next()
                    kb.dma("sp", xt[:], self.x_in[t * 128:(t + 1) * 128, :], x_ch, w=[xt_r])
                    nm.to_hT(xt[:], xt_r, h[:, :, i * 128:(i + 1) * 128], h_r)
                kb.dma("sp", self.hT[:, :, c * 512:(c + 1) * 512].rearrange("k p s -> p k s"), h[:], h_ch, r=[h_r])

    def attn_finalize(self, po, po_r, rd_ring, dst, dst_r, eng_mul="dve"):
        kb = self.kb
        rd, rd_r, _ = rd_ring.next()
        kb.op("dve", lambda e: e.reciprocal(out=rd[64:128, :], in_=po[64:128, :]), r=[po_r], w=[rd_r])
        kb.op("dve", lambda e: e.tensor_tensor(out=dst, in0=po[0:64, :], in1=rd[64:128, :], op=ALU.mult),
              r=[po_r, rd_r], w=[dst_r])

    def causal_attn(self, *args, **kw):
        for _ in self.causal_attn_gen(*args, **kw):
            pass

    def causal_attn_gen(self, c, kT_ap, kT_r, qT_ap, qT_r, V_ap, V_r, ps_ring, pt_ring, po, po_r, scale,
                        bias_fn=None, bias_r=(), mask_fn=None, tp=None):
        kb = self.kb
        J = 4 * c + 4
        sts = {}

        def emit_S(j):
            i = j - 4 * c
            col0 = max(i, 0) * 128
            ps, ps_r, _ = ps_ring.next()
            extra = mask_fn(j, col0) if mask_fn is not None else []
            mm(kb, ps[:, col0:512], kT_ap(j), qT_ap(col0), True, len(extra) == 0, [kT_r(j), qT_r], [ps_r], tp)
            for n, (lt, rh, c0, c1, rr) in enumerate(extra):
                mm(kb, ps[:, c0:c1], lt, rh, False, n == len(extra) - 1, rr, [ps_r])
            sts[j] = (ps, ps_r, col0, i)

        emit_S(0)
        for j in range(J):
            if j + 1 < J:
                emit_S(j + 1)
            ps, ps_r, col0, i = sts.pop(j)
            pt, pt_r, _ = pt_ring.next()
            if bias_fn is not None:
                b = bias_fn(j)
                kb.op("act", lambda e: e.activation(out=pt[:, col0:512], in_=ps[:, col0:512], func=AF.Exp, scale=scale, bias=b),
                      r=[ps_r] + list(bias_r), w=[pt_r])
            else:
                kb.op("act", lambda e: e.activation(out=pt[:, col0:512], in_=ps[:, col0:512], func=AF.Exp, scale=scale),
                      r=[ps_r], w=[pt_r])
            if i >= 0:
                kb.op("pool", lambda e: e.tensor_tensor(out=pt[:, col0:col0 + 128], in0=pt[:, col0:col0 + 128],
                                                        in1=self.trib[:], op=ALU.mult), r=[pt_r, self.cst_r], w=[pt_r])
            mm(kb, po[:, col0:512], V_ap(j), pt[:, col0:512], j == 0, j == J - 1, [V_r(j), pt_r], [po_r])
            yield

    def store_oT(self, n, c, oTs, oTs_r, chan):
        self.kb.dma("sp", self.oT[n].rearrange("(h p) s -> p h s", p=64)[:, :, c * 512:(c + 1) * 512], oTs[0:64, :, :],
                    chan, r=[oTs_r])

    def proj_fm(self, ps, ps_r, W, W_r, col, h, h_r, n=512):
        for k in range(KC):
            mm(self.kb, ps[:, 0:n], W[:, k, col:col + 128], h[:, k, 0:n], k == 0, k == KC - 1, [W_r, h_r], [ps_r])

    def proj_tm(self, ps, ps_r, W, W_r, col, ncol, h, h_r, i):
        for k in range(KC):
            mm(self.kb, ps[:, 0:ncol], h[:, k, i * 128:(i + 1) * 128], W[:, k, col:col + ncol], k == 0, k == KC - 1,
               [W_r, h_r], [ps_r])

    def load_hT(self, src, ring, c):
        h, h_r, h_ch = ring.next()
        self.kb.dma("sp", h[:], src[:, :, c * 512:(c + 1) * 512].rearrange("k p s -> p k s"), h_ch, w=[h_r])
        return h, h_r

    def rope_evac(self, q_ps, q_r, p_ps, p_r, Ct, St, tab_r, tmp_ring, dst, dst_r):
        kb = self.kb
        t1, t1_r, _ = tmp_ring.next()
        t2, t2_r, _ = tmp_ring.next()
        kb.op("dve", lambda e: e.tensor_tensor(out=t1[:], in0=p_ps, in1=St, op=ALU.mult), r=[p_r, tab_r], w=[t1_r])
        kb.op("dve", lambda e: e.tensor_tensor(out=t2[:], in0=q_ps, in1=Ct, op=ALU.mult), r=[q_r, tab_r], w=[t2_r])
        kb.op("pool", lambda e: e.tensor_tensor(out=dst, in0=t1[:], in1=t2[:], op=ALU.add), r=[t1_r, t2_r], w=[dst_r])

    def phase_fox(self, l):
        kb, S, NT, NCH = self.kb, self.S, self.NT, self.NCH
        with kb.phase() as ph:
            W = ph.sb([128, KC, 772], BF16, "fW")
            W_r = Res()
            self.load_w(W[:, :, 0:512], W_r, self.wA_fm[l], KC, "fW")
            self.load_w(W[:, :, 512:772], W_r, self.wA_tm[l], KC, "fW")
            kT = ph.sb([128, 2, S], BF16, "fkT")
            kT_rs = [Res() for _ in range(NCH)]
            V1 = ph.sb([128, NT, 4, 128], BF16, "fV1")
            V_rs = [Res() for _ in range(NT)]
            ones_r = Res()
            kb.op("pool", lambda e: e.memset(V1[:, :, :, 64:128], 1.0), w=[ones_r])
            for r_ in V_rs:
                r_.w = ones_r.w
            cumn = ph.sb([128, NT, 4], F32, "fcum")
            carry = ph.sb([128, NT + 1, 4], F32, "fcar")
            cum_r = Res()
            kb.op("pool", lambda e: e.memset(carry[:, 0, :], 0.0), w=[cum_r])
            bfg = ph.sb([128, 4], F32, "fbf")
            bfg_r = Res()
            self.load_bc(bfg[:], bfg_r, self.b_forget[l], 4, "fbf")
            hring = ph.ring(2, [128, KC, 512], BF16, "fh")
            qring = ph.ring(2, [128, 2, 512], BF16, "fq")
            pp = ph.ring(2, [128, 512], F32, "fpp", psum=True)
            psr = ph.ring(3, [128, 512], F32, "fps", psum=True)
            por = ph.ring(2, [128, 512], F32, "fpo", psum=True)
            pcs = ph.ring(1, [128, 8], F32, "fpc", psum=True)
            ptr = ph.ring(3, [128, 512], BF16, "fpt")
            rdr = ph.ring(2, [128, 512], F32, "frd")
            otr = ph.ring(2, [128, 4, 512], BF16, "fot")
            fsm = ph.ring(2, [128, 12], F32, "fsm")
            fhl = ph.ring(2, [128, 8], BF16, "fhl")
            bias = ph.ring(2, [128, NT, 4], F32, "fbi")
            for c in range(NCH):
                h, h_r = self.load_hT(self.hT, hring, c)
                qT, qT_r, _ = qring.next()
                for ci in range(2):
                    ps, ps_r, _ = pp.next()
                    self.proj_fm(ps, ps_r, W, W_r, ci * 128, h, h_r)
                    kb.op("act", lambda e: e.activation(out=qT[:, ci, :], in_=ps[:], func=AF.Copy), r=[ps_r], w=[qT_r])
                for ci in range(2):
                    ps, ps_r, _ = pp.next()
                    self.proj_fm(ps, ps_r, W, W_r, 256 + ci * 128, h, h_r)
                    kb.op("dve", lambda e: e.tensor_copy(out=kT[:, ci, c * 512:(c + 1) * 512], in_=ps[:]), r=[ps_r], w=[kT_rs[c]])
                for i in range(4):
                    t = 4 * c + i
                    ps, ps_r, _ = pp.next()
                    self.proj_tm(ps, ps_r, W, W_r, 512, 260, h, h_r, i)
                    kb.op("act", lambda e: e.activation(out=V1[:, t, :, 0:64], in_=ps[:, 0:256].rearrange("p (h d) -> p h d", h=4),
                                                        func=AF.Copy), r=[ps_r], w=[V_rs[t]])
                    if STOP == 1:
                        continue
                    sm, sm_r, _ = fsm.next()
                    kb.op("dve", lambda e: e.tensor_tensor(out=sm[:, 0:4], in0=ps[:, 256:260], in1=bfg[:], op=ALU.add),
                          r=[ps_r, bfg_r], w=[sm_r])
                    kb.op("act", lambda e: e.activation(out=sm[:, 4:8], in_=sm[:, 0:4], func=AF.Exp, scale=-1.0), r=[sm_r], w=[sm_r])
                    kb.op("act", lambda e: e.activation(out=sm[:, 8:12], in_=sm[:, 4:8], func=AF.Ln, bias=1.0), r=[sm_r], w=[sm_r])
                    hl, hl_r, _ = fhl.next()
                    kb.op("dve", lambda e: e.tensor_copy(out=hl[:, 0:4], in_=sm[:, 8:12]), r=[sm_r], w=[hl_r])
                    kb.op("dve", lambda e: e.tensor_copy(out=sm[:, 4:8], in_=hl[:, 0:4]), r=[hl_r], w=[sm_r])
                    kb.op("dve", lambda e: e.tensor_tensor(out=hl[:, 4:8], in0=sm[:, 8:12], in1=sm[:, 4:8], op=ALU.subtract),
                          r=[sm_r], w=[hl_r])
                    pc, pc_r, _ = pcs.next()
                    mm(kb, pc[:, 0:4], self.trib[:], hl[:, 0:4], True, False, [hl_r, self.cst_r], [pc_r])
                    mm(kb, pc[:, 0:4], self.trib[:], hl[:, 4:8], False, True, [hl_r, self.cst_r], [pc_r])
                    mm(kb, pc[:, 4:8], self.onesb[:], hl[:, 0:4], True, False, [hl_r, self.cst_r], [pc_r])
                    mm(kb, pc[:, 4:8], self.onesb[:], hl[:, 4:8], False, True, [hl_r, self.cst_r], [pc_r])
                    kb.op("dve", lambda e: e.tensor_tensor(out=cumn[:, t, :], in0=pc[:, 0:4], in1=carry[:, t, :], op=ALU.add),
                          r=[pc_r, cum_r], w=[cum_r])
                    kb.op("dve", lambda e: e.tensor_tensor(out=carry[:, t + 1, :], in0=pc[:, 4:8], in1=carry[:, t, :], op=ALU.add),
                          r=[pc_r, cum_r], w=[cum_r])
                if STOP in (1, 2):
                    continue
                J = 4 * c + 4
                bi, bi_r, _ = bias.next()
                kb.op("dve", lambda e: e.tensor_tensor(out=bi[:, 0:J, :], in0=cumn[:, 0:J, :],
                                                       in1=carry[:, 4 * c + 2:4 * c + 3, :].to_broadcast([128, J, 4]),
                                                       op=ALU.subtract), r=[cum_r], w=[bi_r])
                if STOP == 3:
                    continue
                oTs, oTs_r, o_ch = otr.next()
                for hd in range(4):
                    cb, pb = hd // 2, (hd % 2) * 64
                    po, po_r, _ = por.next()
                    self.causal_attn(
                        c,
                        lambda j: kT[pb:pb + 64, cb, j * 128:(j + 1) * 128], lambda j: kT_rs[j // 4],
                        lambda col0: qT[pb:pb + 64, cb, col0:512], qT_r,
                        lambda j: V1[:, j, hd, :], lambda j: V_rs[j],
                        psr, ptr, po, po_r, 0.125,
                        bias_fn=lambda j: bi[:, j, hd:hd + 1], bias_r=[bi_r])
                    if STOP == 4:
                        continue
                    self.attn_finalize(po, po_r, rdr, oTs[0:64, hd, :], oTs_r)
                if STOP in (4, 5):
                    continue
                self.store_oT(0, c, oTs, oTs_r, o_ch)

    def phase_dsa(self, l):
        kb, S, NT, NCH = self.kb, self.S, self.NT, self.NCH
        with kb.phase() as ph:
            W = ph.sb([128, KC, 1608], BF16, "bW")
            W_r = Res()
            self.load_w(W[:, :, 0:1536], W_r, self.wB_fm[l], KC, "bW")
            self.load_w(W[:, :, 1536:1608], W_r, self.wB_tm[l], KC, "bW")
            kbT = ph.sb([128, S], BF16, "bkb")
            kiT = ph.sb([128, S], BF16, "bki")
            k_rs = [Res() for _ in range(NCH)]
            V1 = ph.sb([128, NT, 128], BF16, "bV1")
            V_rs = [Res() for _ in range(NT)]
            ones_r = Res()
            kb.op("pool", lambda e: e.memset(V1[:, :, 64:128], 1.0), w=[ones_r])
            for r_ in V_rs:
                r_.w = ones_r.w
            hring = ph.ring(1, [128, KC, 512], BF16, "bh")
            tabr = ph.ring(1, [128, 4, 512], F32, "btab")
            qbr = ph.ring(2, [128, 2, 512], BF16, "bqb")
            qir = ph.ring(2, [128, 2, 512], BF16, "bqi")
            tmpr = ph.ring(2, [128, 512], F32, "btmp")
            wir = ph.ring(2, [128, 4, 8], F32, "bwi")
            dgr = ph.ring(2, [128, 8, 128], BF16, "bdg")
            Rr = ph.ring(8, [128, 512], BF16, "bR")
            scs = [ph.sb([128, S], F32, "bsc") for _ in range(2)]
            sc_rs = [Res(), Res()]
            mbs = [[ph.sb([128, S], BF16, "bmb") for _ in range(4)] for _ in range(2)]
            mb_rs = [[Res() for _ in range(4)] for _ in range(2)]
            bst = ph.ring(4, [128, 8 + 2 * (NBIS + 1) + 2], F32, "bst")
            pp = ph.ring(2, [128, 512], F32, "bpp", psum=True)
            psr = ph.ring(3, [128, 512], F32, "bps", psum=True)
            por = ph.ring(2, [128, 512], F32, "bpo", psum=True)
            pscr = ph.ring(1, [128, 512], F32, "bpsc", psum=True)
            ptr = ph.ring(3, [128, 512], BF16, "bpt")
            rdr = ph.ring(1, [128, 512], F32, "brd")
            otr = ph.ring(2, [128, 4, 512], BF16, "bot")
            W0 = 8
            M0 = W0 + NBIS + 1
            wconst = ph.sb([128, 2, NBIS + 1], F32, "bwc")
            wc_r = Res()
            for k in range(NBIS + 1):
                cval = (1.0 + 2.0 ** -10) * 2.0 ** -(k + 1)
                kb.op("pool", lambda e: e.memset(wconst[:, 0, k:k + 1], cval), w=[wc_r])
                kb.op("pool", lambda e: e.memset(wconst[:, 1, k:k + 1], -cval), w=[wc_r])

            def proj(c):
                h, h_r = self.load_hT(self.hT, hring, c)
                tab, tab_r, t_ch = tabr.next()
                kb.dma("sp", tab[:], self.rope[:, :, c * 512:(c + 1) * 512].rearrange("t p s -> p t s"), t_ch, w=[tab_r])
                qb, qb_r, _ = qbr.next()
                qi, qi_r, _ = qir.next()

                def roped2(col, colp, pat, dst, dst_r):
                    ps, ps_r, _ = pp.next()
                    self.proj_fm(ps, ps_r, W, W_r, col, h, h_r)
                    p2, p2_r, _ = pp.next()
                    self.proj_fm(p2, p2_r, W, W_r, colp, h, h_r)
                    self.rope_evac(ps[:], ps_r, p2[:], p2_r, tab[:, 2 * pat, :], tab[:, 2 * pat + 1, :], tab_r, tmpr, dst, dst_r)

                for ci in range(2):
                    roped2(ci * 128, 256 + ci * 128, 0, qb[:, ci, :], qb_r)
                roped2(512, 640, 0, kbT[:, c * 512:(c + 1) * 512], k_rs[c])
                for ci in range(2):
                    roped2(768 + ci * 128, 1024 + ci * 128, 1, qi[:, ci, :], qi_r)
                roped2(1280, 1408, 1, kiT[:, c * 512:(c + 1) * 512], k_rs[c])
                wi, wi_r, _ = wir.next()
                for i in range(4):
                    t = 4 * c + i
                    ps, ps_r, _ = pp.next()
                    self.proj_tm(ps, ps_r, W, W_r, 1536, 72, h, h_r, i)
                    kb.op("act", lambda e: e.activation(out=V1[:, t, 0:64], in_=ps[:, 0:64], func=AF.Copy), r=[ps_r], w=[V_rs[t]])
                    kb.op("act", lambda e: e.activation(out=wi[:, i, :], in_=ps[:, 64:72], func=AF.Copy,
                                                        scale=(8.0 ** -0.5) * (32.0 ** -0.5)), r=[ps_r], w=[wi_r])
                jD = tab[:].rearrange("p a b -> p (a b)").bitcast(BF16)
                jA = h[:].rearrange("p k s -> p (k s)")
                return dict(qb=qb, qb_r=qb_r, qi=qi, qi_r=qi_r, wi=wi, wi_r=wi_r, jD=jD, jD_r=tab_r, jA=jA, jA_r=h_r)

            def trivial(c, i):
                qt = 4 * c + i
                N = (qt + 1) * 128
                mb, mb_r = mbs[c % 2][i], mb_rs[c % 2][i]
                if qt == 1:
                    kb.op("pool", lambda e: e.memset(mb[:, 0:128], 0.0), w=[mb_r])
                kb.op("pool", lambda e: e.tensor_copy(out=mb[:, N - 128:N], in_=self.cnegb[:]), r=[self.cst_r], w=[mb_r])

            def scores(c, i, P, sc, sc_r):
                qt = 4 * c + i
                N = (qt + 1) * 128
                qi, qi_r, wi, wi_r = P["qi"], P["qi_r"], P["wi"], P["wi_r"]
                dg, dg_r, _ = dgr.next()
                for hh in range(8):
                    kb.op("dve", lambda e: e.tensor_scalar(out=dg[:, hh, :], in0=self.identf, scalar1=wi[:, i, hh:hh + 1],
                                                           scalar2=None, op0=ALU.mult), r=[wi_r, self.cm_r], w=[dg_r])
                nkc = (N + 511) // 512
                for kc_ in range(nkc):
                    k0 = kc_ * 512
                    kn = min(512, N - k0)
                    Rs = []
                    for hh in range(8):
                        pbh = (hh % 4) * 32
                        ps, ps_r, _ = pp.next()
                        mm(kb, ps[:, 0:kn], qi[pbh:pbh + 32, hh // 4, i * 128:(i + 1) * 128], kiT[pbh:pbh + 32, k0:k0 + kn],
                           True, True, [qi_r, k_rs[kc_]], [ps_r], pbase_tp(pbh))
                        R, R_r, _ = Rr.next()
                        if hh % 2 == 0:
                            kb.op("act", lambda e: e.activation(out=R[:, 0:kn], in_=ps[:, 0:kn], func=AF.Relu), r=[ps_r], w=[R_r])
                        else:
                            kb.op("dve", lambda e: e.tensor_scalar(out=R[:, 0:kn], in0=ps[:, 0:kn], scalar1=0.0, scalar2=None,
                                                                   op0=ALU.max), r=[ps_r], w=[R_r])
                        Rs.append((R, R_r))
                    psc, psc_r, _ = pscr.next()
                    for hh in range(8):
                        mm(kb, psc[:, 0:kn], dg[:, hh, :], Rs[hh][0][:, 0:kn], hh == 0, hh == 7, [dg_r, Rs[hh][1]], [psc_r])
                    if kc_ == nkc - 1:
                        dcol = kn - 128
                        if dcol > 0:
                            kb.op("act", lambda e: e.activation(out=sc[:, k0:k0 + dcol], in_=psc[:, 0:dcol], func=AF.Copy),
                                  r=[psc_r], w=[sc_r])
                        kb.op("dve", lambda e: e.tensor_tensor(out=sc[:, k0 + dcol:k0 + kn], in0=psc[:, dcol:kn],
                                                               in1=self.cnegf, op=ALU.add), r=[psc_r, self.cm_r], w=[sc_r])
                    else:
                        kb.op("act", lambda e: e.activation(out=sc[:, k0:k0 + kn], in_=psc[:, 0:kn], func=AF.Copy),
                              r=[psc_r], w=[sc_r])

            def bisect_pair(c, P):
                def gen(iA, iB):
                    tiles = []
                    for slot, i in ((0, iA), (1, iB)):
                        scores(c, i, P, scs[slot], sc_rs[slot])
                        yield
                    for slot, i in ((0, iA), (1, iB)):
                        neg = (slot == 1)
                        sc, sc_r = scs[slot], sc_rs[slot]
                        N = (4 * c + i + 1) * 128
                        st, st_r, _ = bst.next()
                        kb.op("dve", lambda e: e.tensor_reduce(out=st[:, 0:1], in_=sc[:, 0:N], axis=mybir.AxisListType.X, op=ALU.max),
                              r=[sc_r], w=[st_r])
                        kb.op("dve", lambda e: e.tensor_reduce(out=st[:, 1:2], in_=sc[:, 0:N - 128], axis=mybir.AxisListType.X, op=ALU.min),
                              r=[sc_r], w=[st_r])
                        kb.op("dve", lambda e: e.tensor_tensor(out=st[:, 2:3], in0=st[:, 0:1], in1=st[:, 1:2], op=ALU.subtract), r=[st_r], w=[st_r])
                        kb.op("dve", lambda e: e.tensor_tensor(out=st[:, W0:W0 + NBIS + 1], in0=st[:, 2:3].to_broadcast([128, NBIS + 1]),
                                                               in1=wconst[:, 1 if neg else 0, :], op=ALU.mult), r=[st_r, wc_r], w=[st_r])
                        if not neg:
                            kb.op("dve", lambda e: e.tensor_tensor(out=st[:, M0:M0 + 1], in0=st[:, 1:2], in1=st[:, W0:W0 + 1], op=ALU.add), r=[st_r], w=[st_r])
                        else:
                            kb.op("dve", lambda e: e.tensor_tensor(out=st[:, M0:M0 + 1], in0=st[:, W0:W0 + 1], in1=st[:, 1:2], op=ALU.subtract), r=[st_r], w=[st_r])
                        tiles.append((i, N, sc, sc_r, st, st_r, neg))
                    yield
                    for k in range(NBIS):
                        for (i, N, sc, sc_r, st, st_r, neg) in tiles:
                            mid = st[:, M0 + k:M0 + k + 1]
                            if not neg:
                                kb.op("dve", lambda e: e.tensor_scalar(out=P["jD"][:, 0:N], in0=sc[:, 0:N], scalar1=mid, scalar2=0.0,
                                                                       op0=ALU.is_ge, op1=ALU.add, accum_out=st[:, 3:4]),
                                      r=[sc_r, st_r], w=[P["jD_r"], st_r])
                            else:
                                kb.op("act", lambda e: e.activation(out=P["jA"][:, 0:N], in_=sc[:, 0:N], func=AF.Sign, bias=mid,
                                                                    accum_out=st[:, 3:4]), r=[sc_r, st_r], w=[P["jA_r"], st_r])
                        for (i, N, sc, sc_r, st, st_r, neg) in tiles:
                            mid = st[:, M0 + k:M0 + k + 1]
                            cmpv = float(2 * TOPK - N) if neg else float(TOPK)
                            kb.op("dve", lambda e: e.tensor_scalar(out=st[:, 4:5], in0=st[:, 3:4], scalar1=cmpv, scalar2=st[:, W0 + k:W0 + k + 1],
                                                                   op0=ALU.is_ge, op1=ALU.mult), r=[st_r], w=[st_r])
                            kb.op("dve", lambda e: e.scalar_tensor_tensor(out=st[:, M0 + k + 1:M0 + k + 2], in0=st[:, 4:5],
                                                                          scalar=st[:, W0 + k + 1:W0 + k + 2], in1=mid,
                                                                          op0=ALU.subtract, op1=ALU.add), r=[st_r], w=[st_r])
                        yield
                    for (i, N, sc, sc_r, st, st_r, neg) in tiles:
                        mb, mb_r = mbs[c % 2][i], mb_rs[c % 2][i]
                        if not neg:
                            kb.op("dve", lambda e: e.tensor_tensor(out=st[:, 5:6], in0=st[:, M0 + NBIS:M0 + NBIS + 1],
                                                                   in1=st[:, W0 + NBIS:W0 + NBIS + 1], op=ALU.subtract), r=[st_r], w=[st_r])
                        else:
                            kb.op("dve", lambda e: e.scalar_tensor_tensor(out=st[:, 5:6], in0=st[:, M0 + NBIS:M0 + NBIS + 1], scalar=-1.0,
                                                                          in1=st[:, W0 + NBIS:W0 + NBIS + 1], op0=ALU.mult, op1=ALU.add),
                                  r=[st_r], w=[st_r])
                        kb.op("dve", lambda e: e.tensor_scalar(out=mb[:, 0:N], in0=sc[:, 0:N], scalar1=st[:, 5:6], scalar2=NEG_B,
                                                               op0=ALU.is_lt, op1=ALU.mult), r=[sc_r, st_r], w=[mb_r])
                        yield
                return gen

            def attn_heads(c, P, heads, oTs, oTs_r):
                qb, qb_r = P["qb"], P["qb_r"]
                for hd in heads:
                    cb, pb = hd // 2, (hd % 2) * 64
                    po, po_r, _ = por.next()

                    def mask_fn(j, col0):
                        ex = []
                        for i2 in range(col0 // 128, 4):
                            ex.append((mbs[c % 2][i2][:, j * 128:(j + 1) * 128], self.identb[:], i2 * 128, (i2 + 1) * 128,
                                       [mb_rs[c % 2][i2], self.cst_r]))
                        return ex

                    yield from self.causal_attn_gen(
                        c,
                        lambda j: kbT[pb:pb + 64, j * 128:(j + 1) * 128], lambda j: k_rs[j // 4],
                        lambda col0: qb[pb:pb + 64, cb, col0:512], qb_r,
                        lambda j: V1[:, j, :], lambda j: V_rs[j],
                        psr, ptr, po, po_r, 0.125, mask_fn=mask_fn)
                    self.attn_finalize(po, po_r, rdr, oTs[0:64, hd, :], oTs_r)
                    yield

            def zip_run(ga, na, gb, nb):
                done_a = done_b = False
                ia = ib = 0
                while not (done_a and done_b):
                    if not done_a and (done_b or ia * max(nb, 1) <= ib * max(na, 1)):
                        try:
                            next(ga)
                            ia += 1
                        except StopIteration:
                            done_a = True
                    else:
                        try:
                            next(gb)
                            ib += 1
                        except StopIteration:
                            done_b = True

            def empty():
                return
                yield

            Ps = {0: proj(0)}
            trivial(0, 0)
            trivial(0, 1)
            for _ in bisect_pair(0, Ps[0])(2, 3):
                pass
            for c in range(NCH):
                nxt = c + 1 < NCH
                if nxt:
                    Ps[c + 1] = proj(c + 1)
                oTs, oTs_r, o_ch = otr.next()
                for half in range(2):
                    ga = attn_heads(c, Ps[c], (2 * half, 2 * half + 1), oTs, oTs_r)
                    na = 2 * (4 * c + 5)
                    if nxt:
                        gb = bisect_pair(c + 1, Ps[c + 1])(2 * half, 2 * half + 1)
                        nb = NBIS + 5
                    else:
                        gb, nb = empty(), 0
                    zip_run(ga, na, gb, nb)
                self.store_oT(1, c, oTs, oTs_r, o_ch)

    def phase_diff(self, l):
        kb, S, NT, NCH = self.kb, self.S, self.NT, self.NCH
        lam_init = 0.8 - 0.6 * math.exp(-0.3 * l)
        with kb.phase() as ph:
            W = ph.sb([128, KC, 1280], BF16, "cW")
            W_r = Res()
            self.load_w(W[:, :, 0:1024], W_r, self.wC_fm[l], KC, "cW")
            self.load_w(W[:, :, 1024:1280], W_r, self.wC_tm[l], KC, "cW")
            kT = ph.sb([128, 2, S], BF16, "ckT")
            kT_rs = [Res() for _ in range(NCH)]
            V1 = ph.sb([128, NT, 4, 128], BF16, "cV1")
            V_rs = [Res() for _ in range(NT)]
            ones_r = Res()
            kb.op("pool", lambda e: e.memset(V1[:, :, :, 64:128], 1.0), w=[ones_r])
            for r_ in V_rs:
                r_.w = ones_r.w
            lm = ph.sb([128, 128], F32, "clm")
            ls = ph.sb([128, 8], F32, "cls")
            lm_r = Res()
            self.load_bc(lm[:], lm_r, self.dlam[l], 128, "clm")
            kb.op("dve", lambda e: e.tensor_tensor(out=lm[:, 0:32], in0=lm[:, 0:32], in1=lm[:, 32:64], op=ALU.mult), r=[lm_r], w=[lm_r])
            kb.op("dve", lambda e: e.tensor_tensor(out=lm[:, 64:96], in0=lm[:, 64:96], in1=lm[:, 96:128], op=ALU.mult), r=[lm_r], w=[lm_r])
            kb.op("dve", lambda e: e.tensor_reduce(out=ls[:, 0:1], in_=lm[:, 0:32], axis=mybir.AxisListType.X, op=ALU.add), r=[lm_r], w=[lm_r])
            kb.op("dve", lambda e: e.tensor_reduce(out=ls[:, 1:2], in_=lm[:, 64:96], axis=mybir.AxisListType.X, op=ALU.add), r=[lm_r], w=[lm_r])
            kb.op("act", lambda e: e.activation(out=ls[:, 2:4], in_=ls[:, 0:2], func=AF.Exp), r=[lm_r], w=[lm_r])
            kb.op("dve", lambda e: e.tensor_tensor(out=ls[:, 4:5], in0=ls[:, 2:3], in1=ls[:, 3:4], op=ALU.subtract), r=[lm_r], w=[lm_r])
            kb.op("dve", lambda e: e.tensor_scalar(out=ls[:, 5:6], in0=ls[:, 4:5], scalar1=lam_init, scalar2=-1.0, op0=ALU.add, op1=ALU.mult),
                  r=[lm_r], w=[lm_r])
            sg = ph.sb([128, 2], F32, "csg")
            sg_r = Res()
            kb.dma("sp", sg[:, 0:1], self.subg[l], "csg", w=[sg_r])
            kb.op("dve", lambda e: e.tensor_scalar(out=sg[:, 1:2], in0=sg[:, 0:1], scalar1=(1.0 - lam_init), scalar2=None, op0=ALU.mult),
                  r=[sg_r], w=[sg_r])
            hring = ph.ring(2, [128, KC, 512], BF16, "ch")
            tabr = ph.ring(1, [128, 2, 512], F32, "ctab")
            qring = ph.ring(2, [128, 2, 512], BF16, "cq")
            tmpr = ph.ring(4, [128, 512], F32, "ctmp")
            pp = ph.ring(2, [128, 512], F32, "cpp", psum=True)
            psr = ph.ring(3, [128, 512], F32, "cps", psum=True)
            por = ph.ring(2, [128, 512], F32, "cpo", psum=True)
            pssr = ph.ring(1, [128, 512], F32, "cpss", psum=True)
            ptr = ph.ring(3, [128, 512], BF16, "cpt")
            rdr = ph.ring(2, [128, 512], F32, "crd")
            otr = ph.ring(2, [128, 4, 512], BF16, "cot")
            o0r = ph.ring(2, [128, 512], F32, "co0")
            o1r = ph.ring(2, [128, 512], F32, "co1")
            sqr = ph.ring(2, [128, 512], F32, "csq")
            sqbr = ph.ring(2, [128, 512], BF16, "csqb")
            for c in range(NCH):
                h, h_r = self.load_hT(self.hT, hring, c)
                tab, tab_r, t_ch = tabr.next()
                kb.dma("sp", tab[:], self.rope[2:4, :, c * 512:(c + 1) * 512].rearrange("t p s -> p t s"), t_ch, w=[tab_r])
                qT, qT_r, _ = qring.next()

                def roped2(col, colp, dst, dst_r):
                    ps, ps_r, _ = pp.next()
                    self.proj_fm(ps, ps_r, W, W_r, col, h, h_r)
                    p2, p2_r, _ = pp.next()
                    self.proj_fm(p2, p2_r, W, W_r, colp, h, h_r)
                    self.rope_evac(ps[:], ps_r, p2[:], p2_r, tab[:, 0, :], tab[:, 1, :], tab_r, tmpr, dst, dst_r)

                for ci in range(2):
                    roped2(ci * 128, 256 + ci * 128, qT[:, ci, :], qT_r)
                for ci in range(2):
                    roped2(512 + ci * 128, 768 + ci * 128, kT[:, ci, c * 512:(c + 1) * 512], kT_rs[c])
                for i in range(4):
                    t = 4 * c + i
                    ps, ps_r, _ = pp.next()
                    self.proj_tm(ps, ps_r, W, W_r, 1024, 256, h, h_r, i)
                    kb.op("act", lambda e: e.activation(out=V1[:, t, :, 0:64], in_=ps[:, 0:256].rearrange("p (h d) -> p h d", h=4),
                                                        func=AF.Copy), r=[ps_r], w=[V_rs[t]])
                oTs, oTs_r, o_ch = otr.next()
                for hd in range(4):
                    cb = hd // 2
                    outs = []
                    for m in range(2):
                        pb = ((hd % 2) * 2 + m) * 32
                        po, po_r, _ = por.next()
                        self.causal_attn(
                            c,
                            lambda j: kT[pb:pb + 32, cb, j * 128:(j + 1) * 128], lambda j: kT_rs[j // 4],
                            lambda col0: qT[pb:pb + 32, cb, col0:512], qT_r,
                            lambda j: V1[:, j, hd, :], lambda j: V_rs[j],
                            psr, ptr, po, po_r, 32.0 ** -0.5, tp=pbase_tp(pb))
                        o, o_r, _ = (o0r if m == 0 else o1r).next()
                        self.attn_finalize(po, po_r, rdr, o[0:64, :], o_r)
                        outs.append((o, o_r))
                    (o0, o0_r), (o1, o1_r) = outs
                    kb.op("dve", lambda e: e.scalar_tensor_tensor(out=o0[0:64, :], in0=o1[0:64, :], scalar=ls[0:64, 5:6], in1=o0[0:64, :],
                                                                  op0=ALU.mult, op1=ALU.add), r=[o0_r, o1_r, lm_r], w=[o0_r])
                    sq, sq_r, _ = sqr.next()
                    sqb, sqb_r, _ = sqbr.next()
                    kb.op("pool", lambda e: e.tensor_tensor(out=sqb[0:64, :], in0=o0[0:64, :], in1=o0[0:64, :], op=ALU.mult), r=[o0_r], w=[sqb_r])
                    pss, pss_r, _ = pssr.next()
                    mm(kb, pss[0:64, :], self.onesb[0:64, 0:64], sqb[0:64, :], True, True, [sqb_r, self.cst_r], [pss_r])
                    kb.op("dve", lambda e: e.tensor_scalar(out=sq[0:64, :], in0=pss[0:64, :], scalar1=1.0 / 64, scalar2=EPS,
                                                           op0=ALU.mult, op1=ALU.add), r=[pss_r], w=[sq_r])
                    kb.op("act", lambda e: e.activation(out=sq[0:64, :], in_=sq[0:64, :], func=AF.Ln), r=[sq_r], w=[sq_r])
                    kb.op("act", lambda e: e.activation(out=sq[0:64, :], in_=sq[0:64, :], func=AF.Exp, scale=-0.5), r=[sq_r], w=[sq_r])
                    kb.op("dve", lambda e: e.scalar_tensor_tensor(out=oTs[0:64, hd, :], in0=o0[0:64, :], scalar=sg[0:64, 1:2], in1=sq[0:64, :],
                                                                  op0=ALU.mult, op1=ALU.mult), r=[o0_r, sq_r, sg_r], w=[oTs_r])
                self.store_oT(2, c, oTs, oTs_r, o_ch)

    def phase_mem(self, l):
        kb, S, NT, NCH = self.kb, self.S, self.NT, self.NCH
        with kb.phase() as ph:
            W = ph.sb([128, KC, 768], BF16, "mW")
            W_r = Res()
            self.load_w(W[:, :, 0:256], W_r, self.wM_fm[l], KC, "mW")
            self.load_w(W[:, :, 256:768], W_r, self.w_kv[l], KC, "mW")
            nm = Prog.Normer(self, ph, self.g_mem[l], "mg")
            xr = ph.ring(2, [128, D], F32, "mx")
            hm = ph.sb([128, KC, MEM], BF16, "mhm")
            hm_r = Res()
            for t in range(2):
                xt, xt_r, x_ch = xr.next()
                kb.dma("sp", xt[:], self.mem_in[t * 128:(t + 1) * 128, :], x_ch, w=[xt_r])
                nm.to_hT(xt[:], xt_r, hm[:, :, t * 128:(t + 1) * 128], hm_r)
            KmT = ph.sb([128, 2, MEM], BF16, "mK")
            Vm = ph.sb([128, 2, 4, 128], BF16, "mV")
            kv_r = Res()
            kb.op("pool", lambda e: e.memset(Vm[:, :, :, 64:128], 1.0), w=[kv_r])
            pp = ph.ring(2, [128, 512], F32, "mpp", psum=True)
            psr = ph.ring(2, [128, 512], F32, "mps", psum=True)
            por = ph.ring(2, [128, 512], F32, "mpo", psum=True)
            for ci in range(2):
                ps, ps_r, _ = pp.next()
                self.proj_fm(ps, ps_r, W, W_r, 256 + ci * 128, hm, hm_r, n=MEM)
                kb.op("act", lambda e: e.activation(out=KmT[:, ci, :], in_=ps[:, 0:MEM], func=AF.Copy), r=[ps_r], w=[kv_r])
            for t in range(2):
                ps, ps_r, _ = pp.next()
                self.proj_tm(ps, ps_r, W, W_r, 512, 256, hm, hm_r, t)
                kb.op("act", lambda e: e.activation(out=Vm[:, t, :, 0:64], in_=ps[:, 0:256].rearrange("p (h d) -> p h d", h=4),
                                                    func=AF.Copy), r=[ps_r], w=[kv_r])
            hring = ph.ring(2, [128, KC, 512], BF16, "mh")
            qring = ph.ring(2, [128, 2, 512], BF16, "mq")
            ptr = ph.ring(3, [128, 512], BF16, "mpt")
            rdr = ph.ring(2, [128, 512], F32, "mrd")
            otr = ph.ring(2, [128, 4, 512], BF16, "mot")
            for c in range(NCH):
                h, h_r = self.load_hT(self.hT, hring, c)
                qT, qT_r, _ = qring.next()
                for ci in range(2):
                    ps, ps_r, _ = pp.next()
                    self.proj_fm(ps, ps_r, W, W_r, ci * 128, h, h_r)
                    kb.op("act", lambda e: e.activation(out=qT[:, ci, :], in_=ps[:], func=AF.Copy), r=[ps_r], w=[qT_r])
                oTs, oTs_r, o_ch = otr.next()
                for hd in range(4):
                    cb, pb = hd // 2, (hd % 2) * 64
                    po, po_r, _ = por.next()
                    for t in range(2):
                        ps, ps_r, _ = psr.next()
                        mm(kb, ps[:], KmT[pb:pb + 64, cb, t * 128:(t + 1) * 128], qT[pb:pb + 64, cb, :], True, True, [kv_r, qT_r], [ps_r])
                        pt, pt_r, _ = ptr.next()
                        kb.op("act", lambda e: e.activation(out=pt[:], in_=ps[:], func=AF.Exp, scale=0.125), r=[ps_r], w=[pt_r])
                        mm(kb, po[:], Vm[:, t, hd, :], pt[:], t == 0, t == 1, [kv_r, pt_r], [po_r])
                    self.attn_finalize(po, po_r, rdr, oTs[0:64, hd, :], oTs_r)
                self.store_oT(3, c, oTs, oTs_r, o_ch)

    def phase_merge(self, l):
        kb, S, NT, NCH = self.kb, self.S, self.NT, self.NCH
        x_src = self.x_in if l == 0 else self.xres
        with kb.phase() as ph:
            nb = len(self.branches)
            Wg = ph.sb([128, KC, 4096], BF16, "gWg")
            Wb = ph.sb([128, 4, 2, D], BF16, "gWb")
            Wo = ph.sb([128, KC, D], BF16, "gWo")
            bg = ph.sb([128, 32], F32, "gbg")
            Wg_r, Wb_r, Wo_r, bg_r = Res(), Res(), Res(), Res()
            kb.dma("sp", bg[:], self.b_gate[l], "gbg", w=[bg_r])
            for n in self.branches:
                self.load_w(Wb[:, n, :, :], Wb_r, self.w_branch[l, n], 2, "gWb")
            for k in range(KC):
                for n in self.branches:
                    kb.dma("pool", Wg[:, k, n * 1024:(n + 1) * 1024], self.w_gate[l, k * 128:(k + 1) * 128, n * 1024:(n + 1) * 1024],
                           "gWg", w=[Wg_r])
            self.load_w(Wo, Wo_r, self.w_out[l], KC, "gWo")
            nm = Prog.Normer(self, ph, self.g_ffn[l], "gng")
            hring = ph.ring(1, [128, KC, 512], BF16, "gh")
            oring = ph.ring(1, [128, 4, 2, 512], BF16, "go")
            xr = ph.ring(3, [128, D], F32, "gx")
            gsr = ph.ring(2, [128, 512], F32, "ggs")
            tmr = ph.ring(2, [128, 512], F32, "gtm")
            acr = ph.ring(2, [128, 512], F32, "gac")
            mT = ph.sb([128, KC, 512], BF16, "gmT")
            mT_r = Res()
            h2r = ph.ring(1, [128, KC, 512], BF16, "gh2")
            pyr = ph.ring(2, [128, 512], F32, "gpy", psum=True)
            pgr = ph.ring(2, [128, 512], F32, "gpg", psum=True)
            pxr = ph.ring(2, [128, 512], F32, "gpx", psum=True)
            for c in range(NCH):
                h, h_r = self.load_hT(self.hT, hring, c)
                o, o_r, o_ch = oring.next()
                for n in self.branches:
                    kb.dma("sp", o[:, n, :, :], self.oT[n, :, c * 512:(c + 1) * 512].rearrange("(k p) s -> p k s", p=128), o_ch, w=[o_r])
                for f in range(KC):
                    acc, acc_r, _ = acr.next()
                    for bi_, n in enumerate(self.branches):
                        py, py_r, _ = pyr.next()
                        for k in range(2):
                            mm(kb, py[:], Wb[:, n, k, f * 128:(f + 1) * 128], o[:, n, k, :], k == 0, k == 1, [Wb_r, o_r], [py_r])
                        pg, pg_r, _ = pgr.next()
                        for k in range(KC):
                            mm(kb, pg[:], Wg[:, k, n * 1024 + f * 128:n * 1024 + (f + 1) * 128], h[:, k, :], k == 0, k == KC - 1,
                               [Wg_r, h_r], [pg_r])
                        gs, gs_r, _ = gsr.next()
                        kb.op("act", lambda e: e.activation(out=gs[:], in_=pg[:], func=AF.Sigmoid, bias=bg[:, n * 8 + f:n * 8 + f + 1]),
                              r=[pg_r, bg_r], w=[gs_r])
                        lastb = (bi_ == nb - 1)
                        if bi_ == 0:
                            dst, dst_r = (mT[:, f, :], mT_r) if lastb else (acc[:], acc_r)
                            kb.op("dve", lambda e: e.tensor_tensor(out=dst, in0=py[:], in1=gs[:], op=ALU.mult), r=[py_r, gs_r], w=[dst_r])
                        else:
                            tm, tm_r, _ = tmr.next()
                            kb.op("dve", lambda e: e.tensor_tensor(out=tm[:], in0=py[:], in1=gs[:], op=ALU.mult), r=[py_r, gs_r], w=[tm_r])
                            dst, dst_r = (mT[:, f, :], mT_r) if lastb else (acc[:], acc_r)
                            kb.op("pool", lambda e: e.tensor_tensor(out=dst, in0=acc[:], in1=tm[:], op=ALU.add), r=[acc_r, tm_r], w=[dst_r] if lastb else [acc_r])
                h2, h2_r, h2_ch = h2r.next()
                for i in range(4):
                    t = 4 * c + i
                    xt, xt_r, x_ch = xr.next()
                    kb.dma("sp", xt[:], x_src[t * 128:(t + 1) * 128, :], x_ch, w=[xt_r])
                    for hh in range(2):
                        px, px_r, _ = pxr.next()
                        for k in range(KC):
                            mm(kb, px[:], mT[:, k, i * 128:(i + 1) * 128], Wo[:, k, hh * 512:(hh + 1) * 512], k == 0, k == KC - 1,
                               [mT_r, Wo_r], [px_r])
                        kb.op("dve", lambda e: e.tensor_tensor(out=xt[:, hh * 512:(hh + 1) * 512], in0=px[:], in1=xt[:, hh * 512:(hh + 1) * 512],
                                                               op=ALU.add), r=[px_r, xt_r], w=[xt_r])
                    kb.dma("sp", self.xres[t * 128:(t + 1) * 128, :], xt[:], x_ch, r=[xt_r])
                    nm.to_hT(xt[:], xt_r, h2[:, :, i * 128:(i + 1) * 128], h2_r)
                kb.dma("sp", self.h2T[:, :, c * 512:(c + 1) * 512].rearrange("k p s -> p k s"), h2[:], h2_ch, r=[h2_r])

    def phase_ffn(self, l, j0, j1, last):
        kb, S, NT, NCH = self.kb, self.S, self.NT, self.NCH
        nj = j1 - j0
        final = last and (l == self.depth - 1)
        with kb.phase() as ph:
            W1 = ph.sb([128, KC, 2, nj * 128], BF16, "fW1")
            W2 = ph.sb([128, nj, D], BF16, "fW2")
            W1_r, W2_r = Res(), Res()
            for k in range(KC):
                for u in range(2):
                    kb.dma("pool", W1[:, k, u, :], self.w_f1[l, k * 128:(k + 1) * 128, u * FFH + j0 * 128:u * FFH + j1 * 128], "fW1", w=[W1_r])
            self.load_w(W2, W2_r, self.w_f2[l, j0 * 128:j1 * 128, :], nj, "fW2")
            if last:
                g_row = self.g_fin if final else self.g_mix[l + 1]
                nm = Prog.Normer(self, ph, g_row, "fng")
            hring = ph.ring(2, [128, KC, 512], BF16, "fh")
            xr = ph.ring(3, [128, D], F32, "fx")
            sir = ph.ring(2, [128, 512], F32, "fsi")
            aT = ph.sb([128, nj, 512], BF16, "faT")
            aT_r = Res()
            hor = ph.ring(1, [128, KC, 512], BF16, "fho")
            outr = ph.ring(2, [128, D], F32, "fout")
            p1r = ph.ring(2, [128, 512], F32, "fp1", psum=True)
            p2r = ph.ring(2, [128, 512], F32, "fp2", psum=True)
            pxr = ph.ring(2, [128, 512], F32, "fpx", psum=True)
            for c in range(NCH):
                h, h_r = self.load_hT(self.h2T, hring, c)
                for j in range(nj):
                    p1, p1_r, _ = p1r.next()
                    for k in range(KC):
                        mm(kb, p1[:], W1[:, k, 0, j * 128:(j + 1) * 128], h[:, k, :], k == 0, k == KC - 1, [W1_r, h_r], [p1_r])
                    p2, p2_r, _ = p2r.next()
                    for k in range(KC):
                        mm(kb, p2[:], W1[:, k, 1, j * 128:(j + 1) * 128], h[:, k, :], k == 0, k == KC - 1, [W1_r, h_r], [p2_r])
                    si, si_r, _ = sir.next()
                    kb.op("act", lambda e: e.activation(out=si[:], in_=p1[:], func=AF.Silu), r=[p1_r], w=[si_r])
                    kb.op("dve", lambda e: e.tensor_tensor(out=aT[:, j, :], in0=p2[:], in1=si[:], op=ALU.mult), r=[p2_r, si_r], w=[aT_r])
                if last:
                    ho, ho_r, ho_ch = hor.next()
                for i in range(4):
                    t = 4 * c + i
                    xt, xt_r, x_ch = xr.next()
                    kb.dma("sp", xt[:], self.xres[t * 128:(t + 1) * 128, :], x_ch, w=[xt_r])
                    for hh in range(2):
                        px, px_r, _ = pxr.next()
                        for j in range(nj):
                            mm(kb, px[:], aT[:, j, i * 128:(i + 1) * 128], W2[:, j, hh * 512:(hh + 1) * 512], j == 0, j == nj - 1,
                               [aT_r, W2_r], [px_r])
                        kb.op("dve", lambda e: e.tensor_tensor(out=xt[:, hh * 512:(hh + 1) * 512], in0=px[:], in1=xt[:, hh * 512:(hh + 1) * 512],
                                                               op=ALU.add), r=[px_r, xt_r], w=[xt_r])
                    if final:
                        ot, ot_r, ot_ch = outr.next()
                        nm.to_out(xt[:], xt_r, ot[:], ot_r)
                        kb.dma("sp", self.out[t * 128:(t + 1) * 128, :], ot[:], ot_ch, r=[ot_r])
                    else:
                        kb.dma("sp", self.xres[t * 128:(t + 1) * 128, :], xt[:], x_ch, r=[xt_r])
                        if last:
                            nm.to_hT(xt[:], xt_r, ho[:, :, i * 128:(i + 1) * 128], ho_r)
                if last and not final:
                    kb.dma("sp", self.hT[:, :, c * 512:(c + 1) * 512].rearrange("k p s -> p k s"), ho[:], ho_ch, r=[ho_r])


IN_OFF = {}
_off = 0
for _n, _w in (("qa", 256), ("ka", 256), ("va", 256), ("fa", 4), ("qb", 256), ("kb", 64), ("vb", 64), ("qi", 256), ("ki", 32),
               ("wi", 8), ("qc", 256), ("kc", 256), ("vc", 256), ("qm", 256)):
    IN_OFF[_n] = (_off, _off + _w)
    _off += _w


def _cols(name):
    a, b = IN_OFF[name]
    return np.arange(a, b)


def _partner(cols, hd, half):
    c = cols.copy().reshape(-1, hd)
    p = c.copy()
    p[:, 0:half] = c[:, half:2 * half]
    p[:, half:2 * half] = c[:, 0:half]
    return p.reshape(-1)


def _host_consts():
    ident = np.eye(128, dtype=np.float32)
    k = np.arange(128)[:, None]
    q = np.arange(128)[None, :]
    tri = (k <= q).astype(np.float32)
    cneg = np.where(q > k, np.float32(NEG_S), np.float32(0.0)).astype(np.float32)
    cmat = np.concatenate([ident, tri, cneg], axis=1).astype(np.float32)
    p = np.arange(128)
    ccol = np.zeros((128, 4), np.float32)
    for pat, (hd, rot) in enumerate(((64, 16), (32, 8))):
        half = rot // 2
        inv = (np.float32(ROPE_THETA) ** (-(np.arange(0, rot, 2, dtype=np.float32) / np.float32(rot)))).astype(np.float32)
        d = p % hd
        ccol[:, pat] = np.where(d < rot, inv[d % half], 0.0)
        ccol[:, 2 + pat] = np.where(d < half, -1.0, np.where(d < rot, 1.0, 0.0))
    return cmat, ccol


def _prep_shared(inp):
    w_in = np.asarray(inp["w_in"], np.float32)
    L = w_in.shape[0]
    g = lambda cols: np.ascontiguousarray(w_in[:, :, cols])
    qb, kb_, qi, ki, qc, kc = _cols("qb"), _cols("kb"), _cols("qi"), _cols("ki"), _cols("qc"), _cols("kc")
    kb2 = np.concatenate([kb_, kb_])
    ki4 = np.concatenate([ki, ki, ki, ki])
    cmat, ccol = _host_consts()
    sh = {
        "cmat": cmat, "ccol": ccol,
        "g_mix": np.asarray(inp["norm_mix_g"], np.float32).reshape(L, 1, D),
        "g_ffn": np.asarray(inp["norm_ffn_g"], np.float32).reshape(L, 1, D),
        "g_mem": np.asarray(inp["norm_mem_g"], np.float32).reshape(L, 1, D),
        "g_fin": np.asarray(inp["final_norm_g"], np.float32).reshape(1, D),
        "wA_fm": g(np.concatenate([_cols("qa"), _cols("ka")])),
        "wA_tm": g(np.concatenate([_cols("va"), _cols("fa")])),
        "wB_fm": g(np.concatenate([qb, _partner(qb, 64, 8), kb2, _partner(kb2, 64, 8), qi, _partner(qi, 32, 4), ki4, _partner(ki4, 32, 4)])),
        "wB_tm": g(np.concatenate([_cols("vb"), _cols("wi")])),
        "wC_fm": g(np.concatenate([qc, _partner(qc, 32, 4), kc, _partner(kc, 32, 4)])),
        "wC_tm": g(_cols("vc")),
        "wM_fm": g(_cols("qm")),
        "w_kv": np.ascontiguousarray(np.asarray(inp["w_mem_kv"], np.float32)),
        "b_forget": np.asarray(inp["b_forget"], np.float32).reshape(L, 1, 4),
        "dlam": np.asarray(inp["diff_lambda"], np.float32).reshape(L, 1, 128),
        "subg": np.ascontiguousarray(np.tile(np.asarray(inp["diff_subln_g"], np.float32), (1, 2)).reshape(L, 128, 1)),
        "w_branch": np.ascontiguousarray(np.asarray(inp["w_branch"], np.float32)),
        "w_gate": np.ascontiguousarray(np.asarray(inp["w_gate"], np.float32)),
        "b_gate": np.ascontiguousarray(np.asarray(inp["b_gate"], np.float32).reshape(L, 32, 128).transpose(0, 2, 1)),
        "w_out": np.ascontiguousarray(np.asarray(inp["w_out"], np.float32)),
        "w_f1": np.ascontiguousarray(np.asarray(inp["w_ffn_in"], np.float32)),
        "w_f2": np.ascontiguousarray(np.asarray(inp["w_ffn_out"], np.float32)),
    }
    return sh


_PROG_CACHE = {}


def run(inp, S, depth, n_cores, branches=(0, 1, 2, 3), trace=False):
    key = (S, depth, tuple(branches))
    if key not in _PROG_CACHE:
        _PROG_CACHE[key] = Prog(S, depth, branches)
    prog = _PROG_CACHE[key]
    sh = _prep_shared(inp)
    x = np.asarray(inp["x"], np.float32)
    mem = np.asarray(inp["mem"], np.float32)
    pos = np.asarray(inp["positions"], np.int32)
    in_maps = []
    for b in range(n_cores):
        m = dict(sh)
        m["x"] = np.ascontiguousarray(x[b])
        m["mem"] = np.ascontiguousarray(mem[b])
        m["pos"] = np.ascontiguousarray(pos[b].reshape(1, S))
        in_maps.append(m)
    res = run_bass_kernel_spmd(prog.nc, in_maps, core_ids=list(range(n_cores)), trace=trace)
    out = np.stack([np.asarray(r["out"], np.float32) for r in res.results], axis=0)
    return out, res


def kernel(**inputs):
    out, _ = run(inputs, 4096, 4, 8)
    return out
```
